# Optimizing a Trainium2 kernel written in Bass

```python
import jax, jax.numpy as jnp
from jax import lax
import numpy as np

D_MODEL = 1024
BATCH = 8
SEQ = 2048
DEPTH = 1

GRID_W = 64
CTX_LEN = 256
CONV_W = 512
CONV_K = 3
N_HEADS = 8
N_KV_HEADS = 2
HEAD_DIM = 64
ATT_W = N_HEADS * HEAD_DIM
KV_W = N_KV_HEADS * HEAD_DIM
WINDOW = 128
BLOCK = 128
ROPE_BASE = 10000.0
N_GROUPS = 4
EXPERTS_PER_GROUP = 8
N_EXPERTS = N_GROUPS * EXPERTS_PER_GROUP
TOP_K = 2
EXPERT_FF = 256
N_MOD = 6
NORM_EPS = 1e-6
NEG_INF = -1e30
IN_SIZES = (CONV_W, CONV_W, CONV_W, ATT_W, KV_W, KV_W, D_MODEL, D_MODEL)
IN_COLS = sum(IN_SIZES)
KV_OFF = 3 * CONV_W + ATT_W

kernel_name = 'hybrid_shortconv_swa_hmoe_dit_block'


def rmsnorm(x, g):
    xf = x.astype(jnp.float32)
    inv = lax.rsqrt(jnp.mean(xf * xf, axis=-1, keepdims=True) + NORM_EPS)
    return (xf * inv).astype(x.dtype) * g


def modulate(h, shift, scale):
    return h * (1 + scale) + shift


def split_in(u):
    idx = np.cumsum(IN_SIZES)[:-1].tolist()
    return jnp.split(u, idx, axis=-1)


def axial_rope(rows):
    n_freq = HEAD_DIM // 4
    inv_freq = ROPE_BASE ** (-jnp.arange(n_freq, dtype=jnp.float32) / n_freq)
    row = jnp.repeat(jnp.arange(rows, dtype=jnp.float32), GRID_W)
    col = jnp.tile(jnp.arange(GRID_W, dtype=jnp.float32), rows)
    ang = jnp.concatenate([row[:, None] * inv_freq, col[:, None] * inv_freq], axis=-1)
    return jnp.cos(ang), jnp.sin(ang)


def apply_rope(t, cos, sin):
    half = HEAD_DIM // 2
    c = cos[:, None, :].astype(t.dtype)
    s = sin[:, None, :].astype(t.dtype)
    t1, t2 = t[..., :half], t[..., half:]
    return jnp.concatenate([t1 * c - t2 * s, t1 * s + t2 * c], axis=-1)


def short_conv_branch(b, cg, xin, w_conv, b_conv, w_a):
    u = cg * xin
    L = u.shape[1]
    pad = CONV_K // 2
    up = jnp.pad(u, ((0, 0), (pad, pad), (0, 0)))
    y = b_conv + sum(up[:, j:j + L] * w_conv[j] for j in range(CONV_K))
    return (b * y) @ w_a


def window_attention(q, k, v, k_ctx, v_ctx, sink):
    B, L = q.shape[:2]
    C = k_ctx.shape[1]
    nb = L // BLOCK
    G = N_HEADS // N_KV_HEADS
    scale = HEAD_DIM ** -0.5
    qb = q.reshape(B, nb, BLOCK, N_KV_HEADS, G, HEAD_DIM)

    def band(t):
        tp = jnp.pad(t, ((0, 0), (BLOCK, BLOCK), (0, 0), (0, 0)))
        tp = tp.reshape(B, nb + 2, BLOCK, N_KV_HEADS, HEAD_DIM)
        return jnp.concatenate([tp[:, :-2], tp[:, 1:-1], tp[:, 2:]], axis=2)

    kw, vw = band(k), band(v)
    s_win = jnp.einsum('bnqkgd,bnjkd->bnkgqj', qb, kw).astype(jnp.float32) * scale
    s_ctx = jnp.einsum('bnqkgd,bckd->bnkgqc', qb, k_ctx).astype(jnp.float32) * scale
    blk = jnp.arange(nb)[:, None, None] * BLOCK
    qpos = blk + jnp.arange(BLOCK)[None, :, None]
    kpos = blk - BLOCK + jnp.arange(3 * BLOCK)[None, None, :]
    valid = (jnp.abs(qpos - kpos) <= WINDOW) & (kpos >= 0) & (kpos < L)
    s_win = jnp.where(valid[None, :, None, None], s_win, NEG_INF)
    s_sink = jnp.broadcast_to(sink.astype(jnp.float32).reshape(N_KV_HEADS, G, 1, 1), s_win.shape[:-1] + (1,))
    p = jax.nn.softmax(jnp.concatenate([s_win, s_ctx, s_sink], axis=-1), axis=-1).astype(v.dtype)
    w3 = 3 * BLOCK
    o = (jnp.einsum('bnkgqj,bnjkd->bnqkgd', p[..., :w3], vw)
         + jnp.einsum('bnkgqc,bckd->bnqkgd', p[..., w3:w3 + C], v_ctx))
    return o.reshape(B, L, ATT_W)


def context_attention(q, k, v, sink):
    B, C = q.shape[:2]
    G = N_HEADS // N_KV_HEADS
    qg = q.reshape(B, C, N_KV_HEADS, G, HEAD_DIM)
    s = jnp.einsum('bqkgd,bckd->bkgqc', qg, k).astype(jnp.float32) * HEAD_DIM ** -0.5
    s_sink = jnp.broadcast_to(sink.astype(jnp.float32).reshape(N_KV_HEADS, G, 1, 1), s.shape[:-1] + (1,))
    p = jax.nn.softmax(jnp.concatenate([s, s_sink], axis=-1), axis=-1).astype(v.dtype)
    o = jnp.einsum('bkgqc,bckd->bqkgd', p[..., :C], v)
    return o.reshape(B, C, ATT_W)


def context_kv(hc, w_in):
    B, C = hc.shape[:2]
    k = (hc @ w_in[:, KV_OFF:KV_OFF + KV_W]).reshape(B, C, N_KV_HEADS, HEAD_DIM)
    v = (hc @ w_in[:, KV_OFF + KV_W:KV_OFF + 2 * KV_W]).reshape(B, C, N_KV_HEADS, HEAD_DIM)
    return k, v


def token_mixer_context(hc, w_in, w_conv, b_conv, w_a, w_b, sink, w_o):
    B, C = hc.shape[:2]
    b, cg, xin, q, k, v, ga, gb = split_in(hc @ w_in)
    k = k.reshape(B, C, N_KV_HEADS, HEAD_DIM)
    v = v.reshape(B, C, N_KV_HEADS, HEAD_DIM)
    ya = short_conv_branch(b, cg, xin, w_conv, b_conv, w_a)
    yb = context_attention(q.reshape(B, C, N_HEADS, HEAD_DIM), k, v, sink) @ w_b
    y = (jax.nn.sigmoid(ga) * ya + jax.nn.sigmoid(gb) * yb) @ w_o
    return y, k, v


def token_mixer_latent(h, k_ctx, v_ctx, cos, sin, w_in, w_conv, b_conv, w_a, w_b, sink, w_o):
    B, L = h.shape[:2]
    b, cg, xin, q, k, v, ga, gb = split_in(h @ w_in)
    ya = short_conv_branch(b, cg, xin, w_conv, b_conv, w_a)
    q = apply_rope(q.reshape(B, L, N_HEADS, HEAD_DIM), cos, sin)
    k = apply_rope(k.reshape(B, L, N_KV_HEADS, HEAD_DIM), cos, sin)
    v = v.reshape(B, L, N_KV_HEADS, HEAD_DIM)
    yb = window_attention(q, k, v, k_ctx, v_ctx, sink) @ w_b
    return (jax.nn.sigmoid(ga) * ya + jax.nn.sigmoid(gb) * yb) @ w_o


def hierarchical_moe(h, w_group, b_group, w_router, b_router, w_up, w_down):
    shp = h.shape
    t = h.reshape(-1, shp[-1])
    n = t.shape[0]
    g_prob = jax.nn.softmax((t @ w_group + b_group).astype(jnp.float32), axis=-1)
    p_g, g_idx = lax.top_k(g_prob, 1)
    e_logits = (t @ w_router + b_router).astype(jnp.float32).reshape(n, N_GROUPS, EXPERTS_PER_GROUP)
    e_sel = jnp.take_along_axis(e_logits, g_idx[:, :, None], axis=1)[:, 0]
    top_p, top_i = lax.top_k(jax.nn.softmax(e_sel, axis=-1), TOP_K)
    gate = p_g * top_p / jnp.sum(top_p, axis=-1, keepdims=True)
    ids = g_idx * EXPERTS_PER_GROUP + top_i
    combine = jnp.sum(jax.nn.one_hot(ids, N_EXPERTS, dtype=jnp.float32) * gate[..., None], axis=1).astype(t.dtype)
    y = jnp.zeros_like(t)
    for e in range(N_EXPERTS):
        a, u = jnp.split(t @ w_up[e], 2, axis=-1)
        y = y + combine[:, e:e + 1] * ((jax.nn.silu(a) * u) @ w_down[e])
    return y.reshape(shp)


def setup_inputs(seed: int = 0) -> dict:
    key = jax.random.key(seed)
    ks = jax.random.split(key, 24)
    f32 = jnp.float32
    D = D_MODEL

    def nrm(k, shape, scale):
        return jax.random.normal(k, shape, f32) * scale

    return {
        'x': nrm(ks[0], (BATCH, SEQ, D), 1.0),
        'c': nrm(ks[1], (BATCH, D), 1.0),
        'ctx': nrm(ks[2], (BATCH, CTX_LEN, D), 1.0),
        'c_ctx': nrm(ks[3], (D,), 1.0),
        'w_ada': nrm(ks[4], (DEPTH, D, N_MOD * D), 0.5 * D ** -0.5),
        'b_ada': nrm(ks[5], (DEPTH, N_MOD * D), 0.02),
        'norm1_g': 1.0 + nrm(ks[6], (DEPTH, D), 0.01),
        'w_in': nrm(ks[7], (DEPTH, D, IN_COLS), D ** -0.5),
        'w_conv': nrm(ks[8], (DEPTH, CONV_K, CONV_W), CONV_K ** -0.5),
        'b_conv': nrm(ks[9], (DEPTH, CONV_W), 0.02),
        'w_a': nrm(ks[10], (DEPTH, CONV_W, D), CONV_W ** -0.5),
        'w_b': nrm(ks[11], (DEPTH, ATT_W, D), ATT_W ** -0.5),
        'sink': nrm(ks[12], (DEPTH, N_HEADS), 0.5),
        'w_o': nrm(ks[13], (DEPTH, D, D), D ** -0.5),
        'norm2_g': 1.0 + nrm(ks[14], (DEPTH, D), 0.01),
        'w_group': nrm(ks[15], (DEPTH, D, N_GROUPS), D ** -0.5),
        'b_group': nrm(ks[16], (DEPTH, N_GROUPS), 0.01),
        'w_router': nrm(ks[17], (DEPTH, D, N_EXPERTS), D ** -0.5),
        'b_router': nrm(ks[18], (DEPTH, N_EXPERTS), 0.01),
        'w_up': nrm(ks[19], (DEPTH, N_EXPERTS, D, 2 * EXPERT_FF), D ** -0.5),
        'w_down': nrm(ks[20], (DEPTH, N_EXPERTS, EXPERT_FF, D), EXPERT_FF ** -0.5),
        'final_g': 1.0 + nrm(ks[21], (D,), 0.01),
    }


def reference(x, c, ctx, c_ctx, w_ada, b_ada, norm1_g, w_in, w_conv, b_conv, w_a, w_b, sink, w_o,
              norm2_g, w_group, b_group, w_router, b_router, w_up, w_down, final_g):
    L = x.shape[1]
    rows = L // GRID_W
    cos, sin = axial_rope(rows)
    xc = ctx
    for l in range(DEPTH):
        last = l == DEPTH - 1
        mod = (jax.nn.silu(c) @ w_ada[l] + b_ada[l])[:, None, :]
        sh1, sc1, g1, sh2, sc2, g2 = jnp.split(mod, N_MOD, axis=-1)
        modc = jax.nn.silu(c_ctx) @ w_ada[l] + b_ada[l]
        csh1, csc1, cg1, csh2, csc2, cg2 = jnp.split(modc, N_MOD, axis=-1)

        hc = modulate(rmsnorm(xc, norm1_g[l]), csh1, csc1)
        if last:
            k_ctx, v_ctx = context_kv(hc, w_in[l])
        else:
            yc, k_ctx, v_ctx = token_mixer_context(hc, w_in[l], w_conv[l], b_conv[l], w_a[l], w_b[l], sink[l], w_o[l])
            xc = xc + cg1 * yc
        h = modulate(rmsnorm(x, norm1_g[l]), sh1, sc1)
        x = x + g1 * token_mixer_latent(h, k_ctx, v_ctx, cos, sin, w_in[l], w_conv[l], b_conv[l],
                                        w_a[l], w_b[l], sink[l], w_o[l])

        h = modulate(rmsnorm(x, norm2_g[l]), sh2, sc2)
        x = x + g2 * hierarchical_moe(h, w_group[l], b_group[l], w_router[l], b_router[l], w_up[l], w_down[l])
        if not last:
            hc = modulate(rmsnorm(xc, norm2_g[l]), csh2, csc2)
            xc = xc + cg2 * hierarchical_moe(hc, w_group[l], b_group[l], w_router[l], b_router[l], w_up[l], w_down[l])
    return rmsnorm(x, final_g)
```

```python
from contextlib import ExitStack
import numpy as np
import concourse.bass as bass
import concourse.mybir as mybir
from concourse.bass_utils import run_bass_kernel_spmd

F32 = mybir.dt.float32
BF16 = mybir.dt.bfloat16
ALU = mybir.AluOpType
AF = mybir.ActivationFunctionType
AX = mybir.AxisListType

L = 2048
CTX = 256
D = 1024
NE = 32
BIG = 1.0e4
NCOLS = 5248
O_KV, O_V, O_CX0, O_CX1, O_B, O_GA0, O_GA1, O_Q0, O_Q1, O_GB0, O_GB1 = (
    0, 512, 640, 1152, 1664, 2176, 2688, 3200, 3712, 4224, 4736)


class Sched:
    ENGS = ("pe", "act", "dve", "pool", "sp")

    def __init__(self, nc, stack):
        self.nc = nc
        self.stack = stack
        self.ops = []
        self.res = {}
        self.esem = {e: stack.enter_context(nc.semaphore("sem_" + e)) for e in self.ENGS}
        self.dsems = {}

    def _dsem(self, key):
        if key not in self.dsems:
            self.dsems[key] = self.stack.enter_context(self.nc.semaphore("dsem_%d" % len(self.dsems)))
        return self.dsems[key]

    def op(self, eng, fn, reads=(), writes=(), dma=None):
        oid = len(self.ops)
        deps = set()
        for r in reads:
            st = self.res.get(r)
            if st is not None and st[0] is not None:
                deps.add(st[0])
        for w in writes:
            st = self.res.get(w)
            if st is not None:
                if st[0] is not None:
                    deps.add(st[0])
                deps.update(st[1])
        self.ops.append(dict(eng=eng, fn=fn, deps=deps, dma=dma, sig=False))
        for r in reads:
            self.res.setdefault(r, [None, []])[1].append(oid)
        for w in writes:
            self.res[w] = [oid, []]
        return oid

    def alias(self, new_keys, old_keys):
        pend = []
        for k in old_keys:
            st = self.res.pop(k, None)
            if st is not None:
                if st[0] is not None:
                    pend.append(st[0])
                pend.extend(st[1])
        pend = sorted(set(pend))
        for k in new_keys:
            st = self.res.setdefault(k, [None, []])
            st[1].extend(pend)

    def emit(self):
        ops = self.ops
        for o in ops:
            for d in o["deps"]:
                do = ops[d]
                if do["eng"] == "pe" and o["eng"] == "pe" and do["dma"] is None and o["dma"] is None:
                    continue
                do["sig"] = True
        for o in ops:
            if o["dma"] is not None:
                o["sig"] = True
        cnt = {e: 0 for e in self.ENGS}
        dcnt = {}
        for o in ops:
            if not o["sig"]:
                o["tick"] = None
                continue
            if o["dma"] is not None:
                k = ("d", o["dma"])
                dcnt[k] = dcnt.get(k, 0) + 16
                o["tick"] = (k, dcnt[k])
            else:
                cnt[o["eng"]] += 1
                o["tick"] = (("e", o["eng"]), cnt[o["eng"]])
        known = {e: {} for e in self.ENGS}
        prog = {e: [] for e in self.ENGS}
        nwaits = 0
        for o in ops:
            e = o["eng"]
            need = {}
            for d in o["deps"]:
                do = ops[d]
                if do["tick"] is None:
                    continue
                k, v = do["tick"]
                if need.get(k, 0) < v:
                    need[k] = v
            waits = []
            for k, v in need.items():
                if known[e].get(k, 0) >= v:
                    continue
                known[e][k] = v
                waits.append((k, v))
            nwaits += len(waits)
            prog[e].append((waits, o))
        final = [(k, v) for k, v in dcnt.items()]
        for e in self.ENGS:
            if e != "sp" and cnt[e] > 0:
                final.append((("e", e), cnt[e]))
        self.stats = dict(n_ops=len(ops), n_waits=nwaits, cnt=cnt, n_dsem=len(dcnt))

        def semof(k):
            return self.esem[k[1]] if k[0] == "e" else self._dsem(k[1])

        for k in dcnt:
            semof(k)
        nc = self.nc
        handles = dict(pe="tensor", act="scalar", dve="vector", pool="gpsimd", sp="sync")
        with nc.Block() as block:
            for e in self.ENGS:
                lst = prog[e]
                fin = final if e == "sp" else []
                if not lst and not fin:
                    continue

                def body(eng, lst=lst, fin=fin):
                    for waits, o in lst:
                        for k, v in waits:
                            eng.wait_ge(semof(k), v)
                        ins = o["fn"](eng)
                        if o["tick"] is not None:
                            k, v = o["tick"]
                            ins.then_inc(semof(k), 16 if k[0] == "d" else 1)
                    for k, v in fin:
                        eng.wait_ge(semof(k), v)

                getattr(block, handles[e])(body)


def build_program():
    nc = bass.Bass("TRN2", target_bir_lowering=False)

    def DIN(n, s):
        return nc.dram_tensor(n, list(s), F32, kind="ExternalInput").ap()

    x_d = DIN("x", [L, D]); ctx_d = DIN("ctx", [CTX, D]); cc_d = DIN("cc", [128, 16])
    wada_d = DIN("wada", [D, 6 * D]); bada1_d = DIN("bada1", [2048]); bada2_d = DIN("bada2", [128, 32])
    ng1_d = DIN("ng1", [D]); ng2_d = DIN("ng2", [128, 8]); fg_d = DIN("fg", [D])
    win_d = DIN("win", [D, NCOLS]); wc_d = DIN("wc", [128, 12]); bc_d = DIN("bc", [128, 4])
    wa_d = DIN("wa", [512, D]); wb_d = DIN("wb", [512, D]); wo_d = DIN("wo", [D, D])
    sink_d = DIN("sinkp", [1, 8]); wrt_d = DIN("wrt", [D, 36]); brt_d = DIN("brt", [36])
    wup_d = DIN("wup", [NE, D, 512]); wdn_d = DIN("wdn", [NE, 256, D])
    c2_d = DIN("c2", [128, L]); s2_d = DIN("s2", [128, L]); id_d = DIN("ident", [128, 128])
    m01_d = DIN("m01", [128, 256])
    pm_d = DIN("pm", [128, 128])
    out_d = nc.dram_tensor("out", [L, D], F32, kind="ExternalOutput").ap()

    with ExitStack() as st:
        def SB(n, s, dt):
            return st.enter_context(nc.sbuf_tensor("s_" + n, list(s), dt))

        XR = SB("XR", [128, 16384], F32)
        ER = SB("ER", [128, 24576], BF16)
        HT = SB("HT", [128, 8, L + CTX], BF16)
        MT = SB("MT", [128, 8192], BF16)
        OV = SB("OV", [128, 6144], F32)
        identf = SB("identf", [128, 128], F32)
        identb = SB("identb", [128, 128], BF16)
        pmb = SB("pmb", [128, 128], BF16)
        m01 = SB("m01", [128, 256], BF16)
        esb = SB("esb", [128, 8], F32)
        otm = [SB("otm%d" % i, [128, 256], BF16) for i in range(2)]
        dsm = SB("dsm", [128, 8], F32)
        hal = SB("hal", [128, 2], F32)
        wcs = SB("wcs", [128, 12], F32); bcs = SB("bcs", [128, 4], F32)
        ccs = SB("ccs", [128, 16], F32)
        scf = SB("scf", [128, 16], F32)
        scb1 = SB("scb1", [128, 16], BF16)
        mod2T = SB("mod2T", [128, 32], F32)
        b2T = SB("b2T", [128, 32], F32)
        ng2T = SB("ng2T", [128, 8], F32)
        a2T = SB("a2T", [128, 8], F32)
        stat = SB("stat", [128, 128], F32)
        wrt = SB("wrt", [128, 8, 36], BF16)
        brtb = SB("brtb", [128, 36], F32)
        ps = [st.enter_context(nc.psum_tensor("ps%d" % i, [128, 512], F32)) for i in range(8)]

        S = Sched(nc, st)
        _bank = [0]

        _m2lock = [True]

        def nb():
            b = _bank[0]
            if _m2lock[0] and b == 7:
                b = 0
            _bank[0] = (b + 1) % 8
            return b

        psm = ps[7]

        x1 = XR[:, :].rearrange("p (t n) -> p t n", n=D)
        mod1 = XR[:, 0:4096].rearrange("p (a n) -> p a n", n=D)
        xs = [XR[:, 4096 + i * 1024: 4096 + (i + 1) * 1024] for i in range(2)]
        xn_a = XR[:, 6144:7168]
        scb = XR[:, 7168:8192].bitcast(BF16).rearrange("p (k m) -> p k m", m=128)
        kT = XR[:, 8192:10496].bitcast(BF16).rearrange("p (a n) -> p a n", a=2)
        Vx = XR[:, 10496:13376].bitcast(BF16).rearrange("p (t n) -> p t n", n=320)
        C2 = XR[:, 13376:14400].bitcast(BF16)
        S2 = XR[:, 14400:15424].bitcast(BF16)
        wsl = [ER[:, i * 4096:(i + 1) * 4096].rearrange("p (k n) -> p k n", n=512) for i in range(3)]
        ucv = ER[:, 12288:18440].rearrange("p (j n) -> p j n", j=4)
        qT = ER[:, 12288:16384].rearrange("p (j n) -> p j n", j=4)
        cvT = ER[:, 18440:22536].rearrange("p (j n) -> p j n", j=4)
        oT = cvT
        mT = MT[:, :].rearrange("p (k n) -> p k n", k=8)
        ngt = OV[:, 0:1024]
        junk = OV[:, 1024:1536].bitcast(BF16)
        tmpA = OV[:, 1536:2048]; tmpB = OV[:, 2048:2560]; tmpC = OV[:, 2560:3072]
        Pb = OV[:, 3072:4608].bitcast(BF16).rearrange("p (t n) -> p t n", n=512)
        rdt = OV[:, 4608:5120]
        qraw = OV[:, 4608:4864].bitcast(BF16)
        qraws = [qraw, OV[:, 4864:5120].bitcast(BF16)]
        Pbs = [Pb, OV[:, 0:1536].bitcast(BF16).rearrange("p (t n) -> p t n", n=512)]
        _pb = [0]
        _ot = [0]
        G1 = OV[:, 5120:6144]

        ss = stat[:, 0:18]; rstd = stat[:, 18:36]; ss2 = stat[:, 36:52]; rstd2 = stat[:, 52:68]
        ss3 = stat[:, 68:84]; rstd3 = stat[:, 84:100]

        def gdma(out, in_, reads, writes, key):
            S.op("pool", lambda e, out=out, in_=in_: e.dma_start(out=out, in_=in_), reads=reads, writes=writes, dma=key)

        def sdma(out, in_, reads, writes, key):
            S.op("sp", lambda e, out=out, in_=in_: e.dma_start(out=out, in_=in_), reads=reads, writes=writes, dma=key)

        _ws = [0]

        def load_slot(src, nk=8, ncols=512, wide=False):
            i = _ws[0]
            _ws[0] = (i + 1) % 3
            if wide:
                dst = ER[:, i * 4096:(i + 1) * 4096].rearrange("p (k n) -> p k n", n=1024)
            else:
                dst = wsl[i][:, 0:nk, 0:ncols]
            gdma(dst, src, [], [("ws", i)], ("ws", i))
            return i

        def win_src(c0, ncols=512):
            return win_d[:, c0:c0 + ncols].rearrange("(k p) n -> p k n", p=128)

        def mmgroup(lst):
            def fn(e, lst=lst):
                ins = None
                for (o, l, r, s0, s1) in lst:
                    ins = e.matmul(o, lhsT=l, rhs=r, start=s0, stop=s1)
                return ins
            return fn

        sdma(identf[:], id_d, [], ["identf"], "c0")
        sdma(wcs[:], wc_d, [], ["wcs"], "c5")
        sdma(bcs[:], bc_d, [], ["bcs"], "c6")
        sdma(ccs[:], cc_d, [], ["ccs"], "c7")
        sdma(b2T[:], bada2_d, [], ["b2T"], "c8")
        sdma(ng2T[:], ng2_d, [], ["ng2T"], "c9")
        sdma(esb[:], sink_d.rearrange("a b -> (a b)").partition_broadcast(128), [], ["esb"], "c10")
        sdma(brtb[:], brt_d.partition_broadcast(128), [], ["brtb"], "c11")
        sdma(ngt, ng1_d.partition_broadcast(128), [], ["ngt"], "c13")
        sdma(mod1[:, 0:2, :].rearrange("p a n -> p (a n)"), bada1_d.partition_broadcast(128), [], ["mod1b"], "c14")
        sdma(mod1[:, 2:4, :].rearrange("p a n -> p (a n)"), bada1_d.partition_broadcast(128), [], ["mod1c"], "c15")

        Vx5 = Vx.rearrange("p t (b n) -> p t b n", n=64)
        for bi in (0, 2, 4):
            S.op("dve", lambda e, bi=bi: e.memset(Vx5[:, :, bi, :], 1.0), writes=[("Vones", bi)])
        S.op("act", lambda e: e.activation(out=esb[:], in_=esb[:], func=AF.Exp), reads=["esb"], writes=["esb"])
        S.op("act", lambda e: e.activation(out=scf[:], in_=ccs[:], func=AF.Silu), reads=["ccs"], writes=["scf"])
        S.op("dve", lambda e: e.tensor_copy(out=scb, in_=scf[:, :].unsqueeze(2).to_broadcast([128, 16, 128])),
             reads=["scf"], writes=["scb"])
        S.op("dve", lambda e: e.tensor_copy(out=scb1[:], in_=scf[:]), reads=["scf"], writes=["scb1"])

        for n in range(4):
            si = load_slot(wada_d[:, n * 512:(n + 1) * 512].rearrange("(k p) n -> p k n", p=128))
            b0 = nb(); b1 = nb()
            S.op("pe", mmgroup([(ps[b0][:], scb[:, k, :], wsl[si][:, k, :], k == 0, k == 7) for k in range(8)]),
                 reads=["scb", ("ws", si)], writes=[("ps", b0)])
            S.op("pe", mmgroup([(ps[b1][:], scb[:, 8 + k, :], wsl[si][:, k, :], k == 0, k == 7) for k in range(8)]),
                 reads=["scb", ("ws", si)], writes=[("ps", b1)])
            a, off = divmod(n, 2)
            dst0 = mod1[:, a, off * 512:(off + 1) * 512]
            dst1 = mod1[:, 2 + a, off * 512:(off + 1) * 512]
            S.op("dve", lambda e, d=dst0, p=ps[b0]: e.tensor_tensor(out=d, in0=p[:], in1=d, op=ALU.add),
                 reads=[("ps", b0), "mod1b"], writes=[("m1", 0, n)])
            S.op("dve", lambda e, d=dst1, p=ps[b1]: e.tensor_tensor(out=d, in0=p[:], in1=d, op=ALU.add),
                 reads=[("ps", b1), "mod1c"], writes=[("m1", 1, n)])
        gdma(identb[:], id_d, [], ["identb"], "c1")
        gdma(pmb[:], pm_d, [], ["pmb"], "c16")
        gdma(m01[:], m01_d, [], ["m01"], "c2")
        gdma(C2, c2_d, [], ["C2"], "c3")
        gdma(S2, s2_d, [], ["S2"], "c4")
        gdma(wrt[:], wrt_d.rearrange("(k p) n -> p k n", p=128), [], ["wrt"], "c12")
        for c in range(2):
            S.op("dve", lambda e, c=c: e.scalar_tensor_tensor(out=mod1[:, 2 * c + 1, :], in0=mod1[:, 2 * c + 1, :], scalar=1.0,
                                                              in1=ngt, op0=ALU.add, op1=ALU.mult),
                 reads=[("m1", c, 2), ("m1", c, 3), "ngt"], writes=[("A1", c)])

        def norm_stats(src, ssc, rsc, rd_keys, key, finish=True):
            S.op("act", lambda e: e.activation(out=junk, in_=src, func=AF.Square, accum_out=ssc),
                 reads=rd_keys, writes=["junk", ("ss", key)])
            if finish:
                norm_rstd(ssc, rsc, [("ss", key)], ("rs", key))

        def norm_rstd(ssc, rsc, rd, wkey):
            S.op("dve", lambda e: e.tensor_scalar(out=rsc, in0=ssc, scalar1=1.0 / D, scalar2=1e-6, op0=ALU.mult, op1=ALU.add),
                 reads=rd, writes=[wkey])
            S.op("act", lambda e: e.activation(out=rsc, in_=rsc, func=AF.Sqrt), reads=[wkey], writes=[wkey])
            S.op("dve", lambda e: e.reciprocal(out=rsc, in_=rsc), reads=[wkey], writes=[wkey])

        def norm_apply(src, rsc, Abc, Bbc, xn, xnkey, col0, rd_keys, rskey, htkey, defer=False, beng="dve"):
            deferred = []
            S.op("dve", lambda e: e.scalar_tensor_tensor(out=xn, in0=src, scalar=rsc, in1=Abc[0], op0=ALU.mult, op1=ALU.mult),
                 reads=list(rd_keys) + [rskey] + Abc[1], writes=[xnkey])
            S.op(beng, lambda e: e.tensor_tensor(out=xn, in0=xn, in1=Bbc[0], op=ALU.add), reads=[xnkey] + Bbc[1], writes=[xnkey])
            ba = nb(); bb_ = nb()
            for half, bk in ((0, ba), (1, bb_)):
                def fn(e, half=half, bk=bk):
                    ins = None
                    for kk in range(4):
                        k = half * 4 + kk
                        ins = e.transpose(ps[bk][:, kk * 128:(kk + 1) * 128], xn[:, k * 128:(k + 1) * 128], identf[:])
                    return ins
                S.op("pe", fn, reads=[xnkey, "identf"], writes=[("ps", bk)])

                def cp(half=half, bk=bk):
                    S.op("act", lambda e, half=half, bk=bk: e.activation(
                        out=HT[:, half * 4:(half + 1) * 4, col0:col0 + 128],
                        in_=ps[bk][:, :].rearrange("p (k n) -> p k n", n=128), func=AF.Copy),
                        reads=[("ps", bk)], writes=[("HT", htkey, half)])
                if defer:
                    deferred.append(cp)
                else:
                    cp()
            return deferred

        xs4 = [xs[0], xs[1], XR[:, 7168:8192], XR[:, 8192:9216]]
        xn_s2 = XR[:, 9216:10240]
        S.alias([("xs", 2)], ["scb"])

        def s0_dma(t):
            sl = t % 4
            src_d = x_d[t * 128:(t + 1) * 128, :] if t < 16 else ctx_d[(t - 16) * 128:(t - 15) * 128, :]
            sdma(xs4[sl], src_d, [], [("xs", sl)], ("xs", sl))

        def s0_sq(t):
            sl = t % 4
            S.op("act", lambda e: e.activation(out=junk, in_=xs4[sl], func=AF.Square, accum_out=ss[:, t:t + 1]),
                 reads=[("xs", sl)], writes=["junk", ("ss", t)])

        def s0_ts(t):
            S.op("dve", lambda e: e.tensor_scalar(out=rstd[:, t:t + 1], in0=ss[:, t:t + 1], scalar1=1.0 / D, scalar2=1e-6, op0=ALU.mult, op1=ALU.add),
                 reads=[("ss", t)], writes=[("rs", t)])

        def s0_sr(t):
            S.op("act", lambda e: e.activation(out=rstd[:, t:t + 1], in_=rstd[:, t:t + 1], func=AF.Sqrt), reads=[("rs", t)], writes=[("rs", t)])

        def s0_rc(t):
            S.op("dve", lambda e: e.reciprocal(out=rstd[:, t:t + 1], in_=rstd[:, t:t + 1]), reads=[("rs", t)], writes=[("rs", t)])

        def s0_apply(t):
            sl = t % 4
            c = 0 if t < 16 else 1
            xb_, xk_ = (xn_a, "xn") if t % 2 == 0 else (xn_s2, "xn2")
            return norm_apply(xs4[sl], rstd[:, t:t + 1],
                              (mod1[:, 2 * c + 1, :], [("A1", c)]),
                              (mod1[:, 2 * c, :], [("m1", c, 0), ("m1", c, 1)]),
                              xb_, xk_, t * 128, [("xs", sl)], ("rs", t), t, defer=True,
                              beng=("pool" if t % 3 == 2 else "dve"))

        NT0 = 18
        for t in range(3):
            s0_dma(t)
        for t in range(3):
            s0_sq(t)
        s0_ts(0); s0_sr(0); s0_rc(0)
        s0_ts(1); s0_sr(1)
        pend_cp = []
        for t in range(NT0):
            if t + 3 < NT0:
                s0_dma(t + 3)
                s0_sq(t + 3)
            if t + 2 < NT0:
                s0_ts(t + 2)
                s0_sr(t + 2)
            if t + 1 < NT0:
                s0_rc(t + 1)
            prev_cp = pend_cp
            pend_cp = s0_apply(t)
            for cp_ in prev_cp:
                cp_()
        for cp_ in pend_cp:
            cp_()
        S.alias([("kT", kvh, tc) for kvh in range(2) for tc in range(5)], [("xs", 3), "xn2"])

        def HTr(tlist):
            return [("HT", t, h) for t in tlist for h in (0, 1)]

        si_kv = load_slot(win_src(O_KV))
        k_units = [(tc, kvh) for tc in range(4) for kvh in range(2)]

        def k_proj(ui):
            tc, kvh = k_units[ui]
            t0 = tc * 512
            tl = list(range(tc * 4, tc * 4 + 4))
            bA = nb()
            qb = qraws[ui % 2]
            S.op("pe", mmgroup([(ps[bA][:], wsl[si_kv][:, k, (2 * kvh) * 128:(2 * kvh + 1) * 128], HT[:, k, t0:t0 + 512], k == 0, k == 7)
                                for k in range(8)]), reads=[("ws", si_kv)] + HTr(tl), writes=[("ps", bA)])
            S.op("act", lambda e, bA=bA, qb=qb: e.activation(out=qb, in_=ps[bA][:], func=AF.Copy), reads=[("ps", bA)], writes=[("qraw", ui % 2), ("psr", bA)])
            return bA

        def k_rope(ui, bA):
            tc, kvh = k_units[ui]
            t0 = tc * 512
            qb = qraws[ui % 2]
            bB = nb()
            S.op("pe", lambda e, bB=bB, qb=qb: e.matmul(ps[bB][:], lhsT=pmb[:], rhs=qb, start=True, stop=True),
                 reads=["pmb", ("qraw", ui % 2)], writes=[("ps", bB)])
            S.op("dve", lambda e, bA=bA, t0=t0: e.tensor_tensor(out=tmpA, in0=ps[bA][:], in1=C2[:, t0:t0 + 512], op=ALU.mult),
                 reads=[("ps", bA), ("psr", bA), "C2"], writes=["tmpA"])
            S.op("dve", lambda e, bB=bB, t0=t0: e.tensor_tensor(out=tmpB, in0=ps[bB][:], in1=S2[:, t0:t0 + 512], op=ALU.mult),
                 reads=[("ps", bB), "S2"], writes=["tmpB"])
            S.op("dve", lambda e, kvh=kvh, t0=t0, tc=tc: e.tensor_tensor(out=kT[:, kvh, t0:t0 + 512], in0=tmpA, in1=tmpB, op=ALU.add),
                 reads=["tmpA", "tmpB"], writes=[("kT", kvh, tc)])

        kbanks = {0: k_proj(0)}
        for ui in range(len(k_units)):
            if ui + 1 < len(k_units):
                kbanks[ui + 1] = k_proj(ui + 1)
            k_rope(ui, kbanks[ui])
        for kvh in range(2):
            bA = nb()
            S.op("pe", mmgroup([(ps[bA][:, 0:256], wsl[si_kv][:, k, (2 * kvh) * 128:(2 * kvh + 1) * 128], HT[:, k, 2048:2304], k == 0, k == 7)
                                for k in range(8)]), reads=[("ws", si_kv)] + HTr([16, 17]), writes=[("ps", bA)])
            S.op("act", lambda e, bA=bA, kvh=kvh: e.activation(out=kT[:, kvh, 2048:2304], in_=ps[bA][:, 0:256], func=AF.Copy),
                 reads=[("ps", bA)], writes=[("kT", kvh, 4)])
        si = load_slot(win_src(O_V, 128), 8, 128)
        for g in range(5):
            tl = list(range(g * 4, min(g * 4 + 4, 18)))
            bk = nb()
            lst = []
            for i, t in enumerate(tl):
                for k in range(8):
                    lst.append((ps[bk][:, i * 128:(i + 1) * 128], HT[:, k, t * 128:(t + 1) * 128], wsl[si][:, k, 0:128], k == 0, k == 7))
            S.op("pe", mmgroup(lst), reads=[("ws", si)] + HTr(tl), writes=[("ps", bk)])
            nt = len(tl)
            for kv in range(2):
                S.op("act", lambda e, bk=bk, g=g, nt=nt, kv=kv: e.activation(
                    out=Vx5[:, g * 4:g * 4 + nt, 1 + 2 * kv, :],
                    in_=ps[bk][:, 0:nt * 128].rearrange("p (t n) -> p t n", n=128)[:, :, kv * 64:(kv + 1) * 64], func=AF.Copy),
                    reads=[("ps", bk)], writes=[("V", g, kv)])

        _m2 = {}

        wsx = [XR[:, i * 2048:(i + 1) * 2048].bitcast(BF16).rearrange("p (k n) -> p k n", n=512) for i in range(2)]

        def mod2_load(n):
            i = n % 2
            gdma(wsx[i], wada_d[:, 2048 + n * 512:2048 + (n + 1) * 512].rearrange("(k p) n -> p k n", p=128), [], [("wsx", i)], ("wsx", i))

        def mod2_chunk(n):
            i = n % 2
            lst = []
            for j in range(4):
                col = n * 4 + j
                for k in range(8):
                    lst.append((psm[:, col:col + 1], wsx[i][:, k, j * 128:(j + 1) * 128], scb1[:, k:k + 1], k == 0, k == 7))
            S.op("pe", mmgroup(lst), reads=[("wsx", i), "scb1"], writes=[("ps", 7)])

        def mod2_finish():
            S.op("dve", lambda e: e.tensor_tensor(out=mod2T[:], in0=psm[:, 0:32], in1=b2T[:], op=ALU.add),
                 reads=[("ps", 7), "b2T"], writes=["mod2T"])
            S.op("dve", lambda e: e.scalar_tensor_tensor(out=a2T[:], in0=mod2T[:, 16:24], scalar=1.0, in1=ng2T[:], op0=ALU.add, op1=ALU.mult),
                 reads=["mod2T", "ng2T"], writes=["a2T"])

        def expand(vecT, dst, rd, wr, bf=False):
            for h in range(2):
                bk = nb()
                S.op("pe", mmgroup([(ps[bk][:, j * 128:(j + 1) * 128], vecT[:, h * 4 + j:h * 4 + j + 1].to_broadcast([128, 128]), identf[:], True, True)
                                    for j in range(4)]), reads=rd + ["identf"], writes=[("ps", bk)])
                S.op("act", lambda e, bk=bk, h=h: e.activation(out=dst[:, h * 512:(h + 1) * 512], in_=ps[bk][:], func=AF.Copy),
                     reads=[("ps", bk)], writes=[(wr, h)])

        mixer_keys_ucv = []
        for half in range(2):
            chunks = [2 * half, 2 * half + 1]
            cx_chunks = chunks
            cx0 = chunks[0]
            halo_tok = 1024 if half == 0 else 1023
            halo_col = 1025 if half == 0 else 0
            pad_col = 0 if half == 0 else 1025
            if half == 1:
                S.alias([("ucv", j, c) for j in range(4) for c in range(4)] + ["ucvpad"] + [("ucvh", j) for j in range(4)],
                        [("qT", j, c) for j in range(4) for c in range(4)])
            S.op("dve", lambda e, pc=pad_col: e.memset(ucv[:, :, pc:pc + 1], 0.0), writes=["ucvpad"])
            for sidx, off in enumerate((O_CX0, O_CX1)):
                si = load_slot(win_src(off))
                for jj in range(2):
                    j = sidx * 2 + jj
                    bH = nb()
                    lst = []
                    for q_ in range(2):
                        for k in range(8):
                            lst.append((ps[bH][:, q_:q_ + 1], wsl[si][:, k, (2 * jj + q_) * 128:(2 * jj + q_ + 1) * 128],
                                        HT[:, k, halo_tok:halo_tok + 1], k == 0, k == 7))
                    S.op("pe", mmgroup(lst), reads=[("ws", si)] + HTr([halo_tok // 128]), writes=[("ps", bH)])
                    S.op("act", lambda e, bH=bH: e.activation(out=hal[:, 0:1], in_=ps[bH][:, 0:1], func=AF.Copy), reads=[("ps", bH)], writes=["hal"])
                    S.op("dve", lambda e, bH=bH, j=j, hc=halo_col: e.tensor_tensor(out=ucv[:, j, hc:hc + 1], in0=ps[bH][:, 1:2], in1=hal[:, 0:1], op=ALU.mult),
                         reads=[("ps", bH), "hal"], writes=[("ucvh", j)])
                for c in cx_chunks:
                    tl = list(range(c * 4, c * 4 + 4))
                    for jj in range(2):
                        j = sidx * 2 + jj
                        bA = nb(); bB = nb()
                        S.op("pe", mmgroup([(ps[bA][:], wsl[si][:, k, (2 * jj) * 128:(2 * jj + 1) * 128], HT[:, k, c * 512:(c + 1) * 512], k == 0, k == 7)
                                            for k in range(8)]), reads=[("ws", si)] + HTr(tl), writes=[("ps", bA)])
                        S.op("pe", mmgroup([(ps[bB][:], wsl[si][:, k, (2 * jj + 1) * 128:(2 * jj + 2) * 128], HT[:, k, c * 512:(c + 1) * 512], k == 0, k == 7)
                                            for k in range(8)]), reads=[("ws", si)] + HTr(tl), writes=[("ps", bB)])
                        S.op("act", lambda e, bA=bA: e.activation(out=tmpA, in_=ps[bA][:], func=AF.Copy), reads=[("ps", bA)], writes=["tmpA"])
                        o0 = 1 + (c - cx0) * 512
                        S.op("dve", lambda e, bB=bB, j=j, o0=o0: e.tensor_tensor(out=ucv[:, j, o0:o0 + 512], in0=ps[bB][:], in1=tmpA, op=ALU.mult),
                             reads=[("ps", bB), "tmpA"], writes=[("ucv", j, c)])
            if half == 1:
                S.alias([("cvT", j, c) for j in range(4) for c in range(2)], [("oT", j, c) for j in range(4) for c in range(2)])
            si = load_slot(win_src(O_B))
            for ci, c in enumerate(chunks):
                tl = list(range(c * 4, c * 4 + 4))
                nbrs = [cc_ for cc_ in (c - 1, c, c + 1) if 0 <= cc_ < 4]
                for j in range(4):
                    o0 = (c - cx0) * 512
                    rdk = [("ucv", j, cc_) for cc_ in nbrs] + ["ucvpad", "wcs", "bcs", ("ucvh", j)]
                    S.op("dve", lambda e, j=j, o0=o0: e.tensor_scalar(out=tmpB, in0=ucv[:, j, o0:o0 + 512], scalar1=wcs[:, j * 3:j * 3 + 1],
                                                                      scalar2=bcs[:, j:j + 1], op0=ALU.mult, op1=ALU.add),
                         reads=rdk, writes=["tmpB"])
                    S.op("dve", lambda e, j=j, o0=o0: e.scalar_tensor_tensor(out=tmpB, in0=ucv[:, j, o0 + 1:o0 + 513], scalar=wcs[:, j * 3 + 1:j * 3 + 2],
                                                                             in1=tmpB, op0=ALU.mult, op1=ALU.add),
                         reads=rdk + ["tmpB"], writes=["tmpB"])
                    S.op("dve", lambda e, j=j, o0=o0: e.scalar_tensor_tensor(out=tmpB, in0=ucv[:, j, o0 + 2:o0 + 514], scalar=wcs[:, j * 3 + 2:j * 3 + 3],
                                                                             in1=tmpB, op0=ALU.mult, op1=ALU.add),
                         reads=rdk + ["tmpB"], writes=["tmpB"])
                    bk = nb()
                    S.op("pe", mmgroup([(ps[bk][:], wsl[si][:, k, j * 128:(j + 1) * 128], HT[:, k, c * 512:(c + 1) * 512], k == 0, k == 7)
                                        for k in range(8)]), reads=[("ws", si)] + HTr(tl), writes=[("ps", bk)])
                    S.op("dve", lambda e, bk=bk, j=j, ci=ci: e.tensor_tensor(out=cvT[:, j, ci * 512:(ci + 1) * 512], in0=ps[bk][:], in1=tmpB, op=ALU.mult),
                         reads=[("ps", bk), "tmpB"], writes=[("cvT", j, ci)])
            sg0_ = load_slot(win_src(O_GA0))
            sa_ = load_slot(wa_d.rearrange("(k p) n -> p k n", p=128), wide=True)
            wa_v = ER[:, sa_ * 4096:(sa_ + 1) * 4096].rearrange("p (k n) -> p k n", n=1024)
            sg = [sg0_, load_slot(win_src(O_GA1))]
            for ci, c in enumerate(chunks):
                tl = list(range(c * 4, c * 4 + 4))
                for oc in range(8):
                    bY = nb(); bG = nb()
                    S.op("pe", mmgroup([(ps[bY][:], wa_v[:, k, oc * 128:(oc + 1) * 128], cvT[:, k, ci * 512:(ci + 1) * 512], k == 0, k == 3)
                                        for k in range(4)]), reads=[("ws", sa_)] + [("cvT", k, ci) for k in range(4)], writes=[("ps", bY)])
                    sgi = sg[oc // 4]
                    S.op("pe", mmgroup([(ps[bG][:], wsl[sgi][:, k, (oc % 4) * 128:(oc % 4 + 1) * 128], HT[:, k, c * 512:(c + 1) * 512], k == 0, k == 7)
                                        for k in range(8)]), reads=[("ws", sgi)] + HTr(tl), writes=[("ps", bG)])
                    S.op("act", lambda e, bG=bG: e.activation(out=tmpA, in_=ps[bG][:], func=AF.Sigmoid), reads=[("ps", bG)], writes=["tmpA"])
                    S.op("dve", lambda e, bY=bY, oc=oc, ci=ci: e.tensor_tensor(out=mT[:, oc, ci * 512:(ci + 1) * 512], in0=ps[bY][:], in1=tmpA, op=ALU.mult),
                         reads=[("ps", bY), "tmpA"], writes=[("mT", oc, ci)])
            S.alias([("qT", j, c) for j in range(4) for c in range(4)],
                    [("ucv", j, c) for j in range(4) for c in range(4)] + ["ucvpad"] + [("ucvh", j) for j in range(4)])
            if half == 0:
                S.alias([("qraw", 0)], ["qraw"])
            q_slots = [load_slot(win_src(O_Q0)), load_slot(win_src(O_Q1))]
            q_units = [(sidx, ci, c, jj) for sidx in range(2) for ci, c in enumerate(chunks) for jj in range(2)]

            def q_proj(ui):
                sidx, ci, c, jj = q_units[ui]
                si = q_slots[sidx]
                tl = list(range(c * 4, c * 4 + 4))
                bA = nb()
                qb = qraws[ui % 2]
                S.op("pe", mmgroup([(ps[bA][:], wsl[si][:, k, (2 * jj) * 128:(2 * jj + 1) * 128], HT[:, k, c * 512:(c + 1) * 512], k == 0, k == 7)
                                    for k in range(8)]), reads=[("ws", si)] + HTr(tl), writes=[("ps", bA)])
                S.op("act", lambda e, bA=bA, qb=qb: e.activation(out=qb, in_=ps[bA][:], func=AF.Copy), reads=[("ps", bA)], writes=[("qraw", ui % 2), ("psr", bA)])
                return bA

            def q_rope(ui, bA):
                sidx, ci, c, jj = q_units[ui]
                j = sidx * 2 + jj
                qb = qraws[ui % 2]
                bB = nb()
                S.op("pe", lambda e, bB=bB, qb=qb: e.matmul(ps[bB][:], lhsT=pmb[:], rhs=qb, start=True, stop=True),
                     reads=["pmb", ("qraw", ui % 2)], writes=[("ps", bB)])
                S.op("dve", lambda e, bA=bA, c=c: e.tensor_tensor(out=tmpA, in0=ps[bA][:], in1=C2[:, c * 512:(c + 1) * 512], op=ALU.mult),
                     reads=[("ps", bA), ("psr", bA), "C2"], writes=["tmpA"])
                S.op("dve", lambda e, bB=bB, c=c: e.tensor_tensor(out=tmpB, in0=ps[bB][:], in1=S2[:, c * 512:(c + 1) * 512], op=ALU.mult),
                     reads=[("ps", bB), "S2"], writes=["tmpB"])
                S.op("dve", lambda e, j=j, ci=ci: e.tensor_tensor(out=qT[:, j, ci * 512:(ci + 1) * 512], in0=tmpA, in1=tmpB, op=ALU.add),
                     reads=["tmpA", "tmpB"], writes=[("qT", j, ci)])

            qbanks = {0: q_proj(0)}
            for ui in range(len(q_units)):
                if ui + 1 < len(q_units):
                    qbanks[ui + 1] = q_proj(ui + 1)
                q_rope(ui, qbanks[ui])
            S.alias([("P", 1, i, hh) for i in range(6) for hh in range(2)], ["junk", "ngt"])
            S.alias([("oT", j, c) for j in range(4) for c in range(2)], [("cvT", j, c) for j in range(4) for c in range(2)])
            def att_unit(nbk, kvh):
                n = half * 8 + nbk
                kts = []
                if n > 0:
                    kts.append(((n - 1) * 128, n - 1, 0))
                kts.append((n * 128, n, None))
                if n < 15:
                    kts.append(((n + 1) * 128, n + 1, 1))
                kts.append((2048, 16, None)); kts.append((2176, 17, None))
                pbi = _pb[0]; _pb[0] ^= 1
                return dict(nbk=nbk, kvh=kvh, n=n, ci=nbk // 4, qc0=nbk * 128, kts=kts, pbi=pbi)

            _pS = [0]; _pO = [0]; _pT = [0]
            poolT = [6] if half == 0 else [6, 7]

            def nbS():
                b_ = (0, 1, 2, 3)[_pS[0] % 4]; _pS[0] += 1
                return b_

            def nbO():
                b_ = (4, 5)[_pO[0] % 2]; _pO[0] += 1
                return b_

            def nbT():
                b_ = poolT[_pT[0] % len(poolT)]; _pT[0] += 1
                return b_

            def att_scores(u):
                kts = u["kts"]; kvh = u["kvh"]; pbi = u["pbi"]; qc0 = u["qc0"]; ci = u["ci"]
                Pb = Pbs[pbi]
                nk = len(kts)
                for p0 in range(0, nk, 2):
                    pr = kts[p0:p0 + 2]
                    w = len(pr) * 256
                    for hh in range(2):
                        bk = nbS()
                        lst = []
                        for i, (kc, vt, mk) in enumerate(pr):
                            lst.append((ps[bk][:, i * 256:(i + 1) * 256], kT[hh * 64:(hh + 1) * 64, kvh, kc:kc + 128],
                                        qT[hh * 64:(hh + 1) * 64, 2 * kvh:2 * kvh + 2, qc0:qc0 + 128], True, True))
                        kc_tcs = sorted(set(kc // 512 for (kc, _, _) in pr))
                        S.op("pe", mmgroup(lst), reads=[("kT", kvh, t_) for t_ in kc_tcs] + [("qT", 2 * kvh, ci), ("qT", 2 * kvh + 1, ci)],
                             writes=[("ps", bk)])
                        S.op("act", lambda e, bk=bk, p0=p0, hh=hh, w=w, npr=len(pr), Pb=Pb: e.activation(
                            out=Pb[:, p0:p0 + npr, hh * 256:(hh + 1) * 256],
                            in_=ps[bk][:, 0:w].rearrange("p (t n) -> p t n", n=256), func=AF.Exp, scale=0.125),
                            reads=[("ps", bk)], writes=[("P", pbi, p0 + i, hh) for i in range(len(pr))])
                for i, (kc, vt, mk) in enumerate(kts):
                    if mk is not None:
                        S.op("pool", lambda e, i=i, mk=mk, Pb=Pb: e.tensor_tensor(
                            out=Pb[:, i, :].rearrange("p (b n) -> p b n", n=128), in0=Pb[:, i, :].rearrange("p (b n) -> p b n", n=128),
                            in1=m01[:, mk * 128:(mk + 1) * 128].unsqueeze(1).to_broadcast([128, 4, 128]), op=ALU.mult),
                            reads=[("P", pbi, i, 0), ("P", pbi, i, 1), "m01"], writes=[("P", pbi, i, 0), ("P", pbi, i, 1)])

            def att_pv(u):
                kts = u["kts"]; kvh = u["kvh"]; pbi = u["pbi"]; qc0 = u["qc0"]; ci = u["ci"]; nbk = u["nbk"]
                Pb = Pbs[pbi]
                nk = len(kts)
                bO = nbO()
                lst = []
                for cb in range(4):
                    for i, (kc, vt, mk) in enumerate(kts):
                        lst.append((ps[bO][:, cb * 65:(cb + 1) * 65], Pb[:, i, cb * 128:(cb + 1) * 128],
                                    Vx[:, vt, (1 + 2 * kvh) * 64:(1 + 2 * kvh) * 64 + 65], i == 0, i == nk - 1))
                vrd = [("V", vt // 4, kvh) for (_, vt, _) in kts] + [("Vones", 0), ("Vones", 2), ("Vones", 4)]
                prd = [("P", pbi, i, hh) for i in range(nk) for hh in range(2)]
                S.op("pe", mmgroup(lst), reads=vrd + prd, writes=[("ps", bO)])
                O4 = ps[bO][:, 0:260].rearrange("p (c n) -> p c n", n=65)
                oi = _ot[0]; _ot[0] ^= 1
                ot = otm[oi]
                S.op("dve", lambda e, O4=O4, kvh=kvh: e.tensor_tensor(out=dsm[:, 0:4], in0=O4[:, :, 64], in1=esb[:, kvh * 4:(kvh + 1) * 4], op=ALU.add),
                     reads=[("ps", bO), "esb"], writes=["dsm"])
                S.op("dve", lambda e: e.reciprocal(out=dsm[:, 4:8], in_=dsm[:, 0:4]), reads=["dsm"], writes=["dsm"])
                for hh in range(2):
                    S.op("dve", lambda e, O4=O4, hh=hh, ot=ot: e.tensor_tensor(
                        out=ot[:, :].rearrange("p (jj hh d) -> p jj hh d", jj=2, hh=2)[:, :, hh, :],
                        in0=O4[:, hh * 2:hh * 2 + 2, 0:64],
                        in1=dsm[:, 4 + hh * 2:4 + hh * 2 + 2].unsqueeze(2).to_broadcast([128, 2, 64]), op=ALU.mult),
                        reads=[("ps", bO), "dsm"], writes=[("otm", oi, hh)])
                u["oi"] = oi

            def att_tr(u):
                kvh = u["kvh"]; qc0 = u["qc0"]; ci = u["ci"]; nbk = u["nbk"]; oi = u["oi"]
                ot = otm[oi]
                bT = nbT()
                psb = ps[bT][:, :].bitcast(BF16)
                S.op("pe", lambda e, ot=ot, psb=psb: [e.transpose(psb[:, jj * 128:(jj + 1) * 128], ot[:, jj * 128:(jj + 1) * 128], identb[:]) for jj in range(2)][-1],
                     reads=[("otm", oi, 0), ("otm", oi, 1), "identb"], writes=[("ps", bT)])
                S.op("act", lambda e, psb=psb, kvh=kvh, qc0=qc0: e.activation(
                    out=oT[:, 2 * kvh:2 * kvh + 2, qc0:qc0 + 128], in_=psb[:, 0:256].rearrange("p (j n) -> p j n", n=128), func=AF.Copy),
                    reads=[("ps", bT)], writes=[("oTp", 2 * kvh, ci, nbk, 0), ("oT", 2 * kvh, ci), ("oT", 2 * kvh + 1, ci)])

            units = [att_unit(nbk, kvh) for nbk in range(8) for kvh in range(2)]
            if half == 0:
                S.alias([("wsx", 0), ("wsx", 1)], [("m1", c_, n_) for c_ in range(2) for n_ in range(4)] + [("A1", 0), ("A1", 1), "mod1b", "mod1c"])
                mod2_load(0); mod2_load(1)
            sg_D = [load_slot(win_src(O_GB0)), load_slot(win_src(O_GB1))]
            sb_ = load_slot(wb_d.rearrange("(k p) n -> p k n", p=128), wide=True)
            NU = len(units)
            for ui in range(NU + 2):
                if ui < NU:
                    att_scores(units[ui])
                if 0 <= ui - 1 < NU:
                    att_pv(units[ui - 1])
                if 0 <= ui - 2 < NU:
                    att_tr(units[ui - 2])
                if half == 0 and ui >= 2 and ui % 2 == 0 and ui // 2 - 1 < 7:
                    n_ = ui // 2 - 1
                    mod2_chunk(n_)
                    if n_ + 2 < 8:
                        mod2_load(n_ + 2)
            if half == 0:
                mod2_chunk(7)
                mod2_finish()
                _m2lock[0] = False
                expand(mod2T[:, 0:8], G1, ["mod2T"], "G1")
            S.alias(["junk", "ngt"], [("P", 1, i, hh) for i in range(6) for hh in range(2)])
            wb_v = ER[:, sb_ * 4096:(sb_ + 1) * 4096].rearrange("p (k n) -> p k n", n=1024)
            sg = sg_D
            for ci, c in enumerate(chunks):
                tl = list(range(c * 4, c * 4 + 4))
                ord_ = [("oTp", 2 * kvh, ci, nbk, 0) for kvh in range(2) for nbk in range(ci * 4, ci * 4 + 4)]
                for oc in range(8):
                    bY = nb(); bG = nb()
                    S.op("pe", mmgroup([(ps[bY][:], wb_v[:, k, oc * 128:(oc + 1) * 128], oT[:, k, ci * 512:(ci + 1) * 512], k == 0, k == 3)
                                        for k in range(4)]), reads=[("ws", sb_)] + ord_ + [("oT", k, ci) for k in range(4)], writes=[("ps", bY)])
                    sgi = sg[oc // 4]
                    S.op("pe", mmgroup([(ps[bG][:], wsl[sgi][:, k, (oc % 4) * 128:(oc % 4 + 1) * 128], HT[:, k, c * 512:(c + 1) * 512], k == 0, k == 7)
                                        for k in range(8)]), reads=[("ws", sgi)] + HTr(tl), writes=[("ps", bG)])
                    S.op("act", lambda e, bG=bG: e.activation(out=tmpA, in_=ps[bG][:], func=AF.Sigmoid), reads=[("ps", bG)], writes=["tmpA"])
                    S.op("dve", lambda e, bY=bY: e.tensor_tensor(out=tmpC.bitcast(BF16)[:, 0:512], in0=ps[bY][:], in1=tmpA, op=ALU.mult),
                         reads=[("ps", bY), "tmpA"], writes=["tmpC"])
                    S.op("dve", lambda e, oc=oc, ci=ci: e.tensor_tensor(out=mT[:, oc, ci * 512:(ci + 1) * 512], in0=mT[:, oc, ci * 512:(ci + 1) * 512],
                                                                        in1=tmpC.bitcast(BF16)[:, 0:512], op=ALU.add),
                         reads=[("mT", oc, ci), "tmpC"], writes=[("mT", oc, ci)])
            so = [load_slot(wo_d[:, h * 512:(h + 1) * 512].rearrange("(k p) n -> p k n", p=128)) for h in range(2)]
            if half == 0:
                S.alias([("x1", t) for t in range(8)], [("m1", c, n) for c in range(2) for n in range(4)] + [("A1", 0), ("A1", 1), ("xs", 0), ("xs", 1), ("xs", 2), "xn", "scb", "mod1b", "mod1c", ("wsx", 0), ("wsx", 1)])
            else:
                S.alias([("x1", t) for t in range(8, 16)], [("kT", kvh, tc) for kvh in range(2) for tc in range(5)] + [("V", g, kv) for g in range(5) for kv in range(2)]
                        + [("Vones", 0), ("Vones", 2), ("Vones", 4), "C2", "S2"])
            for tt in range(8):
                t = half * 8 + tt
                sdma(x1[:, t, :], x_d[t * 128:(t + 1) * 128, :], [], [("x1", t)], ("x1", t))
            for h in range(2):
                for tt in range(8):
                    t = half * 8 + tt
                    ci = tt // 4
                    bk = nb()
                    S.op("pe", mmgroup([(ps[bk][:], mT[:, k, tt * 128:(tt + 1) * 128], wsl[so[h]][:, k, :], k == 0, k == 7) for k in range(8)]),
                         reads=[("ws", so[h])] + [("mT", k, ci) for k in range(8)], writes=[("ps", bk)])
                    S.op("dve", lambda e, bk=bk, h=h: e.tensor_tensor(out=tmpA, in0=ps[bk][:], in1=G1[:, h * 512:(h + 1) * 512], op=ALU.mult),
                         reads=[("ps", bk), ("G1", h)], writes=["tmpA"])
                    S.op("dve", lambda e, t=t, h=h: e.tensor_tensor(out=x1[:, t, h * 512:(h + 1) * 512], in0=x1[:, t, h * 512:(h + 1) * 512], in1=tmpA, op=ALU.add),
                         reads=[("x1", t), "tmpA"], writes=[("x1", t)])
                    if h == 1:
                        norm_stats(x1[:, t, :], ss2[:, t:t + 1], None, [("x1", t)], ("n2", t), finish=False)

        A2b = OV[:, 1536:2560]; B2b = OV[:, 2560:3584]; xn_b = OV[:, 3584:4608]
        S.alias([("A2b", 0), ("A2b", 1), ("B2b", 0), ("B2b", 1), "xn", "G2b"],
                ["tmpA", "tmpB", "tmpC", "rd0", "rd1", "qraw", ("qraw", 0), ("qraw", 1)] + [("P", 0, i, hh) for i in range(6) for hh in range(2)])
        expand(a2T[:, :], A2b, ["a2T"], "A2b")
        expand(mod2T[:, 8:16], B2b, ["mod2T"], "B2b")
        lg = MT[:, 0:1152].bitcast(F32).rearrange("p (t n) -> p t n", n=36)
        RKEYS = [("lg", t) for t in range(16)] + ["rt", "gmax", "gsh", "gm", "gsum", "m1v", "k1", "ml2", "m2v", "k2", "dd", "s1", "s2"] + [("ml", g) for g in range(4)]
        S.alias(RKEYS, [("mT", oc, ci) for oc in range(8) for ci in range(2)])
        xn_c = OV[:, 0:1024]
        S.alias(["xnc"], ["ngt"])
        norm_rstd(ss2, rstd2, [("ss", ("n2", t)) for t in range(16)], "rs2all")

        def n2_router(t):
            bk = nb()
            S.op("pe", mmgroup([(ps[bk][:, 0:36], HT[:, k, t * 128:(t + 1) * 128], wrt[:, k, :], k == 0, k == 7) for k in range(8)]),
                 reads=HTr([t]) + ["wrt"], writes=[("ps", bk)])
            return bk

        def n2_lgadd(t, bk):
            S.op("dve", lambda e, bk=bk, t=t: e.tensor_tensor(out=lg[:, t, :], in0=ps[bk][:, 0:36], in1=brtb[:], op=ALU.add),
                 reads=[("ps", bk), "brtb"], writes=[("lg", t)])

        prev = None
        for t in range(16):
            xb_, xk_ = (xn_b, "xn") if t % 2 == 0 else (xn_c, "xnc")
            norm_apply(x1[:, t, :], rstd2[:, t:t + 1], (A2b, [("A2b", 0), ("A2b", 1)]), (B2b, [("B2b", 0), ("B2b", 1)]),
                       xb_, xk_, t * 128, [("x1", t)], "rs2all", t, beng=("pool" if t % 3 == 2 else "dve"))
            bk = n2_router(t)
            if prev is not None:
                n2_lgadd(*prev)
            prev = (t, bk)
        n2_lgadd(*prev)
        RT = MT[:, 1152:8192].bitcast(F32)
        r_gsh = RT[:, 0:64].rearrange("p (t n) -> p t n", n=4)
        r_gm = RT[:, 64:128].rearrange("p (t n) -> p t n", n=4)
        r_ml = RT[:, 128:640].rearrange("p (t n) -> p t n", n=32)
        r_k1 = RT[:, 640:1152].rearrange("p (t n) -> p t n", n=32)
        r_ml2 = RT[:, 1152:1664].rearrange("p (t n) -> p t n", n=32)
        r_k2 = RT[:, 1664:2176].rearrange("p (t n) -> p t n", n=32)
        r_cmb = RT[:, 2176:2688].rearrange("p (t n) -> p t n", n=32)
        sm = RT[:, 2688:2944].rearrange("p (a t) -> p a t", t=16)
        combb = SB("combb", [128, 16, 32], BF16)
        lgk = [("lg", t) for t in range(16)]

        def V_(eng, fn, rd, wr):
            S.op(eng, fn, reads=rd, writes=wr)

        lgg = lg[:, :, 0:4]; lge = lg[:, :, 4:36]
        V_("dve", lambda e: e.tensor_reduce(out=sm[:, 0, :], in_=lgg, axis=AX.X, op=ALU.max), lgk, ["gmax"])
        V_("dve", lambda e: e.tensor_tensor(out=r_gsh, in0=lgg, in1=sm[:, 0, :].unsqueeze(2).to_broadcast([128, 16, 4]), op=ALU.subtract), lgk + ["gmax"], ["gsh"])
        V_("dve", lambda e: e.tensor_tensor(out=r_gm, in0=lgg, in1=sm[:, 0, :].unsqueeze(2).to_broadcast([128, 16, 4]), op=ALU.is_equal), lgk + ["gmax"], ["gm"])
        V_("act", lambda e: e.activation(out=r_gsh, in_=r_gsh, func=AF.Exp), ["gsh"], ["gsh"])
        V_("dve", lambda e: e.tensor_reduce(out=sm[:, 1, :], in_=r_gsh, axis=AX.X, op=ALU.add), ["gsh"], ["gsum"])
        V_("dve", lambda e: e.reciprocal(out=sm[:, 1, :], in_=sm[:, 1, :]), ["gsum"], ["gsum"])
        V_("dve", lambda e: e.tensor_scalar(out=r_gm, in0=r_gm, scalar1=BIG, scalar2=-BIG, op0=ALU.mult, op1=ALU.add), ["gm"], ["gm"])
        for g in range(4):
            V_("dve", lambda e, g=g: e.tensor_tensor(out=r_ml[:, :, g * 8:(g + 1) * 8], in0=lge[:, :, g * 8:(g + 1) * 8],
                                                     in1=r_gm[:, :, g:g + 1].to_broadcast([128, 16, 8]), op=ALU.add), lgk + ["gm"], [("ml", g)])
        mlk = [("ml", g) for g in range(4)]
        V_("dve", lambda e: e.tensor_reduce(out=sm[:, 2, :], in_=r_ml, axis=AX.X, op=ALU.max), mlk, ["m1v"])
        V_("dve", lambda e: e.tensor_tensor(out=r_k1, in0=r_ml, in1=sm[:, 2, :].unsqueeze(2).to_broadcast([128, 16, 32]), op=ALU.is_equal), mlk + ["m1v"], ["k1"])
        V_("dve", lambda e: e.scalar_tensor_tensor(out=r_ml2, in0=r_k1, scalar=-BIG, in1=r_ml, op0=ALU.mult, op1=ALU.add), mlk + ["k1"], ["ml2"])
        V_("dve", lambda e: e.tensor_reduce(out=sm[:, 3, :], in_=r_ml2, axis=AX.X, op=ALU.max), ["ml2"], ["m2v"])
        V_("dve", lambda e: e.tensor_tensor(out=r_k2, in0=r_ml2, in1=sm[:, 3, :].unsqueeze(2).to_broadcast([128, 16, 32]), op=ALU.is_equal), ["ml2", "m2v"], ["k2"])
        V_("dve", lambda e: e.tensor_tensor(out=sm[:, 4, :], in0=sm[:, 3, :], in1=sm[:, 2, :], op=ALU.subtract), ["m1v", "m2v"], ["dd"])
        V_("act", lambda e: e.activation(out=sm[:, 4, :], in_=sm[:, 4, :], func=AF.Exp), ["dd"], ["dd"])
        V_("dve", lambda e: e.tensor_scalar(out=sm[:, 5, :], in0=sm[:, 4, :], scalar1=1.0, scalar2=None, op0=ALU.add), ["dd"], ["s1"])
        V_("dve", lambda e: e.reciprocal(out=sm[:, 5, :], in_=sm[:, 5, :]), ["s1"], ["s1"])
        V_("dve", lambda e: e.tensor_tensor(out=sm[:, 5, :], in0=sm[:, 5, :], in1=sm[:, 1, :], op=ALU.mult), ["s1", "gsum"], ["s1"])
        V_("dve", lambda e: e.tensor_tensor(out=sm[:, 6, :], in0=sm[:, 5, :], in1=sm[:, 4, :], op=ALU.mult), ["s1", "dd"], ["s2"])
        V_("dve", lambda e: e.tensor_tensor(out=r_k1, in0=r_k1, in1=sm[:, 5, :].unsqueeze(2).to_broadcast([128, 16, 32]), op=ALU.mult), ["k1", "s1", "ml2"], ["k1"])
        V_("dve", lambda e: e.tensor_tensor(out=r_k2, in0=r_k2, in1=sm[:, 6, :].unsqueeze(2).to_broadcast([128, 16, 32]), op=ALU.mult), ["k2", "s2"], ["k2"])
        V_("dve", lambda e: e.tensor_tensor(out=combb[:], in0=r_k1, in1=r_k2, op=ALU.add), ["k1", "k2"], ["combb"])

        G2b = OV[:, 4608:5120].bitcast(BF16)
        G2f = A2b
        S.alias([("G2f", 0), ("G2f", 1)], [("A2b", 0), ("A2b", 1)])
        expand(mod2T[:, 24:32], G2f, ["mod2T"], "G2f")
        S.op("dve", lambda e: e.tensor_copy(out=G2b, in_=G2f), reads=[("G2f", 0), ("G2f", 1)], writes=["G2b"])
        er_old = [("ws", i) for i in range(3)] + [("qT", j, c) for j in range(4) for c in range(4)] + \
                 [("oT", j, c) for j in range(4) for c in range(2)] + [("oTp", 2 * kvh, ci, nbk, hh) for kvh in range(2) for ci in range(2)
                                                                      for nbk in range(ci * 4, ci * 4 + 4) for hh in range(2)]
        S.alias([("wu", s) for s in range(4)] + [("wd", s) for s in range(4)], er_old)
        wu = [ER[:, s * 6144:s * 6144 + 4096].rearrange("p (k n) -> p k n", n=512) for s in range(4)]
        wd = [ER[:, s * 6144 + 4096:(s + 1) * 6144].rearrange("p (f n) -> p f n", n=1024) for s in range(4)]
        MTb = MT
        cbs = [MTb[:, i * 1024:(i + 1) * 1024].rearrange("p (e n) -> p e n", n=512) for i in range(2)]
        sab = [MTb[:, 2048 + i * 512:2048 + (i + 1) * 512] for i in range(2)]
        t2b = [MTb[:, 3072 + i * 512:3072 + (i + 1) * 512] for i in range(2)]
        S.alias([("cbs", 0), ("cbs", 1), ("cbs2", 0), ("cbs2", 1), ("sab", 0), ("sab", 1), ("t2b", 0), ("t2b", 1)] + [("actT", bi_, el, j) for bi_ in range(2) for el in range(2) for j in range(2)],
                RKEYS)

        def load_expert(e_, fold=True):
            s = e_ % 4
            gdma(wu[s], wup_d[e_].rearrange("(k p) n -> p k n", p=128), [], [("wu", s)], ("wu", s))
            gdma(wd[s], wdn_d[e_].rearrange("(f p) n -> p f n", p=128), [], [("wd", s)], ("wd", s))
            if fold:
                fold_expert(e_)

        def fold_expert(e_):
            s = e_ % 4
            S.op("dve", lambda e, s=s: e.tensor_tensor(out=wd[s], in0=wd[s], in1=G2b.unsqueeze(1).to_broadcast([128, 2, 1024]), op=ALU.mult),
                 reads=[("wd", s), "G2b"], writes=[("wd", s)])

        UPB = [(0, 1), (2, 3)]
        ACB = [(4, 5), (6, 7)]
        up_i = [0]; ac_i = [0]; sa_i = [0]
        actTs = [MTb[:, 4096 + i * 2048:4096 + (i + 1) * 2048].rearrange("p (e f n) -> p e f n", e=2, f=2) for i in range(2)]
        for e_ in range(4):
            load_expert(e_)

        def up_stage(sidx):
            g, c = divmod(sidx, 4)
            tl = list(range(c * 4, c * 4 + 4))
            bi = sidx % 2
            actT = actTs[bi]
            for el in range(2):
                ex = 2 * g + el
                s_ = ex % 4
                for j in range(2):
                    ub = UPB[up_i[0]]; up_i[0] ^= 1
                    S.op("pe", mmgroup([(ps[ub[0]][:], wu[s_][:, k, j * 128:(j + 1) * 128], HT[:, k, c * 512:(c + 1) * 512], k == 0, k == 7) for k in range(8)]),
                         reads=[("wu", s_)] + HTr(tl), writes=[("ps", ub[0])])
                    S.op("pe", mmgroup([(ps[ub[1]][:], wu[s_][:, k, (2 + j) * 128:(3 + j) * 128], HT[:, k, c * 512:(c + 1) * 512], k == 0, k == 7) for k in range(8)]),
                         reads=[("wu", s_)] + HTr(tl), writes=[("ps", ub[1])])
                    si_ = sa_i[0]; sa_i[0] ^= 1
                    S.op("act", lambda e, ub=ub, si_=si_: e.activation(out=sab[si_], in_=ps[ub[0]][:], func=AF.Silu), reads=[("ps", ub[0])], writes=[("sab", si_)])
                    S.op("dve", lambda e, ub=ub, si_=si_: e.tensor_tensor(out=t2b[si_], in0=ps[ub[1]][:], in1=sab[si_], op=ALU.mult),
                         reads=[("ps", ub[1]), ("sab", si_)], writes=[("t2b", si_)])
                    S.op("dve", lambda e, si_=si_, el=el, j=j, bi=bi, actT=actT: e.tensor_tensor(out=actT[:, el, j, :], in0=t2b[si_], in1=cbs[bi][:, el, :], op=ALU.mult),
                         reads=[("t2b", si_), ("cbs", bi), ("cbs2", bi)], writes=[("actT", bi, el, j)])

        def cb_stage(sidx):
            g, c = divmod(sidx, 4)
            bi = sidx % 2
            ub = UPB[up_i[0]]; up_i[0] ^= 1
            for el in range(2):
                ex = 2 * g + el
                S.op("pe", mmgroup([(ps[ub[el]][:, tt * 128:(tt + 1) * 128], combb[:, c * 4 + tt, ex:ex + 1].to_broadcast([128, 128]), identb[:], True, True)
                                    for tt in range(4)]), reads=["combb", "identb"], writes=[("ps", ub[el])])
                S.op("act", lambda e, ub=ub, bi=bi, el=el: e.activation(out=cbs[bi][:, el, :], in_=ps[ub[el]][:], func=AF.Copy),
                     reads=[("ps", ub[el])], writes=[("cbs", bi)] if el == 0 else [("cbs2", bi)])

        def down_stage(sidx):
            g, c = divmod(sidx, 4)
            bi = sidx % 2
            actT = actTs[bi]
            for h in range(2):
                for tp in range(2):
                    ab = ACB[ac_i[0]]; ac_i[0] ^= 1
                    for ti in range(2):
                        tt = tp * 2 + ti
                        lst = []
                        for el in range(2):
                            s_ = (2 * g + el) % 4
                            for f in range(2):
                                lst.append((ps[ab[ti]][:], actT[:, el, f, tt * 128:(tt + 1) * 128], wd[s_][:, f, h * 512:(h + 1) * 512],
                                            el == 0 and f == 0, el == 1 and f == 1))
                        S.op("pe", mmgroup(lst), reads=[("wd", (2 * g) % 4), ("wd", (2 * g + 1) % 4)] + [("actT", bi, el, f) for el in range(2) for f in range(2)],
                             writes=[("ps", ab[ti])])
                        t = c * 4 + tt
                        S.op("dve", lambda e, b=ab[ti], t=t, h=h: e.tensor_tensor(out=x1[:, t, h * 512:(h + 1) * 512], in0=ps[b][:],
                                                                                 in1=x1[:, t, h * 512:(h + 1) * 512], op=ALU.add),
                             reads=[("ps", ab[ti]), ("x1", t)], writes=[("x1", t)])
            if c == 3:
                for el in range(2):
                    nx = 2 * g + 4 + el
                    if nx < NE:
                        load_expert(nx, fold=False)
            if c == 1 and g + 1 >= 2:
                for el in range(2):
                    nx = 2 * (g + 1) + el
                    if nx < NE:
                        fold_expert(nx)

        def final_tiles(tlist):
            for t in tlist:
                S.op("act", lambda e, t=t: e.activation(out=junk, in_=x1[:, t, :], func=AF.Square, accum_out=ss3[:, t:t + 1]),
                     reads=[("x1", t)], writes=["junk", ("ss3", t)])
                S.op("dve", lambda e, t=t: e.tensor_scalar(out=rstd3[:, t:t + 1], in0=ss3[:, t:t + 1], scalar1=1.0 / D, scalar2=1e-6, op0=ALU.mult, op1=ALU.add),
                     reads=[("ss3", t)], writes=[("rs3", t)])
                S.op("act", lambda e, t=t: e.activation(out=rstd3[:, t:t + 1], in_=rstd3[:, t:t + 1], func=AF.Sqrt), reads=[("rs3", t)], writes=[("rs3", t)])
                S.op("dve", lambda e, t=t: e.reciprocal(out=rstd3[:, t:t + 1], in_=rstd3[:, t:t + 1]), reads=[("rs3", t)], writes=[("rs3", t)])
                S.op("dve", lambda e, t=t: e.scalar_tensor_tensor(out=x1[:, t, :], in0=x1[:, t, :], scalar=rstd3[:, t:t + 1], in1=ngt, op0=ALU.mult, op1=ALU.mult),
                     reads=[("x1", t), ("rs3", t), "FGa"], writes=[("x1", t)])
                sdma(out_d[t * 128:(t + 1) * 128, :], x1[:, t, :], [("x1", t)], [("out", t)], ("out", t % 4))

        S.alias(["FGa"], ["ngt", "xnc"])
        sdma(ngt, fg_d.partition_broadcast(128), [], ["FGa"], "c13")
        NS = (NE // 2) * 4
        cb_stage(0)
        up_stage(0)
        cb_stage(1)
        for sidx in range(1, NS):
            up_stage(sidx)
            if sidx + 1 < NS:
                cb_stage(sidx + 1)
            down_stage(sidx - 1)
            if sidx - 1 >= NS - 4:
                c_ = (sidx - 1) % 4
                final_tiles(range(c_ * 4, c_ * 4 + 4))
        down_stage(NS - 1)
        final_tiles(range(12, 16))

        S.emit()
        build_program.stats = S.stats
    return nc


def _host_consts():
    n_freq = 16
    inv_freq = (np.float32(10000.0) ** (-np.arange(n_freq, dtype=np.float32) / np.float32(n_freq))).astype(np.float32)
    tok = np.arange(L)
    row = (tok // 64).astype(np.float32)
    col = (tok % 64).astype(np.float32)
    ang = np.concatenate([row[:, None] * inv_freq[None, :], col[:, None] * inv_freq[None, :]], axis=1).astype(np.float32)
    cos = np.cos(ang).astype(np.float32); sin = np.sin(ang).astype(np.float32)
    p = np.arange(128)
    c2 = cos[:, p % 32].T.copy()
    sgn = np.where((p % 64) < 32, -1.0, 1.0).astype(np.float32)
    s2 = (sin[:, p % 32].T * sgn[:, None]).astype(np.float32)
    ident = np.eye(128, dtype=np.float32)
    j = np.arange(128)[:, None]; i = np.arange(128)[None, :]
    m_prev = (j >= i).astype(np.float32)
    m_next = (j <= i).astype(np.float32)
    m01 = np.concatenate([m_prev, m_next], axis=1)
    m_ = np.arange(128)
    swm = (m_ // 64) * 64 + ((m_ % 64) + 32) % 64
    pm = np.zeros((128, 128), dtype=np.float32)
    pm[swm, m_] = 1.0
    return np.ascontiguousarray(c2), np.ascontiguousarray(s2), ident, np.ascontiguousarray(m01), pm


def _win_perm():
    b0, cg0, xin0, q0, k0, v0, ga0, gb0 = 0, 512, 1024, 1536, 2048, 2176, 2304, 3328

    def sw(base, nheads):
        idx = []
        for h in range(nheads):
            for i in range(64):
                idx.append(base + h * 64 + (i + 32) % 64)
        return idx

    cols = []
    kh = [list(range(k0 + h * 64, k0 + (h + 1) * 64)) for h in range(2)]
    ksw = sw(k0, 2)
    khs = [ksw[h * 64:(h + 1) * 64] for h in range(2)]
    cols += kh[0] + kh[0] + khs[0] + khs[0] + kh[1] + kh[1] + khs[1] + khs[1]
    cols += list(range(v0, v0 + 128))
    for jj in range(4):
        cols += list(range(cg0 + jj * 128, cg0 + (jj + 1) * 128)) + list(range(xin0 + jj * 128, xin0 + (jj + 1) * 128))
    cols += list(range(b0, b0 + 512))
    cols += list(range(ga0, ga0 + 1024))
    qsw = sw(q0, 8)
    for jj in range(4):
        cols += list(range(q0 + jj * 128, q0 + (jj + 1) * 128)) + qsw[jj * 128:(jj + 1) * 128]
    cols += list(range(gb0, gb0 + 1024))
    assert len(cols) == NCOLS
    return np.array(cols, dtype=np.int64)


_NC_CACHE = {}


def kernel(x, c, ctx, c_ctx, w_ada, b_ada, norm1_g, w_in, w_conv, b_conv, w_a, w_b, sink, w_o,
           norm2_g, w_group, b_group, w_router, b_router, w_up, w_down, final_g):
    f = lambda a: np.ascontiguousarray(np.asarray(a, dtype=np.float32))
    x = f(x); c = f(c); ctx = f(ctx); c_ctx = f(c_ctx)
    if "nc" not in _NC_CACHE:
        _NC_CACHE["nc"] = build_program()
    nc = _NC_CACHE["nc"]
    c2, s2, ident, m01, pm = _host_consts()
    w_in0 = f(w_in)[0]
    win = np.ascontiguousarray(w_in0[:, _win_perm()])
    b_ada0 = f(b_ada)[0]
    shared = dict(
        wada=f(w_ada)[0], bada1=np.ascontiguousarray(b_ada0[:2048]),
        bada2=np.ascontiguousarray(b_ada0[2048:].reshape(32, 128).T),
        ng1=f(norm1_g)[0], ng2=np.ascontiguousarray(f(norm2_g)[0].reshape(8, 128).T), fg=f(final_g),
        win=win,
        wc=np.ascontiguousarray(f(w_conv)[0].T.reshape(4, 128, 3).transpose(1, 0, 2).reshape(128, 12)),
        bc=np.ascontiguousarray(f(b_conv)[0].reshape(4, 128).T),
        wa=f(w_a)[0], wb=f(w_b)[0], wo=f(w_o)[0],
        sinkp=np.ascontiguousarray(f(sink)[0][[0, 2, 1, 3, 4, 6, 5, 7]].reshape(1, 8)),
        wrt=np.ascontiguousarray(np.concatenate([f(w_group)[0], f(w_router)[0]], axis=1)),
        brt=np.ascontiguousarray(np.concatenate([f(b_group)[0], f(b_router)[0]], axis=0)),
        wup=f(w_up)[0], wdn=f(w_down)[0],
        c2=c2, s2=s2, ident=ident, m01=m01, pm=pm,
    )
    cct = np.ascontiguousarray(c_ctx.reshape(8, 128).T)
    in_maps = []
    for b in range(8):
        m = dict(shared)
        m["x"] = x[b]
        m["ctx"] = ctx[b]
        m["cc"] = np.ascontiguousarray(np.concatenate([c[b].reshape(8, 128).T, cct], axis=1))
        in_maps.append(m)
    res = run_bass_kernel_spmd(nc, in_maps, core_ids=list(range(8)))
    return np.stack([np.asarray(res.results[b]["out"], dtype=np.float32) for b in range(8)], axis=0)
```

```python
from contextlib import ExitStack
import numpy as np
import concourse.bass as bass
import concourse.mybir as mybir
from concourse.bass_utils import run_bass_kernel_spmd

F32 = mybir.dt.float32
BF16 = mybir.dt.bfloat16
ALU = mybir.AluOpType
AF = mybir.ActivationFunctionType
AX = mybir.AxisListType

L = 2048
CTX = 256
D = 1024
NE = 32
BIG = 1.0e4
NCOLS = 5248
O_KV, O_V, O_CX0, O_CX1, O_B, O_GA0, O_GA1, O_Q0, O_Q1, O_GB0, O_GB1 = (
    0, 512, 640, 1152, 1664, 2176, 2688, 3200, 3712, 4224, 4736)


class Sched:
    ENGS = ("pe", "act", "dve", "pool", "sp")

    def __init__(self, nc, stack):
        self.nc = nc
        self.stack = stack
        self.ops = []
        self.res = {}
        self.esem = {e: stack.enter_context(nc.semaphore("sem_" + e)) for e in self.ENGS}
        self.dsems = {}

    def _dsem(self, key):
        if key not in self.dsems:
            self.dsems[key] = self.stack.enter_context(self.nc.semaphore("dsem_%d" % len(self.dsems)))
        return self.dsems[key]

    def op(self, eng, fn, reads=(), writes=(), dma=None):
        oid = len(self.ops)
        deps = set()
        for r in reads:
            st = self.res.get(r)
            if st is not None and st[0] is not None:
                deps.add(st[0])
        for w in writes:
            st = self.res.get(w)
            if st is not None:
                if st[0] is not None:
                    deps.add(st[0])
                deps.update(st[1])
        self.ops.append(dict(eng=eng, fn=fn, deps=deps, dma=dma, sig=False))
        for r in reads:
            self.res.setdefault(r, [None, []])[1].append(oid)
        for w in writes:
            self.res[w] = [oid, []]
        return oid

    def alias(self, new_keys, old_keys):
        pend = []
        for k in old_keys:
            st = self.res.pop(k, None)
            if st is not None:
                if st[0] is not None:
                    pend.append(st[0])
                pend.extend(st[1])
        pend = sorted(set(pend))
        for k in new_keys:
            st = self.res.setdefault(k, [None, []])
            st[1].extend(pend)

    def emit(self):
        ops = self.ops
        for o in ops:
            for d in o["deps"]:
                do = ops[d]
                if do["eng"] == "pe" and o["eng"] == "pe" and do["dma"] is None and o["dma"] is None:
                    continue
                do["sig"] = True
        for o in ops:
            if o["dma"] is not None:
                o["sig"] = True
        cnt = {e: 0 for e in self.ENGS}
        dcnt = {}
        for o in ops:
            if not o["sig"]:
                o["tick"] = None
                continue
            if o["dma"] is not None:
                k = ("d", o["dma"])
                dcnt[k] = dcnt.get(k, 0) + 16
                o["tick"] = (k, dcnt[k])
            else:
                cnt[o["eng"]] += 1
                o["tick"] = (("e", o["eng"]), cnt[o["eng"]])
        known = {e: {} for e in self.ENGS}
        prog = {e: [] for e in self.ENGS}
        nwaits = 0
        for o in ops:
            e = o["eng"]
            need = {}
            for d in o["deps"]:
                do = ops[d]
                if do["tick"] is None:
                    continue
                k, v = do["tick"]
                if need.get(k, 0) < v:
                    need[k] = v
            waits = []
            for k, v in need.items():
                if known[e].get(k, 0) >= v:
                    continue
                known[e][k] = v
                waits.append((k, v))
            nwaits += len(waits)
            prog[e].append((waits, o))
        final = [(k, v) for k, v in dcnt.items()]
        for e in self.ENGS:
            if e != "sp" and cnt[e] > 0:
                final.append((("e", e), cnt[e]))
        self.stats = dict(n_ops=len(ops), n_waits=nwaits, cnt=cnt, n_dsem=len(dcnt))

        def semof(k):
            return self.esem[k[1]] if k[0] == "e" else self._dsem(k[1])

        for k in dcnt:
            semof(k)
        nc = self.nc
        handles = dict(pe="tensor", act="scalar", dve="vector", pool="gpsimd", sp="sync")
        with nc.Block() as block:
            for e in self.ENGS:
                lst = prog[e]
                fin = final if e == "sp" else []
                if not lst and not fin:
                    continue

                def body(eng, lst=lst, fin=fin):
                    for waits, o in lst:
                        for k, v in waits:
                            eng.wait_ge(semof(k), v)
                        ins = o["fn"](eng)
                        if o["tick"] is not None:
                            k, v = o["tick"]
                            ins.then_inc(semof(k), 16 if k[0] == "d" else 1)
                    for k, v in fin:
                        eng.wait_ge(semof(k), v)

                getattr(block, handles[e])(body)


def build_program():
    nc = bass.Bass("TRN2", target_bir_lowering=False)

    def DIN(n, s):
        return nc.dram_tensor(n, list(s), F32, kind="ExternalInput").ap()

    x_d = DIN("x", [L, D]); ctx_d = DIN("ctx", [CTX, D]); cc_d = DIN("cc", [128, 16])
    wada_d = DIN("wada", [D, 6 * D]); bada1_d = DIN("bada1", [2048]); bada2_d = DIN("bada2", [128, 32])
    ng1_d = DIN("ng1", [D]); ng2_d = DIN("ng2", [128, 8]); fg_d = DIN("fg", [D])
    win_d = DIN("win", [D, NCOLS]); wc_d = DIN("wc", [128, 12]); bc_d = DIN("bc", [128, 4])
    wa_d = DIN("wa", [512, D]); wb_d = DIN("wb", [512, D]); wo_d = DIN("wo", [D, D])
    sink_d = DIN("sinkp", [1, 8]); wrt_d = DIN("wrt", [D, 36]); brt_d = DIN("brt", [36])
    wup_d = DIN("wup", [NE, D, 512]); wdn_d = DIN("wdn", [NE, 256, D])
    c2_d = DIN("c2", [128, L]); s2_d = DIN("s2", [128, L]); id_d = DIN("ident", [128, 128])
    m01_d = DIN("m01", [128, 256])
    pm_d = DIN("pm", [128, 128])
    out_d = nc.dram_tensor("out", [L, D], F32, kind="ExternalOutput").ap()

    with ExitStack() as st:
        def SB(n, s, dt):
            return st.enter_context(nc.sbuf_tensor("s_" + n, list(s), dt))

        XR = SB("XR", [128, 16384], F32)
        ER = SB("ER", [128, 24576], BF16)
        HT = SB("HT", [128, 8, L + CTX], BF16)
        MT = SB("MT", [128, 8192], BF16)
        OV = SB("OV", [128, 6144], F32)
        identf = SB("identf", [128, 128], F32)
        identb = SB("identb", [128, 128], BF16)
        pmb = SB("pmb", [128, 128], BF16)
        m01 = SB("m01", [128, 256], BF16)
        esb = SB("esb", [128, 8], F32)
        otm = [SB("otm%d" % i, [128, 256], BF16) for i in range(2)]
        dsm = SB("dsm", [128, 8], F32)
        G2bt = SB("G2bt", [128, 1024], BF16)
        hal = SB("hal", [128, 2], F32)
        wcs = SB("wcs", [128, 12], F32); bcs = SB("bcs", [128, 4], F32)
        ccs = SB("ccs", [128, 16], F32)
        scf = SB("scf", [128, 16], F32)
        scb1 = SB("scb1", [128, 16], BF16)
        mod2T = SB("mod2T", [128, 32], F32)
        b2T = SB("b2T", [128, 32], F32)
        ng2T = SB("ng2T", [128, 8], F32)
        a2T = SB("a2T", [128, 8], F32)
        stat = SB("stat", [128, 128], F32)
        wrt = SB("wrt", [128, 8, 36], BF16)
        brtb = SB("brtb", [128, 36], F32)
        ps = [st.enter_context(nc.psum_tensor("ps%d" % i, [128, 512], F32)) for i in range(8)]

        S = Sched(nc, st)
        _bank = [0]

        _m2lock = [True]

        def nb():
            b = _bank[0]
            if _m2lock[0] and b == 7:
                b = 0
            _bank[0] = (b + 1) % 8
            return b

        psm = ps[7]

        x1 = XR[:, :].rearrange("p (t n) -> p t n", n=D)
        mod1 = XR[:, 0:4096].rearrange("p (a n) -> p a n", n=D)
        xs = [XR[:, 4096 + i * 1024: 4096 + (i + 1) * 1024] for i in range(2)]
        xn_a = XR[:, 6144:7168]
        scb = XR[:, 7168:8192].bitcast(BF16).rearrange("p (k m) -> p k m", m=128)
        kT = XR[:, 8192:10496].bitcast(BF16).rearrange("p (a n) -> p a n", a=2)
        Vx = XR[:, 10496:13376].bitcast(BF16).rearrange("p (t n) -> p t n", n=320)
        C2 = XR[:, 13376:14400].bitcast(BF16)
        S2 = XR[:, 14400:15424].bitcast(BF16)
        wsl = [ER[:, i * 4096:(i + 1) * 4096].rearrange("p (k n) -> p k n", n=512) for i in range(3)]
        ucv = ER[:, 12288:18440].rearrange("p (j n) -> p j n", j=4)
        qT = ER[:, 12288:16384].rearrange("p (j n) -> p j n", j=4)
        cvT = ER[:, 18440:22536].rearrange("p (j n) -> p j n", j=4)
        oT = cvT
        mT = MT[:, :].rearrange("p (k n) -> p k n", k=8)
        ngt = OV[:, 0:1024]
        junk = OV[:, 1024:1536].bitcast(BF16)
        tmpA = OV[:, 1536:2048]; tmpB = OV[:, 2048:2560]; tmpC = OV[:, 2560:3072]
        Pb = OV[:, 3072:4608].bitcast(BF16).rearrange("p (t n) -> p t n", n=512)
        rdt = OV[:, 4608:5120]
        qraw = OV[:, 4608:4864].bitcast(BF16)
        qraws = [qraw, OV[:, 4864:5120].bitcast(BF16)]
        Pbs = [Pb, OV[:, 0:1536].bitcast(BF16).rearrange("p (t n) -> p t n", n=512)]
        _pb = [0]
        _ot = [0]
        G1 = OV[:, 5120:6144]

        ss = stat[:, 0:18]; rstd = stat[:, 18:36]; ss2 = stat[:, 36:52]; rstd2 = stat[:, 52:68]
        ss3 = stat[:, 68:84]; rstd3 = stat[:, 84:100]

        def gdma(out, in_, reads, writes, key):
            S.op("pool", lambda e, out=out, in_=in_: e.dma_start(out=out, in_=in_), reads=reads, writes=writes, dma=key)

        def sdma(out, in_, reads, writes, key):
            S.op("sp", lambda e, out=out, in_=in_: e.dma_start(out=out, in_=in_), reads=reads, writes=writes, dma=key)

        _ws = [0]

        def load_slot(src, nk=8, ncols=512, wide=False):
            i = _ws[0]
            _ws[0] = (i + 1) % 3
            if wide:
                dst = ER[:, i * 4096:(i + 1) * 4096].rearrange("p (k n) -> p k n", n=1024)
            else:
                dst = wsl[i][:, 0:nk, 0:ncols]
            gdma(dst, src, [], [("ws", i)], ("ws", i))
            return i

        def win_src(c0, ncols=512):
            return win_d[:, c0:c0 + ncols].rearrange("(k p) n -> p k n", p=128)

        def mmgroup(lst):
            def fn(e, lst=lst):
                ins = None
                for (o, l, r, s0, s1) in lst:
                    ins = e.matmul(o, lhsT=l, rhs=r, start=s0, stop=s1)
                return ins
            return fn

        sdma(identf[:], id_d, [], ["identf"], "c0")
        sdma(wcs[:], wc_d, [], ["wcs"], "c5")
        sdma(bcs[:], bc_d, [], ["bcs"], "c6")
        sdma(ccs[:], cc_d, [], ["ccs"], "c7")
        sdma(b2T[:], bada2_d, [], ["b2T"], "c8")
        sdma(ng2T[:], ng2_d, [], ["ng2T"], "c9")
        sdma(esb[:], sink_d.rearrange("a b -> (a b)").partition_broadcast(128), [], ["esb"], "c10")
        sdma(brtb[:], brt_d.partition_broadcast(128), [], ["brtb"], "c11")
        sdma(ngt, ng1_d.partition_broadcast(128), [], ["ngt"], "c13")
        sdma(mod1[:, 0:2, :].rearrange("p a n -> p (a n)"), bada1_d.partition_broadcast(128), [], ["mod1b"], "c14")
        sdma(mod1[:, 2:4, :].rearrange("p a n -> p (a n)"), bada1_d.partition_broadcast(128), [], ["mod1c"], "c15")

        Vx5 = Vx.rearrange("p t (b n) -> p t b n", n=64)
        for bi in (0, 2, 4):
            S.op("dve", lambda e, bi=bi: e.memset(Vx5[:, :, bi, :], 1.0), writes=[("Vones", bi)])
        S.op("act", lambda e: e.activation(out=esb[:], in_=esb[:], func=AF.Exp), reads=["esb"], writes=["esb"])
        S.op("act", lambda e: e.activation(out=scf[:], in_=ccs[:], func=AF.Silu), reads=["ccs"], writes=["scf"])
        S.op("dve", lambda e: e.tensor_copy(out=scb, in_=scf[:, :].unsqueeze(2).to_broadcast([128, 16, 128])),
             reads=["scf"], writes=["scb"])
        S.op("dve", lambda e: e.tensor_copy(out=scb1[:], in_=scf[:]), reads=["scf"], writes=["scb1"])

        for n in range(4):
            si = load_slot(wada_d[:, n * 512:(n + 1) * 512].rearrange("(k p) n -> p k n", p=128))
            b0 = nb(); b1 = nb()
            S.op("pe", mmgroup([(ps[b0][:], scb[:, k, :], wsl[si][:, k, :], k == 0, k == 7) for k in range(8)]),
                 reads=["scb", ("ws", si)], writes=[("ps", b0)])
            S.op("pe", mmgroup([(ps[b1][:], scb[:, 8 + k, :], wsl[si][:, k, :], k == 0, k == 7) for k in range(8)]),
                 reads=["scb", ("ws", si)], writes=[("ps", b1)])
            a, off = divmod(n, 2)
            dst0 = mod1[:, a, off * 512:(off + 1) * 512]
            dst1 = mod1[:, 2 + a, off * 512:(off + 1) * 512]
            S.op("dve", lambda e, d=dst0, p=ps[b0]: e.tensor_tensor(out=d, in0=p[:], in1=d, op=ALU.add),
                 reads=[("ps", b0), "mod1b"], writes=[("m1", 0, n)])
            S.op("dve", lambda e, d=dst1, p=ps[b1]: e.tensor_tensor(out=d, in0=p[:], in1=d, op=ALU.add),
                 reads=[("ps", b1), "mod1c"], writes=[("m1", 1, n)])
        gdma(identb[:], id_d, [], ["identb"], "c1")
        gdma(pmb[:], pm_d, [], ["pmb"], "c16")
        gdma(m01[:], m01_d, [], ["m01"], "c2")
        gdma(C2, c2_d, [], ["C2"], "c3")
        gdma(S2, s2_d, [], ["S2"], "c4")
        gdma(wrt[:], wrt_d.rearrange("(k p) n -> p k n", p=128), [], ["wrt"], "c12")
        for c in range(2):
            S.op("dve", lambda e, c=c: e.scalar_tensor_tensor(out=mod1[:, 2 * c + 1, :], in0=mod1[:, 2 * c + 1, :], scalar=1.0,
                                                              in1=ngt, op0=ALU.add, op1=ALU.mult),
                 reads=[("m1", c, 2), ("m1", c, 3), "ngt"], writes=[("A1", c)])

        def norm_stats(src, ssc, rsc, rd_keys, key, finish=True):
            S.op("act", lambda e: e.activation(out=junk, in_=src, func=AF.Square, accum_out=ssc),
                 reads=rd_keys, writes=["junk", ("ss", key)])
            if finish:
                norm_rstd(ssc, rsc, [("ss", key)], ("rs", key))

        def norm_rstd(ssc, rsc, rd, wkey):
            S.op("dve", lambda e: e.tensor_scalar(out=rsc, in0=ssc, scalar1=1.0 / D, scalar2=1e-6, op0=ALU.mult, op1=ALU.add),
                 reads=rd, writes=[wkey])
            S.op("act", lambda e: e.activation(out=rsc, in_=rsc, func=AF.Sqrt), reads=[wkey], writes=[wkey])
            S.op("dve", lambda e: e.reciprocal(out=rsc, in_=rsc), reads=[wkey], writes=[wkey])

        def norm_apply(src, rsc, Abc, Bbc, xn, xnkey, col0, rd_keys, rskey, htkey, defer=False):
            deferred = []
            S.op("dve", lambda e: e.scalar_tensor_tensor(out=xn, in0=src, scalar=rsc, in1=Abc[0], op0=ALU.mult, op1=ALU.mult),
                 reads=list(rd_keys) + [rskey] + Abc[1], writes=[xnkey])
            S.op("dve", lambda e: e.tensor_tensor(out=xn, in0=xn, in1=Bbc[0], op=ALU.add), reads=[xnkey] + Bbc[1], writes=[xnkey])
            ba = nb(); bb_ = nb()
            for half, bk in ((0, ba), (1, bb_)):
                def fn(e, half=half, bk=bk):
                    ins = None
                    for kk in range(4):
                        k = half * 4 + kk
                        ins = e.transpose(ps[bk][:, kk * 128:(kk + 1) * 128], xn[:, k * 128:(k + 1) * 128], identf[:])
                    return ins
                S.op("pe", fn, reads=[xnkey, "identf"], writes=[("ps", bk)])

                def cp(half=half, bk=bk):
                    S.op("act", lambda e, half=half, bk=bk: e.activation(
                        out=HT[:, half * 4:(half + 1) * 4, col0:col0 + 128],
                        in_=ps[bk][:, :].rearrange("p (k n) -> p k n", n=128), func=AF.Copy),
                        reads=[("ps", bk)], writes=[("HT", htkey, half)])
                if defer:
                    deferred.append(cp)
                else:
                    cp()
            return deferred

        xs4 = [xs[0], xs[1], XR[:, 7168:8192], XR[:, 8192:9216]]
        xn_s2 = XR[:, 9216:10240]
        S.alias([("xs", 2)], ["scb"])

        def s0_dma(t):
            sl = t % 4
            src_d = x_d[t * 128:(t + 1) * 128, :] if t < 16 else ctx_d[(t - 16) * 128:(t - 15) * 128, :]
            sdma(xs4[sl], src_d, [], [("xs", sl)], ("xs", sl))

        def s0_sq(t):
            sl = t % 4
            S.op("act", lambda e: e.activation(out=junk, in_=xs4[sl], func=AF.Square, accum_out=ss[:, t:t + 1]),
                 reads=[("xs", sl)], writes=["junk", ("ss", t)])

        def s0_ts(t):
            S.op("dve", lambda e: e.tensor_scalar(out=rstd[:, t:t + 1], in0=ss[:, t:t + 1], scalar1=1.0 / D, scalar2=1e-6, op0=ALU.mult, op1=ALU.add),
                 reads=[("ss", t)], writes=[("rs", t)])

        def s0_sr(t):
            S.op("act", lambda e: e.activation(out=rstd[:, t:t + 1], in_=rstd[:, t:t + 1], func=AF.Sqrt), reads=[("rs", t)], writes=[("rs", t)])

        def s0_rc(t):
            S.op("dve", lambda e: e.reciprocal(out=rstd[:, t:t + 1], in_=rstd[:, t:t + 1]), reads=[("rs", t)], writes=[("rs", t)])

        def s0_apply(t):
            sl = t % 4
            c = 0 if t < 16 else 1
            xb_, xk_ = (xn_a, "xn") if t % 2 == 0 else (xn_s2, "xn2")
            return norm_apply(xs4[sl], rstd[:, t:t + 1],
                              (mod1[:, 2 * c + 1, :], [("A1", c)]),
                              (mod1[:, 2 * c, :], [("m1", c, 0), ("m1", c, 1)]),
                              xb_, xk_, t * 128, [("xs", sl)], ("rs", t), t, defer=True)

        NT0 = 18
        for t in range(3):
            s0_dma(t)
        for t in range(3):
            s0_sq(t)
        s0_ts(0); s0_sr(0); s0_rc(0)
        s0_ts(1); s0_sr(1)
        pend_cp = []
        for t in range(NT0):
            if t + 3 < NT0:
                s0_dma(t + 3)
                s0_sq(t + 3)
            if t + 2 < NT0:
                s0_ts(t + 2)
                s0_sr(t + 2)
            if t + 1 < NT0:
                s0_rc(t + 1)
            prev_cp = pend_cp
            pend_cp = s0_apply(t)
            for cp_ in prev_cp:
                cp_()
        for cp_ in pend_cp:
            cp_()
        S.alias([("kT", kvh, tc) for kvh in range(2) for tc in range(5)], [("xs", 3), "xn2"])

        def HTr(tlist):
            return [("HT", t, h) for t in tlist for h in (0, 1)]

        si_kv = load_slot(win_src(O_KV))
        k_units = [(tc, kvh) for tc in range(4) for kvh in range(2)]

        def k_proj(ui):
            tc, kvh = k_units[ui]
            t0 = tc * 512
            tl = list(range(tc * 4, tc * 4 + 4))
            bA = nb()
            qb = qraws[ui % 2]
            S.op("pe", mmgroup([(ps[bA][:], wsl[si_kv][:, k, (2 * kvh) * 128:(2 * kvh + 1) * 128], HT[:, k, t0:t0 + 512], k == 0, k == 7)
                                for k in range(8)]), reads=[("ws", si_kv)] + HTr(tl), writes=[("ps", bA)])
            S.op("act", lambda e, bA=bA, qb=qb: e.activation(out=qb, in_=ps[bA][:], func=AF.Copy), reads=[("ps", bA)], writes=[("qraw", ui % 2), ("psr", bA)])
            return bA

        def k_rope(ui, bA):
            tc, kvh = k_units[ui]
            t0 = tc * 512
            qb = qraws[ui % 2]
            bB = nb()
            S.op("pe", lambda e, bB=bB, qb=qb: e.matmul(ps[bB][:], lhsT=pmb[:], rhs=qb, start=True, stop=True),
                 reads=["pmb", ("qraw", ui % 2)], writes=[("ps", bB)])
            S.op("dve", lambda e, bA=bA, t0=t0: e.tensor_tensor(out=tmpA, in0=ps[bA][:], in1=C2[:, t0:t0 + 512], op=ALU.mult),
                 reads=[("ps", bA), ("psr", bA), "C2"], writes=["tmpA"])
            S.op("dve", lambda e, bB=bB, t0=t0: e.tensor_tensor(out=tmpB, in0=ps[bB][:], in1=S2[:, t0:t0 + 512], op=ALU.mult),
                 reads=[("ps", bB), "S2"], writes=["tmpB"])
            S.op("dve", lambda e, kvh=kvh, t0=t0, tc=tc: e.tensor_tensor(out=kT[:, kvh, t0:t0 + 512], in0=tmpA, in1=tmpB, op=ALU.add),
                 reads=["tmpA", "tmpB"], writes=[("kT", kvh, tc)])

        kbanks = {0: k_proj(0)}
        for ui in range(len(k_units)):
            if ui + 1 < len(k_units):
                kbanks[ui + 1] = k_proj(ui + 1)
            k_rope(ui, kbanks[ui])
        for kvh in range(2):
            bA = nb()
            S.op("pe", mmgroup([(ps[bA][:, 0:256], wsl[si_kv][:, k, (2 * kvh) * 128:(2 * kvh + 1) * 128], HT[:, k, 2048:2304], k == 0, k == 7)
                                for k in range(8)]), reads=[("ws", si_kv)] + HTr([16, 17]), writes=[("ps", bA)])
            S.op("act", lambda e, bA=bA, kvh=kvh: e.activation(out=kT[:, kvh, 2048:2304], in_=ps[bA][:, 0:256], func=AF.Copy),
                 reads=[("ps", bA)], writes=[("kT", kvh, 4)])
        si = load_slot(win_src(O_V, 128), 8, 128)
        for g in range(5):
            tl = list(range(g * 4, min(g * 4 + 4, 18)))
            bk = nb()
            lst = []
            for i, t in enumerate(tl):
                for k in range(8):
                    lst.append((ps[bk][:, i * 128:(i + 1) * 128], HT[:, k, t * 128:(t + 1) * 128], wsl[si][:, k, 0:128], k == 0, k == 7))
            S.op("pe", mmgroup(lst), reads=[("ws", si)] + HTr(tl), writes=[("ps", bk)])
            nt = len(tl)
            for kv in range(2):
                S.op("act", lambda e, bk=bk, g=g, nt=nt, kv=kv: e.activation(
                    out=Vx5[:, g * 4:g * 4 + nt, 1 + 2 * kv, :],
                    in_=ps[bk][:, 0:nt * 128].rearrange("p (t n) -> p t n", n=128)[:, :, kv * 64:(kv + 1) * 64], func=AF.Copy),
                    reads=[("ps", bk)], writes=[("V", g, kv)])

        _m2 = {}

        wsx = [XR[:, i * 2048:(i + 1) * 2048].bitcast(BF16).rearrange("p (k n) -> p k n", n=512) for i in range(2)]

        def mod2_load(n):
            i = n % 2
            gdma(wsx[i], wada_d[:, 2048 + n * 512:2048 + (n + 1) * 512].rearrange("(k p) n -> p k n", p=128), [], [("wsx", i)], ("wsx", i))

        def mod2_chunk(n):
            i = n % 2
            lst = []
            for j in range(4):
                col = n * 4 + j
                for k in range(8):
                    lst.append((psm[:, col:col + 1], wsx[i][:, k, j * 128:(j + 1) * 128], scb1[:, k:k + 1], k == 0, k == 7))
            S.op("pe", mmgroup(lst), reads=[("wsx", i), "scb1"], writes=[("ps", 7)])

        def mod2_finish():
            S.op("dve", lambda e: e.tensor_tensor(out=mod2T[:], in0=psm[:, 0:32], in1=b2T[:], op=ALU.add),
                 reads=[("ps", 7), "b2T"], writes=["mod2T"])
            S.op("dve", lambda e: e.scalar_tensor_tensor(out=a2T[:], in0=mod2T[:, 16:24], scalar=1.0, in1=ng2T[:], op0=ALU.add, op1=ALU.mult),
                 reads=["mod2T", "ng2T"], writes=["a2T"])

        def expand(vecT, dst, rd, wr, bf=False):
            for h in range(2):
                bk = nb()
                S.op("pe", mmgroup([(ps[bk][:, j * 128:(j + 1) * 128], vecT[:, h * 4 + j:h * 4 + j + 1].to_broadcast([128, 128]), identf[:], True, True)
                                    for j in range(4)]), reads=rd + ["identf"], writes=[("ps", bk)])
                S.op("act", lambda e, bk=bk, h=h: e.activation(out=dst[:, h * 512:(h + 1) * 512], in_=ps[bk][:], func=AF.Copy),
                     reads=[("ps", bk)], writes=[(wr, h)])

        mixer_keys_ucv = []
        for half in range(2):
            chunks = [2 * half, 2 * half + 1]
            cx_chunks = chunks
            cx0 = chunks[0]
            halo_tok = 1024 if half == 0 else 1023
            halo_col = 1025 if half == 0 else 0
            pad_col = 0 if half == 0 else 1025
            if half == 1:
                S.alias([("ucv", j, c) for j in range(4) for c in range(4)] + ["ucvpad"] + [("ucvh", j) for j in range(4)],
                        [("qT", j, c) for j in range(4) for c in range(4)])
            S.op("dve", lambda e, pc=pad_col: e.memset(ucv[:, :, pc:pc + 1], 0.0), writes=["ucvpad"])
            for sidx, off in enumerate((O_CX0, O_CX1)):
                si = load_slot(win_src(off))
                for jj in range(2):
                    j = sidx * 2 + jj
                    bH = nb()
                    lst = []
                    for q_ in range(2):
                        for k in range(8):
                            lst.append((ps[bH][:, q_:q_ + 1], wsl[si][:, k, (2 * jj + q_) * 128:(2 * jj + q_ + 1) * 128],
                                        HT[:, k, halo_tok:halo_tok + 1], k == 0, k == 7))
                    S.op("pe", mmgroup(lst), reads=[("ws", si)] + HTr([halo_tok // 128]), writes=[("ps", bH)])
                    S.op("act", lambda e, bH=bH: e.activation(out=hal[:, 0:1], in_=ps[bH][:, 0:1], func=AF.Copy), reads=[("ps", bH)], writes=["hal"])
                    S.op("dve", lambda e, bH=bH, j=j, hc=halo_col: e.tensor_tensor(out=ucv[:, j, hc:hc + 1], in0=ps[bH][:, 1:2], in1=hal[:, 0:1], op=ALU.mult),
                         reads=[("ps", bH), "hal"], writes=[("ucvh", j)])
                for c in cx_chunks:
                    tl = list(range(c * 4, c * 4 + 4))
                    for jj in range(2):
                        j = sidx * 2 + jj
                        bA = nb(); bB = nb()
                        S.op("pe", mmgroup([(ps[bA][:], wsl[si][:, k, (2 * jj) * 128:(2 * jj + 1) * 128], HT[:, k, c * 512:(c + 1) * 512], k == 0, k == 7)
                                            for k in range(8)]), reads=[("ws", si)] + HTr(tl), writes=[("ps", bA)])
                        S.op("pe", mmgroup([(ps[bB][:], wsl[si][:, k, (2 * jj + 1) * 128:(2 * jj + 2) * 128], HT[:, k, c * 512:(c + 1) * 512], k == 0, k == 7)
                                            for k in range(8)]), reads=[("ws", si)] + HTr(tl), writes=[("ps", bB)])
                        S.op("act", lambda e, bA=bA: e.activation(out=tmpA, in_=ps[bA][:], func=AF.Copy), reads=[("ps", bA)], writes=["tmpA"])
                        o0 = 1 + (c - cx0) * 512
                        S.op("dve", lambda e, bB=bB, j=j, o0=o0: e.tensor_tensor(out=ucv[:, j, o0:o0 + 512], in0=ps[bB][:], in1=tmpA, op=ALU.mult),
                             reads=[("ps", bB), "tmpA"], writes=[("ucv", j, c)])
            if half == 1:
                S.alias([("cvT", j, c) for j in range(4) for c in range(2)], [("oT", j, c) for j in range(4) for c in range(2)])
            si = load_slot(win_src(O_B))
            for ci, c in enumerate(chunks):
                tl = list(range(c * 4, c * 4 + 4))
                nbrs = [cc_ for cc_ in (c - 1, c, c + 1) if 0 <= cc_ < 4]
                for j in range(4):
                    o0 = (c - cx0) * 512
                    rdk = [("ucv", j, cc_) for cc_ in nbrs] + ["ucvpad", "wcs", "bcs", ("ucvh", j)]
                    S.op("dve", lambda e, j=j, o0=o0: e.tensor_scalar(out=tmpB, in0=ucv[:, j, o0:o0 + 512], scalar1=wcs[:, j * 3:j * 3 + 1],
                                                                      scalar2=bcs[:, j:j + 1], op0=ALU.mult, op1=ALU.add),
                         reads=rdk, writes=["tmpB"])
                    S.op("dve", lambda e, j=j, o0=o0: e.scalar_tensor_tensor(out=tmpB, in0=ucv[:, j, o0 + 1:o0 + 513], scalar=wcs[:, j * 3 + 1:j * 3 + 2],
                                                                             in1=tmpB, op0=ALU.mult, op1=ALU.add),
                         reads=rdk + ["tmpB"], writes=["tmpB"])
                    S.op("dve", lambda e, j=j, o0=o0: e.scalar_tensor_tensor(out=tmpB, in0=ucv[:, j, o0 + 2:o0 + 514], scalar=wcs[:, j * 3 + 2:j * 3 + 3],
                                                                             in1=tmpB, op0=ALU.mult, op1=ALU.add),
                         reads=rdk + ["tmpB"], writes=["tmpB"])
                    bk = nb()
                    S.op("pe", mmgroup([(ps[bk][:], wsl[si][:, k, j * 128:(j + 1) * 128], HT[:, k, c * 512:(c + 1) * 512], k == 0, k == 7)
                                        for k in range(8)]), reads=[("ws", si)] + HTr(tl), writes=[("ps", bk)])
                    S.op("dve", lambda e, bk=bk, j=j, ci=ci: e.tensor_tensor(out=cvT[:, j, ci * 512:(ci + 1) * 512], in0=ps[bk][:], in1=tmpB, op=ALU.mult),
                         reads=[("ps", bk), "tmpB"], writes=[("cvT", j, ci)])
            sg0_ = load_slot(win_src(O_GA0))
            sa_ = load_slot(wa_d.rearrange("(k p) n -> p k n", p=128), wide=True)
            wa_v = ER[:, sa_ * 4096:(sa_ + 1) * 4096].rearrange("p (k n) -> p k n", n=1024)
            sg = [sg0_, load_slot(win_src(O_GA1))]
            for ci, c in enumerate(chunks):
                tl = list(range(c * 4, c * 4 + 4))
                for oc in range(8):
                    bY = nb(); bG = nb()
                    S.op("pe", mmgroup([(ps[bY][:], wa_v[:, k, oc * 128:(oc + 1) * 128], cvT[:, k, ci * 512:(ci + 1) * 512], k == 0, k == 3)
                                        for k in range(4)]), reads=[("ws", sa_)] + [("cvT", k, ci) for k in range(4)], writes=[("ps", bY)])
                    sgi = sg[oc // 4]
                    S.op("pe", mmgroup([(ps[bG][:], wsl[sgi][:, k, (oc % 4) * 128:(oc % 4 + 1) * 128], HT[:, k, c * 512:(c + 1) * 512], k == 0, k == 7)
                                        for k in range(8)]), reads=[("ws", sgi)] + HTr(tl), writes=[("ps", bG)])
                    S.op("act", lambda e, bG=bG: e.activation(out=tmpA, in_=ps[bG][:], func=AF.Sigmoid), reads=[("ps", bG)], writes=["tmpA"])
                    S.op("dve", lambda e, bY=bY, oc=oc, ci=ci: e.tensor_tensor(out=mT[:, oc, ci * 512:(ci + 1) * 512], in0=ps[bY][:], in1=tmpA, op=ALU.mult),
                         reads=[("ps", bY), "tmpA"], writes=[("mT", oc, ci)])
            S.alias([("qT", j, c) for j in range(4) for c in range(4)],
                    [("ucv", j, c) for j in range(4) for c in range(4)] + ["ucvpad"] + [("ucvh", j) for j in range(4)])
            if half == 0:
                S.alias([("qraw", 0)], ["qraw"])
            q_slots = [load_slot(win_src(O_Q0)), load_slot(win_src(O_Q1))]
            q_units = [(sidx, ci, c, jj) for sidx in range(2) for ci, c in enumerate(chunks) for jj in range(2)]

            def q_proj(ui):
                sidx, ci, c, jj = q_units[ui]
                si = q_slots[sidx]
                tl = list(range(c * 4, c * 4 + 4))
                bA = nb()
                qb = qraws[ui % 2]
                S.op("pe", mmgroup([(ps[bA][:], wsl[si][:, k, (2 * jj) * 128:(2 * jj + 1) * 128], HT[:, k, c * 512:(c + 1) * 512], k == 0, k == 7)
                                    for k in range(8)]), reads=[("ws", si)] + HTr(tl), writes=[("ps", bA)])
                S.op("act", lambda e, bA=bA, qb=qb: e.activation(out=qb, in_=ps[bA][:], func=AF.Copy), reads=[("ps", bA)], writes=[("qraw", ui % 2), ("psr", bA)])
                return bA

            def q_rope(ui, bA):
                sidx, ci, c, jj = q_units[ui]
                j = sidx * 2 + jj
                qb = qraws[ui % 2]
                bB = nb()
                S.op("pe", lambda e, bB=bB, qb=qb: e.matmul(ps[bB][:], lhsT=pmb[:], rhs=qb, start=True, stop=True),
                     reads=["pmb", ("qraw", ui % 2)], writes=[("ps", bB)])
                S.op("dve", lambda e, bA=bA, c=c: e.tensor_tensor(out=tmpA, in0=ps[bA][:], in1=C2[:, c * 512:(c + 1) * 512], op=ALU.mult),
                     reads=[("ps", bA), ("psr", bA), "C2"], writes=["tmpA"])
                S.op("dve", lambda e, bB=bB, c=c: e.tensor_tensor(out=tmpB, in0=ps[bB][:], in1=S2[:, c * 512:(c + 1) * 512], op=ALU.mult),
                     reads=[("ps", bB), "S2"], writes=["tmpB"])
                S.op("dve", lambda e, j=j, ci=ci: e.tensor_tensor(out=qT[:, j, ci * 512:(ci + 1) * 512], in0=tmpA, in1=tmpB, op=ALU.add),
                     reads=["tmpA", "tmpB"], writes=[("qT", j, ci)])

            qbanks = {0: q_proj(0)}
            for ui in range(len(q_units)):
                if ui + 1 < len(q_units):
                    qbanks[ui + 1] = q_proj(ui + 1)
                q_rope(ui, qbanks[ui])
            S.alias([("P", 1, i, hh) for i in range(6) for hh in range(2)], ["junk", "ngt"])
            S.alias([("oT", j, c) for j in range(4) for c in range(2)], [("cvT", j, c) for j in range(4) for c in range(2)])
            def att_unit(nbk, kvh):
                n = half * 8 + nbk
                kts = []
                if n > 0:
                    kts.append(((n - 1) * 128, n - 1, 0))
                kts.append((n * 128, n, None))
                if n < 15:
                    kts.append(((n + 1) * 128, n + 1, 1))
                kts.append((2048, 16, None)); kts.append((2176, 17, None))
                pbi = _pb[0]; _pb[0] ^= 1
                return dict(nbk=nbk, kvh=kvh, n=n, ci=nbk // 4, qc0=nbk * 128, kts=kts, pbi=pbi)

            _pS = [0]; _pO = [0]; _pT = [0]
            poolT = [6] if half == 0 else [6, 7]

            def nbS():
                b_ = (0, 1, 2, 3)[_pS[0] % 4]; _pS[0] += 1
                return b_

            def nbO():
                b_ = (4, 5)[_pO[0] % 2]; _pO[0] += 1
                return b_

            def nbT():
                b_ = poolT[_pT[0] % len(poolT)]; _pT[0] += 1
                return b_

            def att_scores(u):
                kts = u["kts"]; kvh = u["kvh"]; pbi = u["pbi"]; qc0 = u["qc0"]; ci = u["ci"]
                Pb = Pbs[pbi]
                nk = len(kts)
                for p0 in range(0, nk, 2):
                    pr = kts[p0:p0 + 2]
                    w = len(pr) * 256
                    for hh in range(2):
                        bk = nbS()
                        lst = []
                        for i, (kc, vt, mk) in enumerate(pr):
                            lst.append((ps[bk][:, i * 256:(i + 1) * 256], kT[hh * 64:(hh + 1) * 64, kvh, kc:kc + 128],
                                        qT[hh * 64:(hh + 1) * 64, 2 * kvh:2 * kvh + 2, qc0:qc0 + 128], True, True))
                        kc_tcs = sorted(set(kc // 512 for (kc, _, _) in pr))
                        S.op("pe", mmgroup(lst), reads=[("kT", kvh, t_) for t_ in kc_tcs] + [("qT", 2 * kvh, ci), ("qT", 2 * kvh + 1, ci)],
                             writes=[("ps", bk)])
                        S.op("act", lambda e, bk=bk, p0=p0, hh=hh, w=w, npr=len(pr), Pb=Pb: e.activation(
                            out=Pb[:, p0:p0 + npr, hh * 256:(hh + 1) * 256],
                            in_=ps[bk][:, 0:w].rearrange("p (t n) -> p t n", n=256), func=AF.Exp, scale=0.125),
                            reads=[("ps", bk)], writes=[("P", pbi, p0 + i, hh) for i in range(len(pr))])
                for i, (kc, vt, mk) in enumerate(kts):
                    if mk is not None:
                        S.op("pool", lambda e, i=i, mk=mk, Pb=Pb: e.tensor_tensor(
                            out=Pb[:, i, :].rearrange("p (b n) -> p b n", n=128), in0=Pb[:, i, :].rearrange("p (b n) -> p b n", n=128),
                            in1=m01[:, mk * 128:(mk + 1) * 128].unsqueeze(1).to_broadcast([128, 4, 128]), op=ALU.mult),
                            reads=[("P", pbi, i, 0), ("P", pbi, i, 1), "m01"], writes=[("P", pbi, i, 0), ("P", pbi, i, 1)])

            def att_pv(u):
                kts = u["kts"]; kvh = u["kvh"]; pbi = u["pbi"]; qc0 = u["qc0"]; ci = u["ci"]; nbk = u["nbk"]
                Pb = Pbs[pbi]
                nk = len(kts)
                bO = nbO()
                lst = []
                for cb in range(4):
                    for i, (kc, vt, mk) in enumerate(kts):
                        lst.append((ps[bO][:, cb * 65:(cb + 1) * 65], Pb[:, i, cb * 128:(cb + 1) * 128],
                                    Vx[:, vt, (1 + 2 * kvh) * 64:(1 + 2 * kvh) * 64 + 65], i == 0, i == nk - 1))
                vrd = [("V", vt // 4, kvh) for (_, vt, _) in kts] + [("Vones", 0), ("Vones", 2), ("Vones", 4)]
                prd = [("P", pbi, i, hh) for i in range(nk) for hh in range(2)]
                S.op("pe", mmgroup(lst), reads=vrd + prd, writes=[("ps", bO)])
                O4 = ps[bO][:, 0:260].rearrange("p (c n) -> p c n", n=65)
                oi = _ot[0]; _ot[0] ^= 1
                ot = otm[oi]
                S.op("dve", lambda e, O4=O4, kvh=kvh: e.tensor_tensor(out=dsm[:, 0:4], in0=O4[:, :, 64], in1=esb[:, kvh * 4:(kvh + 1) * 4], op=ALU.add),
                     reads=[("ps", bO), "esb"], writes=["dsm"])
                S.op("dve", lambda e: e.reciprocal(out=dsm[:, 4:8], in_=dsm[:, 0:4]), reads=["dsm"], writes=["dsm"])
                for hh in range(2):
                    S.op("dve", lambda e, O4=O4, hh=hh, ot=ot: e.tensor_tensor(
                        out=ot[:, :].rearrange("p (jj hh d) -> p jj hh d", jj=2, hh=2)[:, :, hh, :],
                        in0=O4[:, hh * 2:hh * 2 + 2, 0:64],
                        in1=dsm[:, 4 + hh * 2:4 + hh * 2 + 2].unsqueeze(2).to_broadcast([128, 2, 64]), op=ALU.mult),
                        reads=[("ps", bO), "dsm"], writes=[("otm", oi, hh)])
                u["oi"] = oi

            def att_tr(u):
                kvh = u["kvh"]; qc0 = u["qc0"]; ci = u["ci"]; nbk = u["nbk"]; oi = u["oi"]
                ot = otm[oi]
                bT = nbT()
                psb = ps[bT][:, :].bitcast(BF16)
                S.op("pe", lambda e, ot=ot, psb=psb: [e.transpose(psb[:, jj * 128:(jj + 1) * 128], ot[:, jj * 128:(jj + 1) * 128], identb[:]) for jj in range(2)][-1],
                     reads=[("otm", oi, 0), ("otm", oi, 1), "identb"], writes=[("ps", bT)])
                S.op("act", lambda e, psb=psb, kvh=kvh, qc0=qc0: e.activation(
                    out=oT[:, 2 * kvh:2 * kvh + 2, qc0:qc0 + 128], in_=psb[:, 0:256].rearrange("p (j n) -> p j n", n=128), func=AF.Copy),
                    reads=[("ps", bT)], writes=[("oTp", 2 * kvh, ci, nbk, 0), ("oT", 2 * kvh, ci), ("oT", 2 * kvh + 1, ci)])

            units = [att_unit(nbk, kvh) for nbk in range(8) for kvh in range(2)]
            if half == 0:
                S.alias([("wsx", 0), ("wsx", 1)], [("m1", c_, n_) for c_ in range(2) for n_ in range(4)] + [("A1", 0), ("A1", 1), "mod1b", "mod1c"])
                mod2_load(0); mod2_load(1)
            sg_D = [load_slot(win_src(O_GB0)), load_slot(win_src(O_GB1))]
            sb_ = load_slot(wb_d.rearrange("(k p) n -> p k n", p=128), wide=True)
            NU = len(units)
            for ui in range(NU + 2):
                if ui < NU:
                    att_scores(units[ui])
                if 0 <= ui - 1 < NU:
                    att_pv(units[ui - 1])
                if 0 <= ui - 2 < NU:
                    att_tr(units[ui - 2])
                if half == 0 and ui >= 2 and ui % 2 == 0 and ui // 2 - 1 < 7:
                    n_ = ui // 2 - 1
                    mod2_chunk(n_)
                    if n_ + 2 < 8:
                        mod2_load(n_ + 2)
            if half == 0:
                mod2_chunk(7)
                mod2_finish()
                _m2lock[0] = False
                expand(mod2T[:, 0:8], G1, ["mod2T"], "G1")
                expand(mod2T[:, 24:32], G2bt, ["mod2T"], "G2bt")
            S.alias(["junk", "ngt"], [("P", 1, i, hh) for i in range(6) for hh in range(2)])
            wb_v = ER[:, sb_ * 4096:(sb_ + 1) * 4096].rearrange("p (k n) -> p k n", n=1024)
            sg = sg_D
            for ci, c in enumerate(chunks):
                tl = list(range(c * 4, c * 4 + 4))
                ord_ = [("oTp", 2 * kvh, ci, nbk, 0) for kvh in range(2) for nbk in range(ci * 4, ci * 4 + 4)]
                for oc in range(8):
                    bY = nb(); bG = nb()
                    S.op("pe", mmgroup([(ps[bY][:], wb_v[:, k, oc * 128:(oc + 1) * 128], oT[:, k, ci * 512:(ci + 1) * 512], k == 0, k == 3)
                                        for k in range(4)]), reads=[("ws", sb_)] + ord_ + [("oT", k, ci) for k in range(4)], writes=[("ps", bY)])
                    sgi = sg[oc // 4]
                    S.op("pe", mmgroup([(ps[bG][:], wsl[sgi][:, k, (oc % 4) * 128:(oc % 4 + 1) * 128], HT[:, k, c * 512:(c + 1) * 512], k == 0, k == 7)
                                        for k in range(8)]), reads=[("ws", sgi)] + HTr(tl), writes=[("ps", bG)])
                    S.op("act", lambda e, bG=bG: e.activation(out=tmpA, in_=ps[bG][:], func=AF.Sigmoid), reads=[("ps", bG)], writes=["tmpA"])
                    S.op("dve", lambda e, bY=bY: e.tensor_tensor(out=tmpC.bitcast(BF16)[:, 0:512], in0=ps[bY][:], in1=tmpA, op=ALU.mult),
                         reads=[("ps", bY), "tmpA"], writes=["tmpC"])
                    S.op("dve", lambda e, oc=oc, ci=ci: e.tensor_tensor(out=mT[:, oc, ci * 512:(ci + 1) * 512], in0=mT[:, oc, ci * 512:(ci + 1) * 512],
                                                                        in1=tmpC.bitcast(BF16)[:, 0:512], op=ALU.add),
                         reads=[("mT", oc, ci), "tmpC"], writes=[("mT", oc, ci)])
            so = [load_slot(wo_d[:, h * 512:(h + 1) * 512].rearrange("(k p) n -> p k n", p=128)) for h in range(2)]
            if half == 0:
                S.alias([("x1", t) for t in range(8)], [("m1", c, n) for c in range(2) for n in range(4)] + [("A1", 0), ("A1", 1), ("xs", 0), ("xs", 1), ("xs", 2), "xn", "scb", "mod1b", "mod1c", ("wsx", 0), ("wsx", 1)])
            else:
                S.alias([("x1", t) for t in range(8, 16)], [("kT", kvh, tc) for kvh in range(2) for tc in range(5)] + [("V", g, kv) for g in range(5) for kv in range(2)]
                        + [("Vones", 0), ("Vones", 2), ("Vones", 4), "C2", "S2"])
            for tt in range(8):
                t = half * 8 + tt
                sdma(x1[:, t, :], x_d[t * 128:(t + 1) * 128, :], [], [("x1", t)], ("x1", t))
            for h in range(2):
                for tt in range(8):
                    t = half * 8 + tt
                    ci = tt // 4
                    bk = nb()
                    S.op("pe", mmgroup([(ps[bk][:], mT[:, k, tt * 128:(tt + 1) * 128], wsl[so[h]][:, k, :], k == 0, k == 7) for k in range(8)]),
                         reads=[("ws", so[h])] + [("mT", k, ci) for k in range(8)], writes=[("ps", bk)])
                    S.op("dve", lambda e, bk=bk, h=h: e.tensor_tensor(out=tmpA, in0=ps[bk][:], in1=G1[:, h * 512:(h + 1) * 512], op=ALU.mult),
                         reads=[("ps", bk), ("G1", h)], writes=["tmpA"])
                    S.op("dve", lambda e, t=t, h=h: e.tensor_tensor(out=x1[:, t, h * 512:(h + 1) * 512], in0=x1[:, t, h * 512:(h + 1) * 512], in1=tmpA, op=ALU.add),
                         reads=[("x1", t), "tmpA"], writes=[("x1", t)])
                    if h == 1:
                        norm_stats(x1[:, t, :], ss2[:, t:t + 1], None, [("x1", t)], ("n2", t), finish=False)

        A2b = OV[:, 1536:2560]; B2b = OV[:, 2560:3584]; xn_b = OV[:, 3584:4608]
        S.alias([("A2b", 0), ("A2b", 1), ("B2b", 0), ("B2b", 1), "xn", "G2b"],
                ["tmpA", "tmpB", "tmpC", "rd0", "rd1", "qraw", ("qraw", 0), ("qraw", 1)] + [("P", 0, i, hh) for i in range(6) for hh in range(2)])
        expand(a2T[:, :], A2b, ["a2T"], "A2b")
        expand(mod2T[:, 8:16], B2b, ["mod2T"], "B2b")
        lg = MT[:, 0:1152].bitcast(F32).rearrange("p (t n) -> p t n", n=36)
        RKEYS = [("lg", t) for t in range(16)] + ["rt", "gmax", "gsh", "gm", "gsum", "m1v", "k1", "ml2", "m2v", "k2", "dd", "s1", "s2"] + [("ml", g) for g in range(4)]
        S.alias(RKEYS, [("mT", oc, ci) for oc in range(8) for ci in range(2)])
        xn_c = OV[:, 0:1024]
        S.alias(["xnc"], ["ngt"])
        norm_rstd(ss2, rstd2, [("ss", ("n2", t)) for t in range(16)], "rs2all")

        def n2_router(t):
            bk = nb()
            S.op("pe", mmgroup([(ps[bk][:, 0:36], HT[:, k, t * 128:(t + 1) * 128], wrt[:, k, :], k == 0, k == 7) for k in range(8)]),
                 reads=HTr([t]) + ["wrt"], writes=[("ps", bk)])
            return bk

        def n2_lgadd(t, bk):
            S.op("dve", lambda e, bk=bk, t=t: e.tensor_tensor(out=lg[:, t, :], in0=ps[bk][:, 0:36], in1=brtb[:], op=ALU.add),
                 reads=[("ps", bk), "brtb"], writes=[("lg", t)])

        prev = None
        for t in range(16):
            xb_, xk_ = (xn_b, "xn") if t % 2 == 0 else (xn_c, "xnc")
            norm_apply(x1[:, t, :], rstd2[:, t:t + 1], (A2b, [("A2b", 0), ("A2b", 1)]), (B2b, [("B2b", 0), ("B2b", 1)]),
                       xb_, xk_, t * 128, [("x1", t)], "rs2all", t)
            bk = n2_router(t)
            if prev is not None:
                n2_lgadd(*prev)
            prev = (t, bk)
        n2_lgadd(*prev)
        RT = MT[:, 1152:8192].bitcast(F32)
        r_gsh = RT[:, 0:64].rearrange("p (t n) -> p t n", n=4)
        r_gm = RT[:, 64:128].rearrange("p (t n) -> p t n", n=4)
        r_ml = RT[:, 128:640].rearrange("p (t n) -> p t n", n=32)
        r_k1 = RT[:, 640:1152].rearrange("p (t n) -> p t n", n=32)
        r_ml2 = RT[:, 1152:1664].rearrange("p (t n) -> p t n", n=32)
        r_k2 = RT[:, 1664:2176].rearrange("p (t n) -> p t n", n=32)
        r_cmb = RT[:, 2176:2688].rearrange("p (t n) -> p t n", n=32)
        sm = RT[:, 2688:2944].rearrange("p (a t) -> p a t", t=16)
        combb = SB("combb", [128, 16, 32], BF16)
        lgk = [("lg", t) for t in range(16)]

        def V_(eng, fn, rd, wr):
            S.op(eng, fn, reads=rd, writes=wr)

        lgg = lg[:, :, 0:4]; lge = lg[:, :, 4:36]
        V_("dve", lambda e: e.tensor_reduce(out=sm[:, 0, :], in_=lgg, axis=AX.X, op=ALU.max), lgk, ["gmax"])
        V_("dve", lambda e: e.tensor_tensor(out=r_gsh, in0=lgg, in1=sm[:, 0, :].unsqueeze(2).to_broadcast([128, 16, 4]), op=ALU.subtract), lgk + ["gmax"], ["gsh"])
        V_("dve", lambda e: e.tensor_tensor(out=r_gm, in0=lgg, in1=sm[:, 0, :].unsqueeze(2).to_broadcast([128, 16, 4]), op=ALU.is_equal), lgk + ["gmax"], ["gm"])
        V_("act", lambda e: e.activation(out=r_gsh, in_=r_gsh, func=AF.Exp), ["gsh"], ["gsh"])
        V_("dve", lambda e: e.tensor_reduce(out=sm[:, 1, :], in_=r_gsh, axis=AX.X, op=ALU.add), ["gsh"], ["gsum"])
        V_("dve", lambda e: e.reciprocal(out=sm[:, 1, :], in_=sm[:, 1, :]), ["gsum"], ["gsum"])
        V_("dve", lambda e: e.tensor_scalar(out=r_gm, in0=r_gm, scalar1=BIG, scalar2=-BIG, op0=ALU.mult, op1=ALU.add), ["gm"], ["gm"])
        for g in range(4):
            V_("dve", lambda e, g=g: e.tensor_tensor(out=r_ml[:, :, g * 8:(g + 1) * 8], in0=lge[:, :, g * 8:(g + 1) * 8],
                                                     in1=r_gm[:, :, g:g + 1].to_broadcast([128, 16, 8]), op=ALU.add), lgk + ["gm"], [("ml", g)])
        mlk = [("ml", g) for g in range(4)]
        V_("dve", lambda e: e.tensor_reduce(out=sm[:, 2, :], in_=r_ml, axis=AX.X, op=ALU.max), mlk, ["m1v"])
        V_("dve", lambda e: e.tensor_tensor(out=r_k1, in0=r_ml, in1=sm[:, 2, :].unsqueeze(2).to_broadcast([128, 16, 32]), op=ALU.is_equal), mlk + ["m1v"], ["k1"])
        V_("dve", lambda e: e.scalar_tensor_tensor(out=r_ml2, in0=r_k1, scalar=-BIG, in1=r_ml, op0=ALU.mult, op1=ALU.add), mlk + ["k1"], ["ml2"])
        V_("dve", lambda e: e.tensor_reduce(out=sm[:, 3, :], in_=r_ml2, axis=AX.X, op=ALU.max), ["ml2"], ["m2v"])
        V_("dve", lambda e: e.tensor_tensor(out=r_k2, in0=r_ml2, in1=sm[:, 3, :].unsqueeze(2).to_broadcast([128, 16, 32]), op=ALU.is_equal), ["ml2", "m2v"], ["k2"])
        V_("dve", lambda e: e.tensor_tensor(out=sm[:, 4, :], in0=sm[:, 3, :], in1=sm[:, 2, :], op=ALU.subtract), ["m1v", "m2v"], ["dd"])
        V_("act", lambda e: e.activation(out=sm[:, 4, :], in_=sm[:, 4, :], func=AF.Exp), ["dd"], ["dd"])
        V_("dve", lambda e: e.tensor_scalar(out=sm[:, 5, :], in0=sm[:, 4, :], scalar1=1.0, scalar2=None, op0=ALU.add), ["dd"], ["s1"])
        V_("dve", lambda e: e.reciprocal(out=sm[:, 5, :], in_=sm[:, 5, :]), ["s1"], ["s1"])
        V_("dve", lambda e: e.tensor_tensor(out=sm[:, 5, :], in0=sm[:, 5, :], in1=sm[:, 1, :], op=ALU.mult), ["s1", "gsum"], ["s1"])
        V_("dve", lambda e: e.tensor_tensor(out=sm[:, 6, :], in0=sm[:, 5, :], in1=sm[:, 4, :], op=ALU.mult), ["s1", "dd"], ["s2"])
        V_("dve", lambda e: e.tensor_tensor(out=r_k1, in0=r_k1, in1=sm[:, 5, :].unsqueeze(2).to_broadcast([128, 16, 32]), op=ALU.mult), ["k1", "s1", "ml2"], ["k1"])
        V_("dve", lambda e: e.tensor_tensor(out=r_k2, in0=r_k2, in1=sm[:, 6, :].unsqueeze(2).to_broadcast([128, 16, 32]), op=ALU.mult), ["k2", "s2"], ["k2"])
        V_("dve", lambda e: e.tensor_tensor(out=combb[:], in0=r_k1, in1=r_k2, op=ALU.add), ["k1", "k2"], ["combb"])

        G2b = G2bt[:, :]
        er_old = [("ws", i) for i in range(3)] + [("qT", j, c) for j in range(4) for c in range(4)] + \
                 [("oT", j, c) for j in range(4) for c in range(2)] + [("oTp", 2 * kvh, ci, nbk, hh) for kvh in range(2) for ci in range(2)
                                                                      for nbk in range(ci * 4, ci * 4 + 4) for hh in range(2)]
        S.alias([("wu", s) for s in range(4)] + [("wd", s) for s in range(4)], er_old)
        wu = [ER[:, s * 6144:s * 6144 + 4096].rearrange("p (k n) -> p k n", n=512) for s in range(4)]
        wd = [ER[:, s * 6144 + 4096:(s + 1) * 6144].rearrange("p (f n) -> p f n", n=1024) for s in range(4)]
        MTb = MT
        cbs = [MTb[:, i * 1024:(i + 1) * 1024].rearrange("p (e n) -> p e n", n=512) for i in range(2)]
        sab = [MTb[:, 2048 + i * 512:2048 + (i + 1) * 512] for i in range(2)]
        t2b = [MTb[:, 3072 + i * 512:3072 + (i + 1) * 512] for i in range(2)]
        S.alias([("cbs", 0), ("cbs", 1), ("cbs2", 0), ("cbs2", 1), ("sab", 0), ("sab", 1), ("t2b", 0), ("t2b", 1)] + [("actT", bi_, el, j) for bi_ in range(2) for el in range(2) for j in range(2)],
                RKEYS)

        def load_expert(e_, fold=True):
            s = e_ % 4
            gdma(wu[s], wup_d[e_].rearrange("(k p) n -> p k n", p=128), [], [("wu", s)], ("wu", s))
            gdma(wd[s], wdn_d[e_].rearrange("(f p) n -> p f n", p=128), [], [("wd", s)], ("wd", s))
            if fold:
                fold_expert(e_)

        def fold_expert(e_):
            s = e_ % 4
            S.op("dve", lambda e, s=s: e.tensor_tensor(out=wd[s], in0=wd[s], in1=G2b.unsqueeze(1).to_broadcast([128, 2, 1024]), op=ALU.mult),
                 reads=[("wd", s), ("G2bt", 0), ("G2bt", 1)], writes=[("wd", s)])

        UPB = [(0, 1), (2, 3)]
        ACB = [(4, 5), (6, 7)]
        up_i = [0]; ac_i = [0]; sa_i = [0]
        actTs = [MTb[:, 4096 + i * 2048:4096 + (i + 1) * 2048].rearrange("p (e f n) -> p e f n", e=2, f=2) for i in range(2)]
        for e_ in range(4):
            load_expert(e_)

        def up_stage(sidx):
            g, c = divmod(sidx, 4)
            tl = list(range(c * 4, c * 4 + 4))
            bi = sidx % 2
            actT = actTs[bi]
            for el in range(2):
                ex = 2 * g + el
                s_ = ex % 4
                for j in range(2):
                    ub = UPB[up_i[0]]; up_i[0] ^= 1
                    S.op("pe", mmgroup([(ps[ub[0]][:], wu[s_][:, k, j * 128:(j + 1) * 128], HT[:, k, c * 512:(c + 1) * 512], k == 0, k == 7) for k in range(8)]),
                         reads=[("wu", s_)] + HTr(tl), writes=[("ps", ub[0])])
                    S.op("pe", mmgroup([(ps[ub[1]][:], wu[s_][:, k, (2 + j) * 128:(3 + j) * 128], HT[:, k, c * 512:(c + 1) * 512], k == 0, k == 7) for k in range(8)]),
                         reads=[("wu", s_)] + HTr(tl), writes=[("ps", ub[1])])
                    si_ = sa_i[0]; sa_i[0] ^= 1
                    S.op("act", lambda e, ub=ub, si_=si_: e.activation(out=sab[si_], in_=ps[ub[0]][:], func=AF.Silu), reads=[("ps", ub[0])], writes=[("sab", si_)])
                    S.op("dve", lambda e, ub=ub, si_=si_: e.tensor_tensor(out=t2b[si_], in0=ps[ub[1]][:], in1=sab[si_], op=ALU.mult),
                         reads=[("ps", ub[1]), ("sab", si_)], writes=[("t2b", si_)])
                    S.op("dve", lambda e, si_=si_, el=el, j=j, bi=bi, actT=actT: e.tensor_tensor(out=actT[:, el, j, :], in0=t2b[si_], in1=cbs[bi][:, el, :], op=ALU.mult),
                         reads=[("t2b", si_), ("cbs", bi), ("cbs2", bi)], writes=[("actT", bi, el, j)])

        def cb_stage(sidx):
            g, c = divmod(sidx, 4)
            bi = sidx % 2
            ub = UPB[up_i[0]]; up_i[0] ^= 1
            for el in range(2):
                ex = 2 * g + el
                S.op("pe", mmgroup([(ps[ub[el]][:, tt * 128:(tt + 1) * 128], combb[:, c * 4 + tt, ex:ex + 1].to_broadcast([128, 128]), identb[:], True, True)
                                    for tt in range(4)]), reads=["combb", "identb"], writes=[("ps", ub[el])])
                S.op("act", lambda e, ub=ub, bi=bi, el=el: e.activation(out=cbs[bi][:, el, :], in_=ps[ub[el]][:], func=AF.Copy),
                     reads=[("ps", ub[el])], writes=[("cbs", bi)] if el == 0 else [("cbs2", bi)])

        def down_stage(sidx):
            g, c = divmod(sidx, 4)
            bi = sidx % 2
            actT = actTs[bi]
            for h in range(2):
                for tp in range(2):
                    ab = ACB[ac_i[0]]; ac_i[0] ^= 1
                    for ti in range(2):
                        tt = tp * 2 + ti
                        lst = []
                        for el in range(2):
                            s_ = (2 * g + el) % 4
                            for f in range(2):
                                lst.append((ps[ab[ti]][:], actT[:, el, f, tt * 128:(tt + 1) * 128], wd[s_][:, f, h * 512:(h + 1) * 512],
                                            el == 0 and f == 0, el == 1 and f == 1))
                        S.op("pe", mmgroup(lst), reads=[("wd", (2 * g) % 4), ("wd", (2 * g + 1) % 4)] + [("actT", bi, el, f) for el in range(2) for f in range(2)],
                             writes=[("ps", ab[ti])])
                        t = c * 4 + tt
                        S.op("dve", lambda e, b=ab[ti], t=t, h=h: e.tensor_tensor(out=x1[:, t, h * 512:(h + 1) * 512], in0=ps[b][:],
                                                                                 in1=x1[:, t, h * 512:(h + 1) * 512], op=ALU.add),
                             reads=[("ps", ab[ti]), ("x1", t)], writes=[("x1", t)])
            if c == 3:
                for el in range(2):
                    nx = 2 * g + 4 + el
                    if nx < NE:
                        load_expert(nx, fold=False)
            if c == 1 and g + 1 >= 2:
                for el in range(2):
                    nx = 2 * (g + 1) + el
                    if nx < NE:
                        fold_expert(nx)

        def final_tiles(tlist):
            for t in tlist:
                S.op("act", lambda e, t=t: e.activation(out=junk, in_=x1[:, t, :], func=AF.Square, accum_out=ss3[:, t:t + 1]),
                     reads=[("x1", t)], writes=["junk", ("ss3", t)])
                S.op("dve", lambda e, t=t: e.tensor_scalar(out=rstd3[:, t:t + 1], in0=ss3[:, t:t + 1], scalar1=1.0 / D, scalar2=1e-6, op0=ALU.mult, op1=ALU.add),
                     reads=[("ss3", t)], writes=[("rs3", t)])
                S.op("act", lambda e, t=t: e.activation(out=rstd3[:, t:t + 1], in_=rstd3[:, t:t + 1], func=AF.Sqrt), reads=[("rs3", t)], writes=[("rs3", t)])
                S.op("dve", lambda e, t=t: e.reciprocal(out=rstd3[:, t:t + 1], in_=rstd3[:, t:t + 1]), reads=[("rs3", t)], writes=[("rs3", t)])
                S.op("dve", lambda e, t=t: e.scalar_tensor_tensor(out=x1[:, t, :], in0=x1[:, t, :], scalar=rstd3[:, t:t + 1], in1=ngt, op0=ALU.mult, op1=ALU.mult),
                     reads=[("x1", t), ("rs3", t), "FGa"], writes=[("x1", t)])
                sdma(out_d[t * 128:(t + 1) * 128, :], x1[:, t, :], [("x1", t)], [("out", t)], ("out", t % 4))

        S.alias(["FGa"], ["ngt", "xnc"])
        sdma(ngt, fg_d.partition_broadcast(128), [], ["FGa"], "c13")
        NS = (NE // 2) * 4
        cb_stage(0)
        up_stage(0)
        cb_stage(1)
        for sidx in range(1, NS):
            up_stage(sidx)
            if sidx + 1 < NS:
                cb_stage(sidx + 1)
            down_stage(sidx - 1)
            if sidx - 1 >= NS - 4:
                c_ = (sidx - 1) % 4
                final_tiles(range(c_ * 4, c_ * 4 + 4))
        down_stage(NS - 1)
        final_tiles(range(12, 16))

        S.emit()
        build_program.stats = S.stats
    return nc


def _host_consts():
    n_freq = 16
    inv_freq = (np.float32(10000.0) ** (-np.arange(n_freq, dtype=np.float32) / np.float32(n_freq))).astype(np.float32)
    tok = np.arange(L)
    row = (tok // 64).astype(np.float32)
    col = (tok % 64).astype(np.float32)
    ang = np.concatenate([row[:, None] * inv_freq[None, :], col[:, None] * inv_freq[None, :]], axis=1).astype(np.float32)
    cos = np.cos(ang).astype(np.float32); sin = np.sin(ang).astype(np.float32)
    p = np.arange(128)
    c2 = cos[:, p % 32].T.copy()
    sgn = np.where((p % 64) < 32, -1.0, 1.0).astype(np.float32)
    s2 = (sin[:, p % 32].T * sgn[:, None]).astype(np.float32)
    ident = np.eye(128, dtype=np.float32)
    j = np.arange(128)[:, None]; i = np.arange(128)[None, :]
    m_prev = (j >= i).astype(np.float32)
    m_next = (j <= i).astype(np.float32)
    m01 = np.concatenate([m_prev, m_next], axis=1)
    m_ = np.arange(128)
    swm = (m_ // 64) * 64 + ((m_ % 64) + 32) % 64
    pm = np.zeros((128, 128), dtype=np.float32)
    pm[swm, m_] = 1.0
    return np.ascontiguousarray(c2), np.ascontiguousarray(s2), ident, np.ascontiguousarray(m01), pm


def _win_perm():
    b0, cg0, xin0, q0, k0, v0, ga0, gb0 = 0, 512, 1024, 1536, 2048, 2176, 2304, 3328

    def sw(base, nheads):
        idx = []
        for h in range(nheads):
            for i in range(64):
                idx.append(base + h * 64 + (i + 32) % 64)
        return idx

    cols = []
    kh = [list(range(k0 + h * 64, k0 + (h + 1) * 64)) for h in range(2)]
    ksw = sw(k0, 2)
    khs = [ksw[h * 64:(h + 1) * 64] for h in range(2)]
    cols += kh[0] + kh[0] + khs[0] + khs[0] + kh[1] + kh[1] + khs[1] + khs[1]
    cols += list(range(v0, v0 + 128))
    for jj in range(4):
        cols += list(range(cg0 + jj * 128, cg0 + (jj + 1) * 128)) + list(range(xin0 + jj * 128, xin0 + (jj + 1) * 128))
    cols += list(range(b0, b0 + 512))
    cols += list(range(ga0, ga0 + 1024))
    qsw = sw(q0, 8)
    for jj in range(4):
        cols += list(range(q0 + jj * 128, q0 + (jj + 1) * 128)) + qsw[jj * 128:(jj + 1) * 128]
    cols += list(range(gb0, gb0 + 1024))
    assert len(cols) == NCOLS
    return np.array(cols, dtype=np.int64)


_NC_CACHE = {}


def kernel(x, c, ctx, c_ctx, w_ada, b_ada, norm1_g, w_in, w_conv, b_conv, w_a, w_b, sink, w_o,
           norm2_g, w_group, b_group, w_router, b_router, w_up, w_down, final_g):
    f = lambda a: np.ascontiguousarray(np.asarray(a, dtype=np.float32))
    x = f(x); c = f(c); ctx = f(ctx); c_ctx = f(c_ctx)
    if "nc" not in _NC_CACHE:
        _NC_CACHE["nc"] = build_program()
    nc = _NC_CACHE["nc"]
    c2, s2, ident, m01, pm = _host_consts()
    w_in0 = f(w_in)[0]
    win = np.ascontiguousarray(w_in0[:, _win_perm()])
    b_ada0 = f(b_ada)[0]
    shared = dict(
        wada=f(w_ada)[0], bada1=np.ascontiguousarray(b_ada0[:2048]),
        bada2=np.ascontiguousarray(b_ada0[2048:].reshape(32, 128).T),
        ng1=f(norm1_g)[0], ng2=np.ascontiguousarray(f(norm2_g)[0].reshape(8, 128).T), fg=f(final_g),
        win=win,
        wc=np.ascontiguousarray(f(w_conv)[0].T.reshape(4, 128, 3).transpose(1, 0, 2).reshape(128, 12)),
        bc=np.ascontiguousarray(f(b_conv)[0].reshape(4, 128).T),
        wa=f(w_a)[0], wb=f(w_b)[0], wo=f(w_o)[0],
        sinkp=np.ascontiguousarray(f(sink)[0][[0, 2, 1, 3, 4, 6, 5, 7]].reshape(1, 8)),
        wrt=np.ascontiguousarray(np.concatenate([f(w_group)[0], f(w_router)[0]], axis=1)),
        brt=np.ascontiguousarray(np.concatenate([f(b_group)[0], f(b_router)[0]], axis=0)),
        wup=f(w_up)[0], wdn=f(w_down)[0],
        c2=c2, s2=s2, ident=ident, m01=m01, pm=pm,
    )
    cct = np.ascontiguousarray(c_ctx.reshape(8, 128).T)
    in_maps = []
    for b in range(8):
        m = dict(shared)
        m["x"] = x[b]
        m["ctx"] = ctx[b]
        m["cc"] = np.ascontiguousarray(np.concatenate([c[b].reshape(8, 128).T, cct], axis=1))
        in_maps.append(m)
    res = run_bass_kernel_spmd(nc, in_maps, core_ids=list(range(8)))
    return np.stack([np.asarray(res.results[b]["out"], dtype=np.float32) for b in range(8)], axis=0)
```

```python
from contextlib import ExitStack
import numpy as np
import concourse.bass as bass
import concourse.mybir as mybir
from concourse.bass_utils import run_bass_kernel_spmd

F32 = mybir.dt.float32
BF16 = mybir.dt.bfloat16
ALU = mybir.AluOpType
AF = mybir.ActivationFunctionType
AX = mybir.AxisListType

L = 2048
CTX = 256
D = 1024
NE = 32
BIG = 1.0e4
NCOLS = 5248
O_KV, O_V, O_CX0, O_CX1, O_B, O_GA0, O_GA1, O_Q0, O_Q1, O_GB0, O_GB1 = (
    0, 512, 640, 1152, 1664, 2176, 2688, 3200, 3712, 4224, 4736)


class Sched:
    ENGS = ("pe", "act", "dve", "pool", "sp")

    def __init__(self, nc, stack):
        self.nc = nc
        self.stack = stack
        self.ops = []
        self.res = {}
        self.esem = {e: stack.enter_context(nc.semaphore("sem_" + e)) for e in self.ENGS}
        self.dsems = {}

    def _dsem(self, key):
        if key not in self.dsems:
            self.dsems[key] = self.stack.enter_context(self.nc.semaphore("dsem_%d" % len(self.dsems)))
        return self.dsems[key]

    def op(self, eng, fn, reads=(), writes=(), dma=None):
        oid = len(self.ops)
        deps = set()
        for r in reads:
            st = self.res.get(r)
            if st is not None and st[0] is not None:
                deps.add(st[0])
        for w in writes:
            st = self.res.get(w)
            if st is not None:
                if st[0] is not None:
                    deps.add(st[0])
                deps.update(st[1])
        self.ops.append(dict(eng=eng, fn=fn, deps=deps, dma=dma, sig=False))
        for r in reads:
            self.res.setdefault(r, [None, []])[1].append(oid)
        for w in writes:
            self.res[w] = [oid, []]
        return oid

    def alias(self, new_keys, old_keys):
        pend = []
        for k in old_keys:
            st = self.res.pop(k, None)
            if st is not None:
                if st[0] is not None:
                    pend.append(st[0])
                pend.extend(st[1])
        pend = sorted(set(pend))
        for k in new_keys:
            st = self.res.setdefault(k, [None, []])
            st[1].extend(pend)

    def emit(self):
        ops = self.ops
        for o in ops:
            for d in o["deps"]:
                do = ops[d]
                if do["eng"] == "pe" and o["eng"] == "pe" and do["dma"] is None and o["dma"] is None:
                    continue
                do["sig"] = True
        for o in ops:
            if o["dma"] is not None:
                o["sig"] = True
        cnt = {e: 0 for e in self.ENGS}
        dcnt = {}
        for o in ops:
            if not o["sig"]:
                o["tick"] = None
                continue
            if o["dma"] is not None:
                k = ("d", o["dma"])
                dcnt[k] = dcnt.get(k, 0) + 16
                o["tick"] = (k, dcnt[k])
            else:
                cnt[o["eng"]] += 1
                o["tick"] = (("e", o["eng"]), cnt[o["eng"]])
        known = {e: {} for e in self.ENGS}
        prog = {e: [] for e in self.ENGS}
        nwaits = 0
        for o in ops:
            e = o["eng"]
            need = {}
            for d in o["deps"]:
                do = ops[d]
                if do["tick"] is None:
                    continue
                k, v = do["tick"]
                if need.get(k, 0) < v:
                    need[k] = v
            waits = []
            for k, v in need.items():
                if known[e].get(k, 0) >= v:
                    continue
                known[e][k] = v
                waits.append((k, v))
            nwaits += len(waits)
            prog[e].append((waits, o))
        final = [(k, v) for k, v in dcnt.items()]
        for e in self.ENGS:
            if e != "sp" and cnt[e] > 0:
                final.append((("e", e), cnt[e]))
        self.stats = dict(n_ops=len(ops), n_waits=nwaits, cnt=cnt, n_dsem=len(dcnt))

        def semof(k):
            return self.esem[k[1]] if k[0] == "e" else self._dsem(k[1])

        for k in dcnt:
            semof(k)
        nc = self.nc
        handles = dict(pe="tensor", act="scalar", dve="vector", pool="gpsimd", sp="sync")
        with nc.Block() as block:
            for e in self.ENGS:
                lst = prog[e]
                fin = final if e == "sp" else []
                if not lst and not fin:
                    continue

                def body(eng, lst=lst, fin=fin):
                    for waits, o in lst:
                        for k, v in waits:
                            eng.wait_ge(semof(k), v)
                        ins = o["fn"](eng)
                        if o["tick"] is not None:
                            k, v = o["tick"]
                            ins.then_inc(semof(k), 16 if k[0] == "d" else 1)
                    for k, v in fin:
                        eng.wait_ge(semof(k), v)

                getattr(block, handles[e])(body)


def build_program():
    nc = bass.Bass("TRN2", target_bir_lowering=False)

    def DIN(n, s):
        return nc.dram_tensor(n, list(s), F32, kind="ExternalInput").ap()

    x_d = DIN("x", [L, D]); ctx_d = DIN("ctx", [CTX, D]); cc_d = DIN("cc", [128, 16])
    wada_d = DIN("wada", [D, 6 * D]); bada1_d = DIN("bada1", [2048]); bada2_d = DIN("bada2", [128, 32])
    ng1_d = DIN("ng1", [D]); ng2_d = DIN("ng2", [128, 8]); fg_d = DIN("fg", [D])
    win_d = DIN("win", [D, NCOLS]); wc_d = DIN("wc", [128, 12]); bc_d = DIN("bc", [128, 4])
    wa_d = DIN("wa", [512, D]); wb_d = DIN("wb", [512, D]); wo_d = DIN("wo", [D, D])
    sink_d = DIN("sinkp", [1, 8]); wrt_d = DIN("wrt", [D, 36]); brt_d = DIN("brt", [36])
    wup_d = DIN("wup", [NE, D, 512]); wdn_d = DIN("wdn", [NE, 256, D])
    c2_d = DIN("c2", [128, L]); s2_d = DIN("s2", [128, L]); id_d = DIN("ident", [128, 128])
    m01_d = DIN("m01", [128, 256])
    pm_d = DIN("pm", [128, 128])
    out_d = nc.dram_tensor("out", [L, D], F32, kind="ExternalOutput").ap()

    with ExitStack() as st:
        def SB(n, s, dt):
            return st.enter_context(nc.sbuf_tensor("s_" + n, list(s), dt))

        XR = SB("XR", [128, 16384], F32)
        ER = SB("ER", [128, 24576], BF16)
        HT = SB("HT", [128, 8, L + CTX], BF16)
        MT = SB("MT", [128, 8192], BF16)
        OV = SB("OV", [128, 6144], F32)
        identf = SB("identf", [128, 128], F32)
        identb = SB("identb", [128, 128], BF16)
        pmb = SB("pmb", [128, 128], BF16)
        m01 = SB("m01", [128, 256], BF16)
        esb = SB("esb", [128, 8], F32)
        otm = [SB("otm%d" % i, [128, 256], BF16) for i in range(2)]
        dsm = SB("dsm", [128, 8], F32)
        hal = SB("hal", [128, 2], F32)
        wcs = SB("wcs", [128, 12], F32); bcs = SB("bcs", [128, 4], F32)
        ccs = SB("ccs", [128, 16], F32)
        scf = SB("scf", [128, 16], F32)
        scb1 = SB("scb1", [128, 16], BF16)
        mod2T = SB("mod2T", [128, 32], F32)
        b2T = SB("b2T", [128, 32], F32)
        ng2T = SB("ng2T", [128, 8], F32)
        a2T = SB("a2T", [128, 8], F32)
        stat = SB("stat", [128, 128], F32)
        wrt = SB("wrt", [128, 8, 36], BF16)
        brtb = SB("brtb", [128, 36], F32)
        ps = [st.enter_context(nc.psum_tensor("ps%d" % i, [128, 512], F32)) for i in range(8)]

        S = Sched(nc, st)
        _bank = [0]

        _m2lock = [True]

        def nb():
            b = _bank[0]
            if _m2lock[0] and b == 7:
                b = 0
            _bank[0] = (b + 1) % 8
            return b

        psm = ps[7]

        x1 = XR[:, :].rearrange("p (t n) -> p t n", n=D)
        mod1 = XR[:, 0:4096].rearrange("p (a n) -> p a n", n=D)
        xs = [XR[:, 4096 + i * 1024: 4096 + (i + 1) * 1024] for i in range(2)]
        xn_a = XR[:, 6144:7168]
        scb = XR[:, 7168:8192].bitcast(BF16).rearrange("p (k m) -> p k m", m=128)
        kT = XR[:, 8192:10496].bitcast(BF16).rearrange("p (a n) -> p a n", a=2)
        Vx = XR[:, 10496:13376].bitcast(BF16).rearrange("p (t n) -> p t n", n=320)
        C2 = XR[:, 13376:14400].bitcast(BF16)
        S2 = XR[:, 14400:15424].bitcast(BF16)
        wsl = [ER[:, i * 4096:(i + 1) * 4096].rearrange("p (k n) -> p k n", n=512) for i in range(3)]
        ucv = ER[:, 12288:18440].rearrange("p (j n) -> p j n", j=4)
        qT = ER[:, 12288:16384].rearrange("p (j n) -> p j n", j=4)
        cvT = ER[:, 18440:22536].rearrange("p (j n) -> p j n", j=4)
        oT = cvT
        mT = MT[:, :].rearrange("p (k n) -> p k n", k=8)
        ngt = OV[:, 0:1024]
        junk = OV[:, 1024:1536].bitcast(BF16)
        tmpA = OV[:, 1536:2048]; tmpB = OV[:, 2048:2560]; tmpC = OV[:, 2560:3072]
        Pb = OV[:, 3072:4608].bitcast(BF16).rearrange("p (t n) -> p t n", n=512)
        rdt = OV[:, 4608:5120]
        qraw = OV[:, 4608:4864].bitcast(BF16)
        qraws = [qraw, OV[:, 4864:5120].bitcast(BF16)]
        Pbs = [Pb, OV[:, 0:1536].bitcast(BF16).rearrange("p (t n) -> p t n", n=512)]
        _pb = [0]
        _ot = [0]
        G1 = OV[:, 5120:6144]

        ss = stat[:, 0:18]; rstd = stat[:, 18:36]; ss2 = stat[:, 36:52]; rstd2 = stat[:, 52:68]
        ss3 = stat[:, 68:84]; rstd3 = stat[:, 84:100]

        def gdma(out, in_, reads, writes, key):
            S.op("pool", lambda e, out=out, in_=in_: e.dma_start(out=out, in_=in_), reads=reads, writes=writes, dma=key)

        def sdma(out, in_, reads, writes, key):
            S.op("sp", lambda e, out=out, in_=in_: e.dma_start(out=out, in_=in_), reads=reads, writes=writes, dma=key)

        _ws = [0]

        def load_slot(src, nk=8, ncols=512, wide=False):
            i = _ws[0]
            _ws[0] = (i + 1) % 3
            if wide:
                dst = ER[:, i * 4096:(i + 1) * 4096].rearrange("p (k n) -> p k n", n=1024)
            else:
                dst = wsl[i][:, 0:nk, 0:ncols]
            gdma(dst, src, [], [("ws", i)], ("ws", i))
            return i

        def win_src(c0, ncols=512):
            return win_d[:, c0:c0 + ncols].rearrange("(k p) n -> p k n", p=128)

        def mmgroup(lst):
            def fn(e, lst=lst):
                ins = None
                for (o, l, r, s0, s1) in lst:
                    ins = e.matmul(o, lhsT=l, rhs=r, start=s0, stop=s1)
                return ins
            return fn

        sdma(identf[:], id_d, [], ["identf"], "c0")
        sdma(wcs[:], wc_d, [], ["wcs"], "c5")
        sdma(bcs[:], bc_d, [], ["bcs"], "c6")
        sdma(ccs[:], cc_d, [], ["ccs"], "c7")
        sdma(b2T[:], bada2_d, [], ["b2T"], "c8")
        sdma(ng2T[:], ng2_d, [], ["ng2T"], "c9")
        sdma(esb[:], sink_d.rearrange("a b -> (a b)").partition_broadcast(128), [], ["esb"], "c10")
        sdma(brtb[:], brt_d.partition_broadcast(128), [], ["brtb"], "c11")
        sdma(ngt, ng1_d.partition_broadcast(128), [], ["ngt"], "c13")
        sdma(mod1[:, 0:2, :].rearrange("p a n -> p (a n)"), bada1_d.partition_broadcast(128), [], ["mod1b"], "c14")
        sdma(mod1[:, 2:4, :].rearrange("p a n -> p (a n)"), bada1_d.partition_broadcast(128), [], ["mod1c"], "c15")

        Vx5 = Vx.rearrange("p t (b n) -> p t b n", n=64)
        for bi in (0, 2, 4):
            S.op("dve", lambda e, bi=bi: e.memset(Vx5[:, :, bi, :], 1.0), writes=[("Vones", bi)])
        S.op("act", lambda e: e.activation(out=esb[:], in_=esb[:], func=AF.Exp), reads=["esb"], writes=["esb"])
        S.op("act", lambda e: e.activation(out=scf[:], in_=ccs[:], func=AF.Silu), reads=["ccs"], writes=["scf"])
        S.op("dve", lambda e: e.tensor_copy(out=scb, in_=scf[:, :].unsqueeze(2).to_broadcast([128, 16, 128])),
             reads=["scf"], writes=["scb"])
        S.op("dve", lambda e: e.tensor_copy(out=scb1[:], in_=scf[:]), reads=["scf"], writes=["scb1"])

        for n in range(4):
            si = load_slot(wada_d[:, n * 512:(n + 1) * 512].rearrange("(k p) n -> p k n", p=128))
            b0 = nb(); b1 = nb()
            S.op("pe", mmgroup([(ps[b0][:], scb[:, k, :], wsl[si][:, k, :], k == 0, k == 7) for k in range(8)]),
                 reads=["scb", ("ws", si)], writes=[("ps", b0)])
            S.op("pe", mmgroup([(ps[b1][:], scb[:, 8 + k, :], wsl[si][:, k, :], k == 0, k == 7) for k in range(8)]),
                 reads=["scb", ("ws", si)], writes=[("ps", b1)])
            a, off = divmod(n, 2)
            dst0 = mod1[:, a, off * 512:(off + 1) * 512]
            dst1 = mod1[:, 2 + a, off * 512:(off + 1) * 512]
            S.op("dve", lambda e, d=dst0, p=ps[b0]: e.tensor_tensor(out=d, in0=p[:], in1=d, op=ALU.add),
                 reads=[("ps", b0), "mod1b"], writes=[("m1", 0, n)])
            S.op("dve", lambda e, d=dst1, p=ps[b1]: e.tensor_tensor(out=d, in0=p[:], in1=d, op=ALU.add),
                 reads=[("ps", b1), "mod1c"], writes=[("m1", 1, n)])
        gdma(identb[:], id_d, [], ["identb"], "c1")
        gdma(pmb[:], pm_d, [], ["pmb"], "c16")
        gdma(m01[:], m01_d, [], ["m01"], "c2")
        gdma(C2, c2_d, [], ["C2"], "c3")
        gdma(S2, s2_d, [], ["S2"], "c4")
        gdma(wrt[:], wrt_d.rearrange("(k p) n -> p k n", p=128), [], ["wrt"], "c12")
        for c in range(2):
            S.op("dve", lambda e, c=c: e.scalar_tensor_tensor(out=mod1[:, 2 * c + 1, :], in0=mod1[:, 2 * c + 1, :], scalar=1.0,
                                                              in1=ngt, op0=ALU.add, op1=ALU.mult),
                 reads=[("m1", c, 2), ("m1", c, 3), "ngt"], writes=[("A1", c)])

        def norm_stats(src, ssc, rsc, rd_keys, key, finish=True):
            S.op("act", lambda e: e.activation(out=junk, in_=src, func=AF.Square, accum_out=ssc),
                 reads=rd_keys, writes=["junk", ("ss", key)])
            if finish:
                norm_rstd(ssc, rsc, [("ss", key)], ("rs", key))

        def norm_rstd(ssc, rsc, rd, wkey):
            S.op("dve", lambda e: e.tensor_scalar(out=rsc, in0=ssc, scalar1=1.0 / D, scalar2=1e-6, op0=ALU.mult, op1=ALU.add),
                 reads=rd, writes=[wkey])
            S.op("act", lambda e: e.activation(out=rsc, in_=rsc, func=AF.Sqrt), reads=[wkey], writes=[wkey])
            S.op("dve", lambda e: e.reciprocal(out=rsc, in_=rsc), reads=[wkey], writes=[wkey])

        def norm_apply(src, rsc, Abc, Bbc, xn, xnkey, col0, rd_keys, rskey, htkey, defer=False):
            deferred = []
            S.op("dve", lambda e: e.scalar_tensor_tensor(out=xn, in0=src, scalar=rsc, in1=Abc[0], op0=ALU.mult, op1=ALU.mult),
                 reads=list(rd_keys) + [rskey] + Abc[1], writes=[xnkey])
            S.op("dve", lambda e: e.tensor_tensor(out=xn, in0=xn, in1=Bbc[0], op=ALU.add), reads=[xnkey] + Bbc[1], writes=[xnkey])
            ba = nb(); bb_ = nb()
            for half, bk in ((0, ba), (1, bb_)):
                def fn(e, half=half, bk=bk):
                    ins = None
                    for kk in range(4):
                        k = half * 4 + kk
                        ins = e.transpose(ps[bk][:, kk * 128:(kk + 1) * 128], xn[:, k * 128:(k + 1) * 128], identf[:])
                    return ins
                S.op("pe", fn, reads=[xnkey, "identf"], writes=[("ps", bk)])

                def cp(half=half, bk=bk):
                    S.op("act", lambda e, half=half, bk=bk: e.activation(
                        out=HT[:, half * 4:(half + 1) * 4, col0:col0 + 128],
                        in_=ps[bk][:, :].rearrange("p (k n) -> p k n", n=128), func=AF.Copy),
                        reads=[("ps", bk)], writes=[("HT", htkey, half)])
                if defer:
                    deferred.append(cp)
                else:
                    cp()
            return deferred

        xs4 = [xs[0], xs[1], XR[:, 7168:8192], XR[:, 8192:9216]]
        xn_s2 = XR[:, 9216:10240]
        S.alias([("xs", 2)], ["scb"])

        def s0_dma(t):
            sl = t % 4
            src_d = x_d[t * 128:(t + 1) * 128, :] if t < 16 else ctx_d[(t - 16) * 128:(t - 15) * 128, :]
            sdma(xs4[sl], src_d, [], [("xs", sl)], ("xs", sl))

        def s0_sq(t):
            sl = t % 4
            S.op("act", lambda e: e.activation(out=junk, in_=xs4[sl], func=AF.Square, accum_out=ss[:, t:t + 1]),
                 reads=[("xs", sl)], writes=["junk", ("ss", t)])

        def s0_ts(t):
            S.op("dve", lambda e: e.tensor_scalar(out=rstd[:, t:t + 1], in0=ss[:, t:t + 1], scalar1=1.0 / D, scalar2=1e-6, op0=ALU.mult, op1=ALU.add),
                 reads=[("ss", t)], writes=[("rs", t)])

        def s0_sr(t):
            S.op("act", lambda e: e.activation(out=rstd[:, t:t + 1], in_=rstd[:, t:t + 1], func=AF.Sqrt), reads=[("rs", t)], writes=[("rs", t)])

        def s0_rc(t):
            S.op("dve", lambda e: e.reciprocal(out=rstd[:, t:t + 1], in_=rstd[:, t:t + 1]), reads=[("rs", t)], writes=[("rs", t)])

        def s0_apply(t):
            sl = t % 4
            c = 0 if t < 16 else 1
            xb_, xk_ = (xn_a, "xn") if t % 2 == 0 else (xn_s2, "xn2")
            return norm_apply(xs4[sl], rstd[:, t:t + 1],
                              (mod1[:, 2 * c + 1, :], [("A1", c)]),
                              (mod1[:, 2 * c, :], [("m1", c, 0), ("m1", c, 1)]),
                              xb_, xk_, t * 128, [("xs", sl)], ("rs", t), t, defer=True)

        NT0 = 18
        for t in range(3):
            s0_dma(t)
        for t in range(3):
            s0_sq(t)
        s0_ts(0); s0_sr(0); s0_rc(0)
        s0_ts(1); s0_sr(1)
        pend_cp = []
        for t in range(NT0):
            if t + 3 < NT0:
                s0_dma(t + 3)
                s0_sq(t + 3)
            if t + 2 < NT0:
                s0_ts(t + 2)
                s0_sr(t + 2)
            if t + 1 < NT0:
                s0_rc(t + 1)
            prev_cp = pend_cp
            pend_cp = s0_apply(t)
            for cp_ in prev_cp:
                cp_()
        for cp_ in pend_cp:
            cp_()
        S.alias([("kT", kvh, tc) for kvh in range(2) for tc in range(5)], [("xs", 3), "xn2"])

        def HTr(tlist):
            return [("HT", t, h) for t in tlist for h in (0, 1)]

        si_kv = load_slot(win_src(O_KV))
        k_units = [(tc, kvh) for tc in range(4) for kvh in range(2)]

        def k_proj(ui):
            tc, kvh = k_units[ui]
            t0 = tc * 512
            tl = list(range(tc * 4, tc * 4 + 4))
            bA = nb()
            qb = qraws[ui % 2]
            S.op("pe", mmgroup([(ps[bA][:], wsl[si_kv][:, k, (2 * kvh) * 128:(2 * kvh + 1) * 128], HT[:, k, t0:t0 + 512], k == 0, k == 7)
                                for k in range(8)]), reads=[("ws", si_kv)] + HTr(tl), writes=[("ps", bA)])
            S.op("act", lambda e, bA=bA, qb=qb: e.activation(out=qb, in_=ps[bA][:], func=AF.Copy), reads=[("ps", bA)], writes=[("qraw", ui % 2), ("psr", bA)])
            return bA

        def k_rope(ui, bA):
            tc, kvh = k_units[ui]
            t0 = tc * 512
            qb = qraws[ui % 2]
            bB = nb()
            S.op("pe", lambda e, bB=bB, qb=qb: e.matmul(ps[bB][:], lhsT=pmb[:], rhs=qb, start=True, stop=True),
                 reads=["pmb", ("qraw", ui % 2)], writes=[("ps", bB)])
            S.op("dve", lambda e, bA=bA, t0=t0: e.tensor_tensor(out=tmpA, in0=ps[bA][:], in1=C2[:, t0:t0 + 512], op=ALU.mult),
                 reads=[("ps", bA), ("psr", bA), "C2"], writes=["tmpA"])
            S.op("dve", lambda e, bB=bB, t0=t0: e.tensor_tensor(out=tmpB, in0=ps[bB][:], in1=S2[:, t0:t0 + 512], op=ALU.mult),
                 reads=[("ps", bB), "S2"], writes=["tmpB"])
            S.op("dve", lambda e, kvh=kvh, t0=t0, tc=tc: e.tensor_tensor(out=kT[:, kvh, t0:t0 + 512], in0=tmpA, in1=tmpB, op=ALU.add),
                 reads=["tmpA", "tmpB"], writes=[("kT", kvh, tc)])

        kbanks = {0: k_proj(0)}
        for ui in range(len(k_units)):
            if ui + 1 < len(k_units):
                kbanks[ui + 1] = k_proj(ui + 1)
            k_rope(ui, kbanks[ui])
        for kvh in range(2):
            bA = nb()
            S.op("pe", mmgroup([(ps[bA][:, 0:256], wsl[si_kv][:, k, (2 * kvh) * 128:(2 * kvh + 1) * 128], HT[:, k, 2048:2304], k == 0, k == 7)
                                for k in range(8)]), reads=[("ws", si_kv)] + HTr([16, 17]), writes=[("ps", bA)])
            S.op("act", lambda e, bA=bA, kvh=kvh: e.activation(out=kT[:, kvh, 2048:2304], in_=ps[bA][:, 0:256], func=AF.Copy),
                 reads=[("ps", bA)], writes=[("kT", kvh, 4)])
        si = load_slot(win_src(O_V, 128), 8, 128)
        for g in range(5):
            tl = list(range(g * 4, min(g * 4 + 4, 18)))
            bk = nb()
            lst = []
            for i, t in enumerate(tl):
                for k in range(8):
                    lst.append((ps[bk][:, i * 128:(i + 1) * 128], HT[:, k, t * 128:(t + 1) * 128], wsl[si][:, k, 0:128], k == 0, k == 7))
            S.op("pe", mmgroup(lst), reads=[("ws", si)] + HTr(tl), writes=[("ps", bk)])
            nt = len(tl)
            for kv in range(2):
                S.op("act", lambda e, bk=bk, g=g, nt=nt, kv=kv: e.activation(
                    out=Vx5[:, g * 4:g * 4 + nt, 1 + 2 * kv, :],
                    in_=ps[bk][:, 0:nt * 128].rearrange("p (t n) -> p t n", n=128)[:, :, kv * 64:(kv + 1) * 64], func=AF.Copy),
                    reads=[("ps", bk)], writes=[("V", g, kv)])

        _m2 = {}

        wsx = [XR[:, i * 2048:(i + 1) * 2048].bitcast(BF16).rearrange("p (k n) -> p k n", n=512) for i in range(2)]

        def mod2_load(n):
            i = n % 2
            gdma(wsx[i], wada_d[:, 2048 + n * 512:2048 + (n + 1) * 512].rearrange("(k p) n -> p k n", p=128), [], [("wsx", i)], ("wsx", i))

        def mod2_chunk(n):
            i = n % 2
            lst = []
            for j in range(4):
                col = n * 4 + j
                for k in range(8):
                    lst.append((psm[:, col:col + 1], wsx[i][:, k, j * 128:(j + 1) * 128], scb1[:, k:k + 1], k == 0, k == 7))
            S.op("pe", mmgroup(lst), reads=[("wsx", i), "scb1"], writes=[("ps", 7)])

        def mod2_finish():
            S.op("dve", lambda e: e.tensor_tensor(out=mod2T[:], in0=psm[:, 0:32], in1=b2T[:], op=ALU.add),
                 reads=[("ps", 7), "b2T"], writes=["mod2T"])
            S.op("dve", lambda e: e.scalar_tensor_tensor(out=a2T[:], in0=mod2T[:, 16:24], scalar=1.0, in1=ng2T[:], op0=ALU.add, op1=ALU.mult),
                 reads=["mod2T", "ng2T"], writes=["a2T"])

        def expand(vecT, dst, rd, wr, bf=False):
            for h in range(2):
                bk = nb()
                S.op("pe", mmgroup([(ps[bk][:, j * 128:(j + 1) * 128], vecT[:, h * 4 + j:h * 4 + j + 1].to_broadcast([128, 128]), identf[:], True, True)
                                    for j in range(4)]), reads=rd + ["identf"], writes=[("ps", bk)])
                S.op("act", lambda e, bk=bk, h=h: e.activation(out=dst[:, h * 512:(h + 1) * 512], in_=ps[bk][:], func=AF.Copy),
                     reads=[("ps", bk)], writes=[(wr, h)])

        mixer_keys_ucv = []
        for half in range(2):
            chunks = [2 * half, 2 * half + 1]
            cx_chunks = chunks
            cx0 = chunks[0]
            halo_tok = 1024 if half == 0 else 1023
            halo_col = 1025 if half == 0 else 0
            pad_col = 0 if half == 0 else 1025
            if half == 1:
                S.alias([("ucv", j, c) for j in range(4) for c in range(4)] + ["ucvpad"] + [("ucvh", j) for j in range(4)],
                        [("qT", j, c) for j in range(4) for c in range(4)])
            S.op("dve", lambda e, pc=pad_col: e.memset(ucv[:, :, pc:pc + 1], 0.0), writes=["ucvpad"])
            for sidx, off in enumerate((O_CX0, O_CX1)):
                si = load_slot(win_src(off))
                for jj in range(2):
                    j = sidx * 2 + jj
                    bH = nb()
                    lst = []
                    for q_ in range(2):
                        for k in range(8):
                            lst.append((ps[bH][:, q_:q_ + 1], wsl[si][:, k, (2 * jj + q_) * 128:(2 * jj + q_ + 1) * 128],
                                        HT[:, k, halo_tok:halo_tok + 1], k == 0, k == 7))
                    S.op("pe", mmgroup(lst), reads=[("ws", si)] + HTr([halo_tok // 128]), writes=[("ps", bH)])
                    S.op("act", lambda e, bH=bH: e.activation(out=hal[:, 0:1], in_=ps[bH][:, 0:1], func=AF.Copy), reads=[("ps", bH)], writes=["hal"])
                    S.op("dve", lambda e, bH=bH, j=j, hc=halo_col: e.tensor_tensor(out=ucv[:, j, hc:hc + 1], in0=ps[bH][:, 1:2], in1=hal[:, 0:1], op=ALU.mult),
                         reads=[("ps", bH), "hal"], writes=[("ucvh", j)])
                for c in cx_chunks:
                    tl = list(range(c * 4, c * 4 + 4))
                    for jj in range(2):
                        j = sidx * 2 + jj
                        bA = nb(); bB = nb()
                        S.op("pe", mmgroup([(ps[bA][:], wsl[si][:, k, (2 * jj) * 128:(2 * jj + 1) * 128], HT[:, k, c * 512:(c + 1) * 512], k == 0, k == 7)
                                            for k in range(8)]), reads=[("ws", si)] + HTr(tl), writes=[("ps", bA)])
                        S.op("pe", mmgroup([(ps[bB][:], wsl[si][:, k, (2 * jj + 1) * 128:(2 * jj + 2) * 128], HT[:, k, c * 512:(c + 1) * 512], k == 0, k == 7)
                                            for k in range(8)]), reads=[("ws", si)] + HTr(tl), writes=[("ps", bB)])
                        S.op("act", lambda e, bA=bA: e.activation(out=tmpA, in_=ps[bA][:], func=AF.Copy), reads=[("ps", bA)], writes=["tmpA"])
                        o0 = 1 + (c - cx0) * 512
                        S.op("dve", lambda e, bB=bB, j=j, o0=o0: e.tensor_tensor(out=ucv[:, j, o0:o0 + 512], in0=ps[bB][:], in1=tmpA, op=ALU.mult),
                             reads=[("ps", bB), "tmpA"], writes=[("ucv", j, c)])
            if half == 1:
                S.alias([("cvT", j, c) for j in range(4) for c in range(2)], [("oT", j, c) for j in range(4) for c in range(2)])
            si = load_slot(win_src(O_B))
            for ci, c in enumerate(chunks):
                tl = list(range(c * 4, c * 4 + 4))
                nbrs = [cc_ for cc_ in (c - 1, c, c + 1) if 0 <= cc_ < 4]
                for j in range(4):
                    o0 = (c - cx0) * 512
                    rdk = [("ucv", j, cc_) for cc_ in nbrs] + ["ucvpad", "wcs", "bcs", ("ucvh", j)]
                    S.op("dve", lambda e, j=j, o0=o0: e.tensor_scalar(out=tmpB, in0=ucv[:, j, o0:o0 + 512], scalar1=wcs[:, j * 3:j * 3 + 1],
                                                                      scalar2=bcs[:, j:j + 1], op0=ALU.mult, op1=ALU.add),
                         reads=rdk, writes=["tmpB"])
                    S.op("dve", lambda e, j=j, o0=o0: e.scalar_tensor_tensor(out=tmpB, in0=ucv[:, j, o0 + 1:o0 + 513], scalar=wcs[:, j * 3 + 1:j * 3 + 2],
                                                                             in1=tmpB, op0=ALU.mult, op1=ALU.add),
                         reads=rdk + ["tmpB"], writes=["tmpB"])
                    S.op("dve", lambda e, j=j, o0=o0: e.scalar_tensor_tensor(out=tmpB, in0=ucv[:, j, o0 + 2:o0 + 514], scalar=wcs[:, j * 3 + 2:j * 3 + 3],
                                                                             in1=tmpB, op0=ALU.mult, op1=ALU.add),
                         reads=rdk + ["tmpB"], writes=["tmpB"])
                    bk = nb()
                    S.op("pe", mmgroup([(ps[bk][:], wsl[si][:, k, j * 128:(j + 1) * 128], HT[:, k, c * 512:(c + 1) * 512], k == 0, k == 7)
                                        for k in range(8)]), reads=[("ws", si)] + HTr(tl), writes=[("ps", bk)])
                    S.op("dve", lambda e, bk=bk, j=j, ci=ci: e.tensor_tensor(out=cvT[:, j, ci * 512:(ci + 1) * 512], in0=ps[bk][:], in1=tmpB, op=ALU.mult),
                         reads=[("ps", bk), "tmpB"], writes=[("cvT", j, ci)])
            sg0_ = load_slot(win_src(O_GA0))
            sa_ = load_slot(wa_d.rearrange("(k p) n -> p k n", p=128), wide=True)
            wa_v = ER[:, sa_ * 4096:(sa_ + 1) * 4096].rearrange("p (k n) -> p k n", n=1024)
            sg = [sg0_, load_slot(win_src(O_GA1))]
            for ci, c in enumerate(chunks):
                tl = list(range(c * 4, c * 4 + 4))
                for oc in range(8):
                    bY = nb(); bG = nb()
                    S.op("pe", mmgroup([(ps[bY][:], wa_v[:, k, oc * 128:(oc + 1) * 128], cvT[:, k, ci * 512:(ci + 1) * 512], k == 0, k == 3)
                                        for k in range(4)]), reads=[("ws", sa_)] + [("cvT", k, ci) for k in range(4)], writes=[("ps", bY)])
                    sgi = sg[oc // 4]
                    S.op("pe", mmgroup([(ps[bG][:], wsl[sgi][:, k, (oc % 4) * 128:(oc % 4 + 1) * 128], HT[:, k, c * 512:(c + 1) * 512], k == 0, k == 7)
                                        for k in range(8)]), reads=[("ws", sgi)] + HTr(tl), writes=[("ps", bG)])
                    S.op("act", lambda e, bG=bG: e.activation(out=tmpA, in_=ps[bG][:], func=AF.Sigmoid), reads=[("ps", bG)], writes=["tmpA"])
                    S.op("dve", lambda e, bY=bY, oc=oc, ci=ci: e.tensor_tensor(out=mT[:, oc, ci * 512:(ci + 1) * 512], in0=ps[bY][:], in1=tmpA, op=ALU.mult),
                         reads=[("ps", bY), "tmpA"], writes=[("mT", oc, ci)])
            S.alias([("qT", j, c) for j in range(4) for c in range(4)],
                    [("ucv", j, c) for j in range(4) for c in range(4)] + ["ucvpad"] + [("ucvh", j) for j in range(4)])
            if half == 0:
                S.alias([("qraw", 0)], ["qraw"])
            q_slots = [load_slot(win_src(O_Q0)), load_slot(win_src(O_Q1))]
            q_units = [(sidx, ci, c, jj) for sidx in range(2) for ci, c in enumerate(chunks) for jj in range(2)]

            def q_proj(ui):
                sidx, ci, c, jj = q_units[ui]
                si = q_slots[sidx]
                tl = list(range(c * 4, c * 4 + 4))
                bA = nb()
                qb = qraws[ui % 2]
                S.op("pe", mmgroup([(ps[bA][:], wsl[si][:, k, (2 * jj) * 128:(2 * jj + 1) * 128], HT[:, k, c * 512:(c + 1) * 512], k == 0, k == 7)
                                    for k in range(8)]), reads=[("ws", si)] + HTr(tl), writes=[("ps", bA)])
                S.op("act", lambda e, bA=bA, qb=qb: e.activation(out=qb, in_=ps[bA][:], func=AF.Copy), reads=[("ps", bA)], writes=[("qraw", ui % 2), ("psr", bA)])
                return bA

            def q_rope(ui, bA):
                sidx, ci, c, jj = q_units[ui]
                j = sidx * 2 + jj
                qb = qraws[ui % 2]
                bB = nb()
                S.op("pe", lambda e, bB=bB, qb=qb: e.matmul(ps[bB][:], lhsT=pmb[:], rhs=qb, start=True, stop=True),
                     reads=["pmb", ("qraw", ui % 2)], writes=[("ps", bB)])
                S.op("dve", lambda e, bA=bA, c=c: e.tensor_tensor(out=tmpA, in0=ps[bA][:], in1=C2[:, c * 512:(c + 1) * 512], op=ALU.mult),
                     reads=[("ps", bA), ("psr", bA), "C2"], writes=["tmpA"])
                S.op("dve", lambda e, bB=bB, c=c: e.tensor_tensor(out=tmpB, in0=ps[bB][:], in1=S2[:, c * 512:(c + 1) * 512], op=ALU.mult),
                     reads=[("ps", bB), "S2"], writes=["tmpB"])
                S.op("dve", lambda e, j=j, ci=ci: e.tensor_tensor(out=qT[:, j, ci * 512:(ci + 1) * 512], in0=tmpA, in1=tmpB, op=ALU.add),
                     reads=["tmpA", "tmpB"], writes=[("qT", j, ci)])

            qbanks = {0: q_proj(0)}
            for ui in range(len(q_units)):
                if ui + 1 < len(q_units):
                    qbanks[ui + 1] = q_proj(ui + 1)
                q_rope(ui, qbanks[ui])
            S.alias([("P", 1, i, hh) for i in range(6) for hh in range(2)], ["junk", "ngt"])
            S.alias([("oT", j, c) for j in range(4) for c in range(2)], [("cvT", j, c) for j in range(4) for c in range(2)])
            def att_unit(nbk, kvh):
                n = half * 8 + nbk
                kts = []
                if n > 0:
                    kts.append(((n - 1) * 128, n - 1, 0))
                kts.append((n * 128, n, None))
                if n < 15:
                    kts.append(((n + 1) * 128, n + 1, 1))
                kts.append((2048, 16, None)); kts.append((2176, 17, None))
                pbi = _pb[0]; _pb[0] ^= 1
                return dict(nbk=nbk, kvh=kvh, n=n, ci=nbk // 4, qc0=nbk * 128, kts=kts, pbi=pbi)

            _pS = [0]; _pO = [0]; _pT = [0]
            poolT = [6]
            poolS = (0, 1, 2, 3) if half == 0 else (0, 1, 2, 3, 7)

            def nbS():
                b_ = poolS[_pS[0] % len(poolS)]; _pS[0] += 1
                return b_

            def nbO():
                b_ = (4, 5)[_pO[0] % 2]; _pO[0] += 1
                return b_

            def nbT():
                b_ = poolT[_pT[0] % len(poolT)]; _pT[0] += 1
                return b_

            def att_scores(u):
                kts = u["kts"]; kvh = u["kvh"]; pbi = u["pbi"]; qc0 = u["qc0"]; ci = u["ci"]
                Pb = Pbs[pbi]
                nk = len(kts)
                for p0 in range(0, nk, 2):
                    pr = kts[p0:p0 + 2]
                    w = len(pr) * 256
                    for hh in range(2):
                        bk = nbS()
                        lst = []
                        for i, (kc, vt, mk) in enumerate(pr):
                            lst.append((ps[bk][:, i * 256:(i + 1) * 256], kT[hh * 64:(hh + 1) * 64, kvh, kc:kc + 128],
                                        qT[hh * 64:(hh + 1) * 64, 2 * kvh:2 * kvh + 2, qc0:qc0 + 128], True, True))
                        kc_tcs = sorted(set(kc // 512 for (kc, _, _) in pr))
                        S.op("pe", mmgroup(lst), reads=[("kT", kvh, t_) for t_ in kc_tcs] + [("qT", 2 * kvh, ci), ("qT", 2 * kvh + 1, ci)],
                             writes=[("ps", bk)])
                        S.op("act", lambda e, bk=bk, p0=p0, hh=hh, w=w, npr=len(pr), Pb=Pb: e.activation(
                            out=Pb[:, p0:p0 + npr, hh * 256:(hh + 1) * 256],
                            in_=ps[bk][:, 0:w].rearrange("p (t n) -> p t n", n=256), func=AF.Exp, scale=0.125),
                            reads=[("ps", bk)], writes=[("P", pbi, p0 + i, hh) for i in range(len(pr))])
                for i, (kc, vt, mk) in enumerate(kts):
                    if mk is not None:
                        S.op("pool", lambda e, i=i, mk=mk, Pb=Pb: e.tensor_tensor(
                            out=Pb[:, i, :].rearrange("p (b n) -> p b n", n=128), in0=Pb[:, i, :].rearrange("p (b n) -> p b n", n=128),
                            in1=m01[:, mk * 128:(mk + 1) * 128].unsqueeze(1).to_broadcast([128, 4, 128]), op=ALU.mult),
                            reads=[("P", pbi, i, 0), ("P", pbi, i, 1), "m01"], writes=[("P", pbi, i, 0), ("P", pbi, i, 1)])

            def att_pv(u):
                kts = u["kts"]; kvh = u["kvh"]; pbi = u["pbi"]; qc0 = u["qc0"]; ci = u["ci"]; nbk = u["nbk"]
                Pb = Pbs[pbi]
                nk = len(kts)
                bO = nbO()
                lst = []
                for cb in range(4):
                    for i, (kc, vt, mk) in enumerate(kts):
                        lst.append((ps[bO][:, cb * 65:(cb + 1) * 65], Pb[:, i, cb * 128:(cb + 1) * 128],
                                    Vx[:, vt, (1 + 2 * kvh) * 64:(1 + 2 * kvh) * 64 + 65], i == 0, i == nk - 1))
                vrd = [("V", vt // 4, kvh) for (_, vt, _) in kts] + [("Vones", 0), ("Vones", 2), ("Vones", 4)]
                prd = [("P", pbi, i, hh) for i in range(nk) for hh in range(2)]
                S.op("pe", mmgroup(lst), reads=vrd + prd, writes=[("ps", bO)])
                O4 = ps[bO][:, 0:260].rearrange("p (c n) -> p c n", n=65)
                oi = _ot[0]; _ot[0] ^= 1
                ot = otm[oi]
                S.op("dve", lambda e, O4=O4, kvh=kvh: e.tensor_tensor(out=dsm[:, 0:4], in0=O4[:, :, 64], in1=esb[:, kvh * 4:(kvh + 1) * 4], op=ALU.add),
                     reads=[("ps", bO), "esb"], writes=["dsm"])
                S.op("dve", lambda e: e.reciprocal(out=dsm[:, 4:8], in_=dsm[:, 0:4]), reads=["dsm"], writes=["dsm"])
                for hh in range(2):
                    S.op("dve", lambda e, O4=O4, hh=hh, ot=ot: e.tensor_tensor(
                        out=ot[:, :].rearrange("p (jj hh d) -> p jj hh d", jj=2, hh=2)[:, :, hh, :],
                        in0=O4[:, hh * 2:hh * 2 + 2, 0:64],
                        in1=dsm[:, 4 + hh * 2:4 + hh * 2 + 2].unsqueeze(2).to_broadcast([128, 2, 64]), op=ALU.mult),
                        reads=[("ps", bO), "dsm"], writes=[("otm", oi, hh)])
                u["oi"] = oi

            def att_tr(u):
                kvh = u["kvh"]; qc0 = u["qc0"]; ci = u["ci"]; nbk = u["nbk"]; oi = u["oi"]
                ot = otm[oi]
                bT = nbT()
                psb = ps[bT][:, :].bitcast(BF16)
                S.op("pe", lambda e, ot=ot, psb=psb: [e.transpose(psb[:, jj * 128:(jj + 1) * 128], ot[:, jj * 128:(jj + 1) * 128], identb[:]) for jj in range(2)][-1],
                     reads=[("otm", oi, 0), ("otm", oi, 1), "identb"], writes=[("ps", bT)])
                S.op("act", lambda e, psb=psb, kvh=kvh, qc0=qc0: e.activation(
                    out=oT[:, 2 * kvh:2 * kvh + 2, qc0:qc0 + 128], in_=psb[:, 0:256].rearrange("p (j n) -> p j n", n=128), func=AF.Copy),
                    reads=[("ps", bT)], writes=[("oTp", 2 * kvh, ci, nbk, 0), ("oT", 2 * kvh, ci), ("oT", 2 * kvh + 1, ci)])

            units = [att_unit(nbk, kvh) for nbk in range(8) for kvh in range(2)]
            if half == 0:
                S.alias([("wsx", 0), ("wsx", 1)], [("m1", c_, n_) for c_ in range(2) for n_ in range(4)] + [("A1", 0), ("A1", 1), "mod1b", "mod1c"])
                mod2_load(0); mod2_load(1)
            sg_D = [load_slot(win_src(O_GB0)), load_slot(win_src(O_GB1))]
            sb_ = load_slot(wb_d.rearrange("(k p) n -> p k n", p=128), wide=True)
            NU = len(units)
            for ui in range(NU + 2):
                if ui < NU:
                    att_scores(units[ui])
                if 0 <= ui - 1 < NU:
                    att_pv(units[ui - 1])
                if 0 <= ui - 2 < NU:
                    att_tr(units[ui - 2])
                if half == 0 and ui >= 2 and ui % 2 == 0 and ui // 2 - 1 < 7:
                    n_ = ui // 2 - 1
                    mod2_chunk(n_)
                    if n_ + 2 < 8:
                        mod2_load(n_ + 2)
            if half == 0:
                mod2_chunk(7)
                mod2_finish()
                _m2lock[0] = False
                expand(mod2T[:, 0:8], G1, ["mod2T"], "G1")
            S.alias(["junk", "ngt"], [("P", 1, i, hh) for i in range(6) for hh in range(2)])
            wb_v = ER[:, sb_ * 4096:(sb_ + 1) * 4096].rearrange("p (k n) -> p k n", n=1024)
            sg = sg_D
            for ci, c in enumerate(chunks):
                tl = list(range(c * 4, c * 4 + 4))
                ord_ = [("oTp", 2 * kvh, ci, nbk, 0) for kvh in range(2) for nbk in range(ci * 4, ci * 4 + 4)]
                for oc in range(8):
                    bY = nb(); bG = nb()
                    S.op("pe", mmgroup([(ps[bY][:], wb_v[:, k, oc * 128:(oc + 1) * 128], oT[:, k, ci * 512:(ci + 1) * 512], k == 0, k == 3)
                                        for k in range(4)]), reads=[("ws", sb_)] + ord_ + [("oT", k, ci) for k in range(4)], writes=[("ps", bY)])
                    sgi = sg[oc // 4]
                    S.op("pe", mmgroup([(ps[bG][:], wsl[sgi][:, k, (oc % 4) * 128:(oc % 4 + 1) * 128], HT[:, k, c * 512:(c + 1) * 512], k == 0, k == 7)
                                        for k in range(8)]), reads=[("ws", sgi)] + HTr(tl), writes=[("ps", bG)])
                    S.op("act", lambda e, bG=bG: e.activation(out=tmpA, in_=ps[bG][:], func=AF.Sigmoid), reads=[("ps", bG)], writes=["tmpA"])
                    S.op("dve", lambda e, bY=bY: e.tensor_tensor(out=tmpC.bitcast(BF16)[:, 0:512], in0=ps[bY][:], in1=tmpA, op=ALU.mult),
                         reads=[("ps", bY), "tmpA"], writes=["tmpC"])
                    S.op("dve", lambda e, oc=oc, ci=ci: e.tensor_tensor(out=mT[:, oc, ci * 512:(ci + 1) * 512], in0=mT[:, oc, ci * 512:(ci + 1) * 512],
                                                                        in1=tmpC.bitcast(BF16)[:, 0:512], op=ALU.add),
                         reads=[("mT", oc, ci), "tmpC"], writes=[("mT", oc, ci)])
            so = [load_slot(wo_d[:, h * 512:(h + 1) * 512].rearrange("(k p) n -> p k n", p=128)) for h in range(2)]
            if half == 0:
                S.alias([("x1", t) for t in range(8)], [("m1", c, n) for c in range(2) for n in range(4)] + [("A1", 0), ("A1", 1), ("xs", 0), ("xs", 1), ("xs", 2), "xn", "scb", "mod1b", "mod1c", ("wsx", 0), ("wsx", 1)])
            else:
                S.alias([("x1", t) for t in range(8, 16)], [("kT", kvh, tc) for kvh in range(2) for tc in range(5)] + [("V", g, kv) for g in range(5) for kv in range(2)]
                        + [("Vones", 0), ("Vones", 2), ("Vones", 4), "C2", "S2"])
            for tt in range(8):
                t = half * 8 + tt
                sdma(x1[:, t, :], x_d[t * 128:(t + 1) * 128, :], [], [("x1", t)], ("x1", t))
            for h in range(2):
                for tt in range(8):
                    t = half * 8 + tt
                    ci = tt // 4
                    bk = nb()
                    S.op("pe", mmgroup([(ps[bk][:], mT[:, k, tt * 128:(tt + 1) * 128], wsl[so[h]][:, k, :], k == 0, k == 7) for k in range(8)]),
                         reads=[("ws", so[h])] + [("mT", k, ci) for k in range(8)], writes=[("ps", bk)])
                    S.op("dve", lambda e, bk=bk, h=h: e.tensor_tensor(out=tmpA, in0=ps[bk][:], in1=G1[:, h * 512:(h + 1) * 512], op=ALU.mult),
                         reads=[("ps", bk), ("G1", h)], writes=["tmpA"])
                    S.op("dve", lambda e, t=t, h=h: e.tensor_tensor(out=x1[:, t, h * 512:(h + 1) * 512], in0=x1[:, t, h * 512:(h + 1) * 512], in1=tmpA, op=ALU.add),
                         reads=[("x1", t), "tmpA"], writes=[("x1", t)])
                    if h == 1:
                        norm_stats(x1[:, t, :], ss2[:, t:t + 1], None, [("x1", t)], ("n2", t), finish=False)

        A2b = OV[:, 1536:2560]; B2b = OV[:, 2560:3584]; xn_b = OV[:, 3584:4608]
        S.alias([("A2b", 0), ("A2b", 1), ("B2b", 0), ("B2b", 1), "xn", "G2b"],
                ["tmpA", "tmpB", "tmpC", "rd0", "rd1", "qraw", ("qraw", 0), ("qraw", 1)] + [("P", 0, i, hh) for i in range(6) for hh in range(2)])
        expand(a2T[:, :], A2b, ["a2T"], "A2b")
        expand(mod2T[:, 8:16], B2b, ["mod2T"], "B2b")
        lg = MT[:, 0:1152].bitcast(F32).rearrange("p (t n) -> p t n", n=36)
        RKEYS = [("lg", t) for t in range(16)] + ["rt", "gmax", "gsh", "gm", "gsum", "m1v", "k1", "ml2", "m2v", "k2", "dd", "s1", "s2"] + [("ml", g) for g in range(4)]
        S.alias(RKEYS, [("mT", oc, ci) for oc in range(8) for ci in range(2)])
        xn_c = OV[:, 0:1024]
        S.alias(["xnc"], ["ngt"])
        norm_rstd(ss2, rstd2, [("ss", ("n2", t)) for t in range(16)], "rs2all")

        def n2_router(t):
            bk = nb()
            S.op("pe", mmgroup([(ps[bk][:, 0:36], HT[:, k, t * 128:(t + 1) * 128], wrt[:, k, :], k == 0, k == 7) for k in range(8)]),
                 reads=HTr([t]) + ["wrt"], writes=[("ps", bk)])
            return bk

        def n2_lgadd(t, bk):
            S.op("dve", lambda e, bk=bk, t=t: e.tensor_tensor(out=lg[:, t, :], in0=ps[bk][:, 0:36], in1=brtb[:], op=ALU.add),
                 reads=[("ps", bk), "brtb"], writes=[("lg", t)])

        prev = None
        for t in range(16):
            xb_, xk_ = (xn_b, "xn") if t % 2 == 0 else (xn_c, "xnc")
            norm_apply(x1[:, t, :], rstd2[:, t:t + 1], (A2b, [("A2b", 0), ("A2b", 1)]), (B2b, [("B2b", 0), ("B2b", 1)]),
                       xb_, xk_, t * 128, [("x1", t)], "rs2all", t)
            bk = n2_router(t)
            if prev is not None:
                n2_lgadd(*prev)
            prev = (t, bk)
        n2_lgadd(*prev)
        RT = MT[:, 1152:8192].bitcast(F32)
        r_gsh = RT[:, 0:64].rearrange("p (t n) -> p t n", n=4)
        r_gm = RT[:, 64:128].rearrange("p (t n) -> p t n", n=4)
        r_ml = RT[:, 128:640].rearrange("p (t n) -> p t n", n=32)
        r_k1 = RT[:, 640:1152].rearrange("p (t n) -> p t n", n=32)
        r_ml2 = RT[:, 1152:1664].rearrange("p (t n) -> p t n", n=32)
        r_k2 = RT[:, 1664:2176].rearrange("p (t n) -> p t n", n=32)
        r_cmb = RT[:, 2176:2688].rearrange("p (t n) -> p t n", n=32)
        sm = RT[:, 2688:2944].rearrange("p (a t) -> p a t", t=16)
        combb = SB("combb", [128, 16, 32], BF16)
        lgk = [("lg", t) for t in range(16)]

        def V_(eng, fn, rd, wr):
            S.op(eng, fn, reads=rd, writes=wr)

        lgg = lg[:, :, 0:4]; lge = lg[:, :, 4:36]
        V_("dve", lambda e: e.tensor_reduce(out=sm[:, 0, :], in_=lgg, axis=AX.X, op=ALU.max), lgk, ["gmax"])
        V_("dve", lambda e: e.tensor_tensor(out=r_gsh, in0=lgg, in1=sm[:, 0, :].unsqueeze(2).to_broadcast([128, 16, 4]), op=ALU.subtract), lgk + ["gmax"], ["gsh"])
        V_("dve", lambda e: e.tensor_tensor(out=r_gm, in0=lgg, in1=sm[:, 0, :].unsqueeze(2).to_broadcast([128, 16, 4]), op=ALU.is_equal), lgk + ["gmax"], ["gm"])
        V_("act", lambda e: e.activation(out=r_gsh, in_=r_gsh, func=AF.Exp), ["gsh"], ["gsh"])
        V_("dve", lambda e: e.tensor_reduce(out=sm[:, 1, :], in_=r_gsh, axis=AX.X, op=ALU.add), ["gsh"], ["gsum"])
        V_("dve", lambda e: e.reciprocal(out=sm[:, 1, :], in_=sm[:, 1, :]), ["gsum"], ["gsum"])
        V_("dve", lambda e: e.tensor_scalar(out=r_gm, in0=r_gm, scalar1=BIG, scalar2=-BIG, op0=ALU.mult, op1=ALU.add), ["gm"], ["gm"])
        for g in range(4):
            V_("dve", lambda e, g=g: e.tensor_tensor(out=r_ml[:, :, g * 8:(g + 1) * 8], in0=lge[:, :, g * 8:(g + 1) * 8],
                                                     in1=r_gm[:, :, g:g + 1].to_broadcast([128, 16, 8]), op=ALU.add), lgk + ["gm"], [("ml", g)])
        mlk = [("ml", g) for g in range(4)]
        V_("dve", lambda e: e.tensor_reduce(out=sm[:, 2, :], in_=r_ml, axis=AX.X, op=ALU.max), mlk, ["m1v"])
        V_("dve", lambda e: e.tensor_tensor(out=r_k1, in0=r_ml, in1=sm[:, 2, :].unsqueeze(2).to_broadcast([128, 16, 32]), op=ALU.is_equal), mlk + ["m1v"], ["k1"])
        V_("dve", lambda e: e.scalar_tensor_tensor(out=r_ml2, in0=r_k1, scalar=-BIG, in1=r_ml, op0=ALU.mult, op1=ALU.add), mlk + ["k1"], ["ml2"])
        V_("dve", lambda e: e.tensor_reduce(out=sm[:, 3, :], in_=r_ml2, axis=AX.X, op=ALU.max), ["ml2"], ["m2v"])
        V_("dve", lambda e: e.tensor_tensor(out=r_k2, in0=r_ml2, in1=sm[:, 3, :].unsqueeze(2).to_broadcast([128, 16, 32]), op=ALU.is_equal), ["ml2", "m2v"], ["k2"])
        V_("dve", lambda e: e.tensor_tensor(out=sm[:, 4, :], in0=sm[:, 3, :], in1=sm[:, 2, :], op=ALU.subtract), ["m1v", "m2v"], ["dd"])
        V_("act", lambda e: e.activation(out=sm[:, 4, :], in_=sm[:, 4, :], func=AF.Exp), ["dd"], ["dd"])
        V_("dve", lambda e: e.tensor_scalar(out=sm[:, 5, :], in0=sm[:, 4, :], scalar1=1.0, scalar2=None, op0=ALU.add), ["dd"], ["s1"])
        V_("dve", lambda e: e.reciprocal(out=sm[:, 5, :], in_=sm[:, 5, :]), ["s1"], ["s1"])
        V_("dve", lambda e: e.tensor_tensor(out=sm[:, 5, :], in0=sm[:, 5, :], in1=sm[:, 1, :], op=ALU.mult), ["s1", "gsum"], ["s1"])
        V_("dve", lambda e: e.tensor_tensor(out=sm[:, 6, :], in0=sm[:, 5, :], in1=sm[:, 4, :], op=ALU.mult), ["s1", "dd"], ["s2"])
        V_("dve", lambda e: e.tensor_tensor(out=r_k1, in0=r_k1, in1=sm[:, 5, :].unsqueeze(2).to_broadcast([128, 16, 32]), op=ALU.mult), ["k1", "s1", "ml2"], ["k1"])
        V_("dve", lambda e: e.tensor_tensor(out=r_k2, in0=r_k2, in1=sm[:, 6, :].unsqueeze(2).to_broadcast([128, 16, 32]), op=ALU.mult), ["k2", "s2"], ["k2"])
        V_("dve", lambda e: e.tensor_tensor(out=combb[:], in0=r_k1, in1=r_k2, op=ALU.add), ["k1", "k2"], ["combb"])

        G2b = OV[:, 4608:5120].bitcast(BF16)
        G2f = A2b
        S.alias([("G2f", 0), ("G2f", 1)], [("A2b", 0), ("A2b", 1)])
        expand(mod2T[:, 24:32], G2f, ["mod2T"], "G2f")
        S.op("dve", lambda e: e.tensor_copy(out=G2b, in_=G2f), reads=[("G2f", 0), ("G2f", 1)], writes=["G2b"])
        er_old = [("ws", i) for i in range(3)] + [("qT", j, c) for j in range(4) for c in range(4)] + \
                 [("oT", j, c) for j in range(4) for c in range(2)] + [("oTp", 2 * kvh, ci, nbk, hh) for kvh in range(2) for ci in range(2)
                                                                      for nbk in range(ci * 4, ci * 4 + 4) for hh in range(2)]
        S.alias([("wu", s) for s in range(4)] + [("wd", s) for s in range(4)], er_old)
        wu = [ER[:, s * 6144:s * 6144 + 4096].rearrange("p (k n) -> p k n", n=512) for s in range(4)]
        wd = [ER[:, s * 6144 + 4096:(s + 1) * 6144].rearrange("p (f n) -> p f n", n=1024) for s in range(4)]
        MTb = MT
        cbs = [MTb[:, i * 1024:(i + 1) * 1024].rearrange("p (e n) -> p e n", n=512) for i in range(2)]
        sab = [MTb[:, 2048 + i * 512:2048 + (i + 1) * 512] for i in range(2)]
        t2b = [MTb[:, 3072 + i * 512:3072 + (i + 1) * 512] for i in range(2)]
        S.alias([("cbs", 0), ("cbs", 1), ("cbs2", 0), ("cbs2", 1), ("sab", 0), ("sab", 1), ("t2b", 0), ("t2b", 1)] + [("actT", bi_, el, j) for bi_ in range(2) for el in range(2) for j in range(2)],
                RKEYS)

        def load_expert(e_, fold=True):
            s = e_ % 4
            gdma(wu[s], wup_d[e_].rearrange("(k p) n -> p k n", p=128), [], [("wu", s)], ("wu", s))
            gdma(wd[s], wdn_d[e_].rearrange("(f p) n -> p f n", p=128), [], [("wd", s)], ("wd", s))
            if fold:
                fold_expert(e_)

        def fold_expert(e_):
            s = e_ % 4
            S.op("dve", lambda e, s=s: e.tensor_tensor(out=wd[s], in0=wd[s], in1=G2b.unsqueeze(1).to_broadcast([128, 2, 1024]), op=ALU.mult),
                 reads=[("wd", s), "G2b"], writes=[("wd", s)])

        UPB = [(0, 1), (2, 3)]
        ACB = [(4, 5), (6, 7)]
        up_i = [0]; ac_i = [0]; sa_i = [0]
        actTs = [MTb[:, 4096 + i * 2048:4096 + (i + 1) * 2048].rearrange("p (e f n) -> p e f n", e=2, f=2) for i in range(2)]
        for e_ in range(4):
            load_expert(e_)

        def up_stage(sidx):
            g, c = divmod(sidx, 4)
            tl = list(range(c * 4, c * 4 + 4))
            bi = sidx % 2
            actT = actTs[bi]
            for el in range(2):
                ex = 2 * g + el
                s_ = ex % 4
                for j in range(2):
                    ub = UPB[up_i[0]]; up_i[0] ^= 1
                    S.op("pe", mmgroup([(ps[ub[0]][:], wu[s_][:, k, j * 128:(j + 1) * 128], HT[:, k, c * 512:(c + 1) * 512], k == 0, k == 7) for k in range(8)]),
                         reads=[("wu", s_)] + HTr(tl), writes=[("ps", ub[0])])
                    S.op("pe", mmgroup([(ps[ub[1]][:], wu[s_][:, k, (2 + j) * 128:(3 + j) * 128], HT[:, k, c * 512:(c + 1) * 512], k == 0, k == 7) for k in range(8)]),
                         reads=[("wu", s_)] + HTr(tl), writes=[("ps", ub[1])])
                    si_ = sa_i[0]; sa_i[0] ^= 1
                    S.op("act", lambda e, ub=ub, si_=si_: e.activation(out=sab[si_], in_=ps[ub[0]][:], func=AF.Silu), reads=[("ps", ub[0])], writes=[("sab", si_)])
                    S.op("dve", lambda e, ub=ub, si_=si_: e.tensor_tensor(out=t2b[si_], in0=ps[ub[1]][:], in1=sab[si_], op=ALU.mult),
                         reads=[("ps", ub[1]), ("sab", si_)], writes=[("t2b", si_)])
                    S.op("dve", lambda e, si_=si_, el=el, j=j, bi=bi, actT=actT: e.tensor_tensor(out=actT[:, el, j, :], in0=t2b[si_], in1=cbs[bi][:, el, :], op=ALU.mult),
                         reads=[("t2b", si_), ("cbs", bi), ("cbs2", bi)], writes=[("actT", bi, el, j)])

        def cb_stage(sidx):
            g, c = divmod(sidx, 4)
            bi = sidx % 2
            ub = UPB[up_i[0]]; up_i[0] ^= 1
            for el in range(2):
                ex = 2 * g + el
                S.op("pe", mmgroup([(ps[ub[el]][:, tt * 128:(tt + 1) * 128], combb[:, c * 4 + tt, ex:ex + 1].to_broadcast([128, 128]), identb[:], True, True)
                                    for tt in range(4)]), reads=["combb", "identb"], writes=[("ps", ub[el])])
                S.op("act", lambda e, ub=ub, bi=bi, el=el: e.activation(out=cbs[bi][:, el, :], in_=ps[ub[el]][:], func=AF.Copy),
                     reads=[("ps", ub[el])], writes=[("cbs", bi)] if el == 0 else [("cbs2", bi)])

        def down_stage(sidx):
            g, c = divmod(sidx, 4)
            bi = sidx % 2
            actT = actTs[bi]
            for h in range(2):
                for tp in range(2):
                    ab = ACB[ac_i[0]]; ac_i[0] ^= 1
                    for ti in range(2):
                        tt = tp * 2 + ti
                        lst = []
                        for el in range(2):
                            s_ = (2 * g + el) % 4
                            for f in range(2):
                                lst.append((ps[ab[ti]][:], actT[:, el, f, tt * 128:(tt + 1) * 128], wd[s_][:, f, h * 512:(h + 1) * 512],
                                            el == 0 and f == 0, el == 1 and f == 1))
                        S.op("pe", mmgroup(lst), reads=[("wd", (2 * g) % 4), ("wd", (2 * g + 1) % 4)] + [("actT", bi, el, f) for el in range(2) for f in range(2)],
                             writes=[("ps", ab[ti])])
                        t = c * 4 + tt
                        S.op("dve", lambda e, b=ab[ti], t=t, h=h: e.tensor_tensor(out=x1[:, t, h * 512:(h + 1) * 512], in0=ps[b][:],
                                                                                 in1=x1[:, t, h * 512:(h + 1) * 512], op=ALU.add),
                             reads=[("ps", ab[ti]), ("x1", t)], writes=[("x1", t)])
            if c == 3:
                for el in range(2):
                    nx = 2 * g + 4 + el
                    if nx < NE:
                        load_expert(nx, fold=False)
            if c == 1 and g + 1 >= 2:
                for el in range(2):
                    nx = 2 * (g + 1) + el
                    if nx < NE:
                        fold_expert(nx)

        def final_tiles(tlist):
            for t in tlist:
                S.op("act", lambda e, t=t: e.activation(out=junk, in_=x1[:, t, :], func=AF.Square, accum_out=ss3[:, t:t + 1]),
                     reads=[("x1", t)], writes=["junk", ("ss3", t)])
                S.op("dve", lambda e, t=t: e.tensor_scalar(out=rstd3[:, t:t + 1], in0=ss3[:, t:t + 1], scalar1=1.0 / D, scalar2=1e-6, op0=ALU.mult, op1=ALU.add),
                     reads=[("ss3", t)], writes=[("rs3", t)])
                S.op("act", lambda e, t=t: e.activation(out=rstd3[:, t:t + 1], in_=rstd3[:, t:t + 1], func=AF.Sqrt), reads=[("rs3", t)], writes=[("rs3", t)])
                S.op("dve", lambda e, t=t: e.reciprocal(out=rstd3[:, t:t + 1], in_=rstd3[:, t:t + 1]), reads=[("rs3", t)], writes=[("rs3", t)])
                S.op("dve", lambda e, t=t: e.scalar_tensor_tensor(out=x1[:, t, :], in0=x1[:, t, :], scalar=rstd3[:, t:t + 1], in1=ngt, op0=ALU.mult, op1=ALU.mult),
                     reads=[("x1", t), ("rs3", t), "FGa"], writes=[("x1", t)])
                sdma(out_d[t * 128:(t + 1) * 128, :], x1[:, t, :], [("x1", t)], [("out", t)], ("out", t % 4))

        S.alias(["FGa"], ["ngt", "xnc"])
        sdma(ngt, fg_d.partition_broadcast(128), [], ["FGa"], "c13")
        NS = (NE // 2) * 4
        cb_stage(0)
        up_stage(0)
        cb_stage(1)
        for sidx in range(1, NS):
            up_stage(sidx)
            if sidx + 1 < NS:
                cb_stage(sidx + 1)
            down_stage(sidx - 1)
            if sidx - 1 >= NS - 4:
                c_ = (sidx - 1) % 4
                final_tiles(range(c_ * 4, c_ * 4 + 4))
        down_stage(NS - 1)
        final_tiles(range(12, 16))

        S.emit()
        build_program.stats = S.stats
    return nc


def _host_consts():
    n_freq = 16
    inv_freq = (np.float32(10000.0) ** (-np.arange(n_freq, dtype=np.float32) / np.float32(n_freq))).astype(np.float32)
    tok = np.arange(L)
    row = (tok // 64).astype(np.float32)
    col = (tok % 64).astype(np.float32)
    ang = np.concatenate([row[:, None] * inv_freq[None, :], col[:, None] * inv_freq[None, :]], axis=1).astype(np.float32)
    cos = np.cos(ang).astype(np.float32); sin = np.sin(ang).astype(np.float32)
    p = np.arange(128)
    c2 = cos[:, p % 32].T.copy()
    sgn = np.where((p % 64) < 32, -1.0, 1.0).astype(np.float32)
    s2 = (sin[:, p % 32].T * sgn[:, None]).astype(np.float32)
    ident = np.eye(128, dtype=np.float32)
    j = np.arange(128)[:, None]; i = np.arange(128)[None, :]
    m_prev = (j >= i).astype(np.float32)
    m_next = (j <= i).astype(np.float32)
    m01 = np.concatenate([m_prev, m_next], axis=1)
    m_ = np.arange(128)
    swm = (m_ // 64) * 64 + ((m_ % 64) + 32) % 64
    pm = np.zeros((128, 128), dtype=np.float32)
    pm[swm, m_] = 1.0
    return np.ascontiguousarray(c2), np.ascontiguousarray(s2), ident, np.ascontiguousarray(m01), pm


def _win_perm():
    b0, cg0, xin0, q0, k0, v0, ga0, gb0 = 0, 512, 1024, 1536, 2048, 2176, 2304, 3328

    def sw(base, nheads):
        idx = []
        for h in range(nheads):
            for i in range(64):
                idx.append(base + h * 64 + (i + 32) % 64)
        return idx

    cols = []
    kh = [list(range(k0 + h * 64, k0 + (h + 1) * 64)) for h in range(2)]
    ksw = sw(k0, 2)
    khs = [ksw[h * 64:(h + 1) * 64] for h in range(2)]
    cols += kh[0] + kh[0] + khs[0] + khs[0] + kh[1] + kh[1] + khs[1] + khs[1]
    cols += list(range(v0, v0 + 128))
    for jj in range(4):
        cols += list(range(cg0 + jj * 128, cg0 + (jj + 1) * 128)) + list(range(xin0 + jj * 128, xin0 + (jj + 1) * 128))
    cols += list(range(b0, b0 + 512))
    cols += list(range(ga0, ga0 + 1024))
    qsw = sw(q0, 8)
    for jj in range(4):
        cols += list(range(q0 + jj * 128, q0 + (jj + 1) * 128)) + qsw[jj * 128:(jj + 1) * 128]
    cols += list(range(gb0, gb0 + 1024))
    assert len(cols) == NCOLS
    return np.array(cols, dtype=np.int64)


_NC_CACHE = {}


def kernel(x, c, ctx, c_ctx, w_ada, b_ada, norm1_g, w_in, w_conv, b_conv, w_a, w_b, sink, w_o,
           norm2_g, w_group, b_group, w_router, b_router, w_up, w_down, final_g):
    f = lambda a: np.ascontiguousarray(np.asarray(a, dtype=np.float32))
    x = f(x); c = f(c); ctx = f(ctx); c_ctx = f(c_ctx)
    if "nc" not in _NC_CACHE:
        _NC_CACHE["nc"] = build_program()
    nc = _NC_CACHE["nc"]
    c2, s2, ident, m01, pm = _host_consts()
    w_in0 = f(w_in)[0]
    win = np.ascontiguousarray(w_in0[:, _win_perm()])
    b_ada0 = f(b_ada)[0]
    shared = dict(
        wada=f(w_ada)[0], bada1=np.ascontiguousarray(b_ada0[:2048]),
        bada2=np.ascontiguousarray(b_ada0[2048:].reshape(32, 128).T),
        ng1=f(norm1_g)[0], ng2=np.ascontiguousarray(f(norm2_g)[0].reshape(8, 128).T), fg=f(final_g),
        win=win,
        wc=np.ascontiguousarray(f(w_conv)[0].T.reshape(4, 128, 3).transpose(1, 0, 2).reshape(128, 12)),
        bc=np.ascontiguousarray(f(b_conv)[0].reshape(4, 128).T),
        wa=f(w_a)[0], wb=f(w_b)[0], wo=f(w_o)[0],
        sinkp=np.ascontiguousarray(f(sink)[0][[0, 2, 1, 3, 4, 6, 5, 7]].reshape(1, 8)),
        wrt=np.ascontiguousarray(np.concatenate([f(w_group)[0], f(w_router)[0]], axis=1)),
        brt=np.ascontiguousarray(np.concatenate([f(b_group)[0], f(b_router)[0]], axis=0)),
        wup=f(w_up)[0], wdn=f(w_down)[0],
        c2=c2, s2=s2, ident=ident, m01=m01, pm=pm,
    )
    cct = np.ascontiguousarray(c_ctx.reshape(8, 128).T)
    in_maps = []
    for b in range(8):
        m = dict(shared)
        m["x"] = x[b]
        m["ctx"] = ctx[b]
        m["cc"] = np.ascontiguousarray(np.concatenate([c[b].reshape(8, 128).T, cct], axis=1))
        in_maps.append(m)
    res = run_bass_kernel_spmd(nc, in_maps, core_ids=list(range(8)))
    return np.stack([np.asarray(res.results[b]["out"], dtype=np.float32) for b in range(8)], axis=0)
```
